# Optimizing a Trainium2 kernel written in Bass

```python
import math
import jax, jax.numpy as jnp
from jax import lax
import numpy as np


D_MODEL = 1024
BATCH = 8
SEQ = 2048
DEPTH = 2

HGRN_HEADS = 4
HGRN_HEAD_DIM = 128
HGRN_WIDTH = HGRN_HEADS * HGRN_HEAD_DIM
HGRN_CHUNK = 64
MLA_HEADS = 8
MLA_NOPE_DIM = 64
MLA_ROPE_DIM = 32
MLA_QK_DIM = MLA_NOPE_DIM + MLA_ROPE_DIM
MLA_V_DIM = 64
MLA_Q_RANK = 384
MLA_KV_RANK = 256
MLA_WIDTH = MLA_HEADS * MLA_V_DIM
ROPE_THETA = 10000.0
Q_BLOCK = 128
POOL_WINDOWS = (2, 4, 8, 16)
POOL_GROUPS = 4
POOL_GROUP_DIM = 128
POOL_WIDTH = POOL_GROUPS * POOL_GROUP_DIM
N_EXPERTS = 32
TOP_K = 4
EXPERT_FF = D_MODEL
SWIGLU_LIMIT = 7.0
SWIGLU_ALPHA = 1.702
NORM_EPS = 1e-6
IN_WIDTHS = (HGRN_WIDTH, HGRN_WIDTH, HGRN_WIDTH, HGRN_WIDTH,
             MLA_Q_RANK, MLA_KV_RANK, MLA_ROPE_DIM, POOL_WIDTH,
             D_MODEL, D_MODEL, D_MODEL)
IN_TOTAL = sum(IN_WIDTHS)

kernel_name = 'hybrid_hgrn2_mla_pool_moe_adaln'


def _rmsnorm(x, g):
    xf = x.astype(jnp.float32)
    y = xf * lax.rsqrt(jnp.mean(xf * xf, axis=-1, keepdims=True) + NORM_EPS)
    return (y * g.astype(jnp.float32)).astype(x.dtype)


def _modulate(xn, shift, scale):
    return xn * (1.0 + scale[:, None, :]) + shift[:, None, :]


def _split_cols(t):
    idx, acc = [], 0
    for w in IN_WIDTHS[:-1]:
        acc += w
        idx.append(acc)
    return jnp.split(t, idx, axis=-1)


def _hgrn2(q, f_logit, i, g, lb, onorm_g):
    B, S, _ = q.shape
    H, Dk, C = HGRN_HEADS, HGRN_HEAD_DIM, HGRN_CHUNK
    f32 = jnp.float32
    z = f_logit.astype(f32).reshape(B, S, H, Dk)
    lbh = lb.astype(f32).reshape(H, Dk)
    log_f = jnp.logaddexp(jnp.log1p(-lbh) + jax.nn.log_sigmoid(z), jnp.log(lbh))
    k = (1.0 - lbh) * jax.nn.sigmoid(-z)
    qf = q.astype(f32).reshape(B, S, H, Dk)
    vf = i.astype(f32).reshape(B, S, H, Dk)
    n = S // C

    def to_chunks(t):
        return t.reshape(B, n, C, H, Dk).transpose(1, 0, 3, 2, 4)

    causal = jnp.tril(jnp.ones((C, C), dtype=bool))

    def step(state, inp):
        qc, kc, vc, lfc = inp
        b = jnp.cumsum(lfc, axis=2)
        diff = b[:, :, :, None, :] - b[:, :, None, :, :]
        decay = jnp.exp(jnp.where(causal[:, :, None], diff, -jnp.inf))
        att = jnp.einsum('bhtd,bhsd,bhtsd->bhts', qc, kc, decay)
        o = jnp.einsum('bhts,bhsv->bhtv', att, vc) + \
            jnp.einsum('bhtd,bhdv->bhtv', qc * jnp.exp(b), state)
        b_last = b[:, :, -1:, :]
        state = jnp.exp(b_last[:, :, 0, :])[..., None] * state + \
            jnp.einsum('bhsd,bhsv->bhdv', kc * jnp.exp(b_last - b), vc)
        return state, o

    state0 = jnp.zeros((B, H, Dk, Dk), f32)
    _, o = lax.scan(step, state0, (to_chunks(qf), to_chunks(k), to_chunks(vf), to_chunks(log_f)))
    o = o.transpose(1, 0, 3, 2, 4).reshape(B, S, H, Dk)
    o = _rmsnorm(o, onorm_g) * jax.nn.silu(g.astype(f32).reshape(B, S, H, Dk))
    return o.reshape(B, S, HGRN_WIDTH).astype(q.dtype)


def _rope(x, cos, sin):
    half = x.shape[-1] // 2
    x1, x2 = x[..., :half], x[..., half:]
    return jnp.concatenate([x1 * cos - x2 * sin, x2 * cos + x1 * sin], axis=-1)


def _causal_attention(q, k, v):
    B, S, H, Dq = q.shape
    Dv = v.shape[-1]
    nb = S // Q_BLOCK
    scale = 1.0 / math.sqrt(Dq)
    qb = q.reshape(B, nb, Q_BLOCK, H, Dq).transpose(1, 0, 3, 2, 4)
    kt = k.transpose(0, 2, 1, 3)
    vt = v.transpose(0, 2, 1, 3)
    key_idx = jnp.arange(S)

    def one_block(args):
        qblk, blk = args
        s = jnp.einsum('bhqd,bhkd->bhqk', qblk, kt).astype(jnp.float32) * scale
        q_idx = blk * Q_BLOCK + jnp.arange(Q_BLOCK)
        s = jnp.where(key_idx[None, :] <= q_idx[:, None], s, -jnp.inf)
        p = jax.nn.softmax(s, axis=-1)
        return jnp.einsum('bhqk,bhkd->bhqd', p.astype(v.dtype), vt)

    out = lax.map(one_block, (qb, jnp.arange(nb)))
    return out.transpose(1, 0, 3, 2, 4).reshape(B, S, H, Dv)


def _mla(q_lat, kv_lat, k_rope, positions, qlat_g, kvlat_g, w_uq, w_ukv, qn_g, kn_g):
    B, S, _ = q_lat.shape
    H = MLA_HEADS
    q = (_rmsnorm(q_lat, qlat_g) @ w_uq).reshape(B, S, H, MLA_QK_DIM)
    kv = (_rmsnorm(kv_lat, kvlat_g) @ w_ukv).reshape(B, S, H, MLA_NOPE_DIM + MLA_V_DIM)
    k_nope, v = kv[..., :MLA_NOPE_DIM], kv[..., MLA_NOPE_DIM:]
    k = jnp.concatenate([k_nope, jnp.broadcast_to(k_rope[:, :, None, :], (B, S, H, MLA_ROPE_DIM))], axis=-1)
    q = _rmsnorm(q, qn_g)
    k = _rmsnorm(k, kn_g)
    inv_freq = 1.0 / (ROPE_THETA ** (jnp.arange(0, MLA_ROPE_DIM, 2, dtype=jnp.float32) / MLA_ROPE_DIM))
    ang = positions.astype(jnp.float32)[..., None] * inv_freq
    cos = jnp.cos(ang)[:, :, None, :].astype(q.dtype)
    sin = jnp.sin(ang)[:, :, None, :].astype(q.dtype)
    q = jnp.concatenate([q[..., :MLA_NOPE_DIM], _rope(q[..., MLA_NOPE_DIM:], cos, sin)], axis=-1)
    k = jnp.concatenate([k[..., :MLA_NOPE_DIM], _rope(k[..., MLA_NOPE_DIM:], cos, sin)], axis=-1)
    return _causal_attention(q, k, v).reshape(B, S, MLA_WIDTH)


def _pool_mixer(u, w_pool, scale):
    B, S, _ = u.shape
    uf = u.astype(jnp.float32).reshape(B, S, POOL_GROUPS, POOL_GROUP_DIM)
    cs = jnp.cumsum(uf, axis=1)
    t = jnp.arange(1, S + 1, dtype=jnp.float32)
    outs = []
    for gi, w in enumerate(POOL_WINDOWS):
        csg = cs[:, :, gi, :]
        lo = jnp.pad(csg, ((0, 0), (w, 0), (0, 0)))[:, :S, :]
        cnt = jnp.minimum(t, float(w))[None, :, None]
        outs.append((csg - lo) / cnt)
    pooled = jnp.stack(outs, axis=2)
    mixed = jnp.einsum('bsgc,gcd->bsgd', pooled - uf, w_pool.astype(jnp.float32))
    return (mixed.reshape(B, S, POOL_WIDTH) * scale.astype(jnp.float32)).astype(u.dtype)


def _moe(h, w_r, b_r, w1, b1, w2, b2):
    B, S, D = h.shape
    hf = h.reshape(B * S, D)
    logits = (hf @ w_r + b_r).astype(jnp.float32)
    vals, idx = lax.top_k(logits, TOP_K)
    wts = jax.nn.softmax(vals, axis=-1)
    combine = jnp.einsum('nk,nke->ne', wts, jax.nn.one_hot(idx, N_EXPERTS, dtype=jnp.float32)).astype(h.dtype)
    y = jnp.zeros_like(hf)
    for e in range(N_EXPERTS):
        a = hf @ w1[e] + b1[e]
        glu = jnp.minimum(a[:, :EXPERT_FF], SWIGLU_LIMIT)
        lin = jnp.clip(a[:, EXPERT_FF:], -SWIGLU_LIMIT, SWIGLU_LIMIT)
        act = glu * jax.nn.sigmoid(SWIGLU_ALPHA * glu) * (lin + 1.0)
        y = y + combine[:, e:e + 1] * (act @ w2[e] + b2[e])
    return y.reshape(B, S, D)


def setup_inputs(seed: int = 0) -> dict:
    key = jax.random.key(seed)
    ks = jax.random.split(key, 32)
    f32 = jnp.float32
    L, D, E, F = DEPTH, D_MODEL, N_EXPERTS, EXPERT_FF

    def nrm(k, shape, fan_in):
        return jax.random.normal(k, shape, f32) * fan_in ** -0.5

    def gain(k, shape):
        return 1.0 + 0.05 * jax.random.normal(k, shape, f32)

    def small(k, shape, s):
        return s * jax.random.normal(k, shape, f32)

    return {
        'x': jax.random.normal(ks[0], (BATCH, SEQ, D), f32),
        'c': jax.random.normal(ks[1], (BATCH, D), f32),
        'positions': jnp.arange(SEQ, dtype=jnp.int32)[None, :] + jax.random.randint(ks[2], (BATCH, 1), 0, 4096, dtype=jnp.int32),
        'ada_w': 0.5 * nrm(ks[3], (L, D, 6 * D), D),
        'ada_b': small(ks[4], (L, 6 * D), 0.02),
        'norm1_g': gain(ks[5], (L, D)),
        'norm2_g': gain(ks[6], (L, D)),
        'w_in': nrm(ks[7], (L, D, IN_TOTAL), D),
        'hgrn_lb': jax.random.normal(ks[8], (L, HGRN_WIDTH), f32),
        'hgrn_onorm_g': gain(ks[9], (L, HGRN_HEAD_DIM)),
        'mla_qlat_g': gain(ks[10], (L, MLA_Q_RANK)),
        'mla_kvlat_g': gain(ks[11], (L, MLA_KV_RANK)),
        'w_uq': nrm(ks[12], (L, MLA_Q_RANK, MLA_HEADS * MLA_QK_DIM), MLA_Q_RANK),
        'w_ukv': nrm(ks[13], (L, MLA_KV_RANK, MLA_HEADS * (MLA_NOPE_DIM + MLA_V_DIM)), MLA_KV_RANK),
        'q_norm_g': gain(ks[14], (L, MLA_QK_DIM)),
        'k_norm_g': gain(ks[15], (L, MLA_QK_DIM)),
        'w_pool': nrm(ks[16], (L, POOL_GROUPS, POOL_GROUP_DIM, POOL_GROUP_DIM), POOL_GROUP_DIM),
        'pool_scale': gain(ks[17], (L, POOL_WIDTH)),
        'w_br_a': nrm(ks[18], (L, HGRN_WIDTH, D), HGRN_WIDTH),
        'w_br_b': nrm(ks[19], (L, MLA_WIDTH, D), MLA_WIDTH),
        'w_br_c': nrm(ks[20], (L, POOL_WIDTH, D), POOL_WIDTH),
        'w_out': nrm(ks[21], (L, D, D), D),
        'w_router': nrm(ks[22], (L, D, E), D),
        'b_router': small(ks[23], (L, E), 0.01),
        'w_exp1': nrm(ks[24], (L, E, D, 2 * F), D),
        'b_exp1': small(ks[25], (L, E, 2 * F), 0.02),
        'w_exp2': nrm(ks[26], (L, E, F, D), F),
        'b_exp2': small(ks[27], (L, E, D), 0.02),
    }


def reference(x, c, positions, ada_w, ada_b, norm1_g, norm2_g, w_in, hgrn_lb, hgrn_onorm_g,
              mla_qlat_g, mla_kvlat_g, w_uq, w_ukv, q_norm_g, k_norm_g, w_pool, pool_scale,
              w_br_a, w_br_b, w_br_c, w_out, w_router, b_router, w_exp1, b_exp1, w_exp2, b_exp2):
    lb_all = jnp.cumsum(jax.nn.softmax(hgrn_lb.astype(jnp.float32), axis=0), axis=0)
    lb_all = lb_all - lb_all[0:1]
    cond = jax.nn.silu(c)
    for l in range(DEPTH):
        mod = cond @ ada_w[l] + ada_b[l]
        sh1, sc1, g1, sh2, sc2, g2 = jnp.split(mod, 6, axis=-1)
        h = _modulate(_rmsnorm(x, norm1_g[l]), sh1, sc1)
        (hq, hf, hi, hg, q_lat, kv_lat, k_rope, pool_in,
         gate_a, gate_b, gate_c) = _split_cols(h @ w_in[l])
        y_a = _hgrn2(hq, hf, hi, hg, lb_all[l], hgrn_onorm_g[l]) @ w_br_a[l]
        y_b = _mla(q_lat, kv_lat, k_rope, positions, mla_qlat_g[l], mla_kvlat_g[l],
                   w_uq[l], w_ukv[l], q_norm_g[l], k_norm_g[l]) @ w_br_b[l]
        y_c = _pool_mixer(pool_in, w_pool[l], pool_scale[l]) @ w_br_c[l]
        merged = jax.nn.sigmoid(gate_a) * y_a + jax.nn.sigmoid(gate_b) * y_b + jax.nn.sigmoid(gate_c) * y_c
        x = x + g1[:, None, :] * (merged @ w_out[l])
        h2 = _modulate(_rmsnorm(x, norm2_g[l]), sh2, sc2)
        x = x + g2[:, None, :] * _moe(h2, w_router[l], b_router[l], w_exp1[l], b_exp1[l], w_exp2[l], b_exp2[l])
    return x
```

```python
import math
from contextlib import ExitStack

import numpy as np
import concourse.bass as bass
import concourse.mybir as mybir
from concourse.bass_utils import run_bass_kernel_spmd

F32 = mybir.dt.float32
BF16 = mybir.dt.bfloat16
I32 = mybir.dt.int32
AF = mybir.ActivationFunctionType
ALU = mybir.AluOpType

L = 2
D = 1024
T = 2048
NE = 32
EPS = 1e-6
NTB = 4
C = 64
NCH = T // C

V_N1G = 0
V_N2G = 8
V_ADAB = 16
V_LB = 64
V_ONG = 68
V_QLG = 69
V_KVG = 72
V_QNG = 74
V_QNGP = 75
V_KNG = 76
V_KNGP = 77
V_PSC = 78
V_BR = 82
V_B1 = 114
NVL = 626
C_ID = 0
C_TRI = 128
C_RMASK = 256
C_INVC = 256 + 2048
C_INVF = C_INVC + 16
C_SINSC = C_INVF + 1
C_PCOL = C_SINSC + 1
C_IOTA = C_PCOL + 1
NCST = C_IOTA + 48
TS = 512
NT = 48
NROW = NT * TS

O_HQ, O_HF, O_HI, O_HG, O_QL, O_KVL, O_KR, O_PL, O_GA, O_GB, O_GC = 0, 512, 1024, 1536, 2048, 2432, 2688, 2720, 3232, 4256, 5280


class Buf:
    __slots__ = ("w", "rs")

    def __init__(self):
        self.w = None
        self.rs = {}


class _Eng:
    def __init__(self, name, obj):
        self.name = name
        self.obj = obj
        self.sem = None
        self.cnt = 0
        self.pending = False
        self.known = {}


class _DmaSem:
    def __init__(self, h):
        self.h = h
        self.val = 0


class Sched:
    EPOCH = 12000

    def __init__(self, nc, es):
        self.nc = nc
        self.es = es
        self.E = {
            "pe": _Eng("pe", nc.tensor),
            "act": _Eng("act", nc.scalar),
            "dve": _Eng("dve", nc.vector),
            "pool": _Eng("pool", nc.gpsimd),
            "sp": _Eng("sp", nc.sync),
        }
        self.nsem = 0
        for e in self.E.values():
            e.sem = self._new_sem(e.name)
            e.mysems = [e.sem]
        self.dsems = []
        self.allsems = {}

    def _new_sem(self, name):
        self.nsem += 1
        return self.es.enter_context(self.nc.semaphore(f"{name}_{self.nsem}"))

    def dma_sem(self, name):
        d = _DmaSem(self._new_sem(name))
        self.dsems.append(d)
        return d

    def _wait(self, eng, ev):
        sem, val = ev
        k = id(sem)
        if eng.known.get(k, 0) >= val:
            return
        if eng.name == "pe" and any(sem is s for s in eng.mysems):
            return
        eng.obj.wait_ge(sem, val)
        eng.known[k] = val

    def _deps(self, eng, reads, writes):
        for b in reads:
            if b.w is not None:
                self._wait(eng, b.w)
        for b in writes:
            if b.w is not None:
                self._wait(eng, b.w)
            for ev in b.rs.values():
                self._wait(eng, ev)

    def _record(self, ev, reads, writes):
        k = id(ev[0])
        for b in reads:
            old = b.rs.get(k)
            if old is None or old[1] < ev[1]:
                b.rs[k] = ev
        for b in writes:
            b.w = ev
            b.rs = {}

    def op(self, engname, fn, reads=(), writes=(), inc=True):
        eng = self.E[engname]
        self._deps(eng, reads, writes)
        if eng.cnt >= self.EPOCH and not eng.pending:
            eng.sem = self._new_sem(eng.name)
            eng.mysems.append(eng.sem)
            eng.cnt = 0
        ins = fn(eng.obj)
        if inc:
            ins.then_inc(eng.sem, 1)
            eng.cnt += 1
            eng.pending = False
            ev = (eng.sem, eng.cnt)
        else:
            eng.pending = True
            ev = (eng.sem, eng.cnt + 1)
        self._record(ev, reads, writes)
        return ins

    def dma(self, qname, fn, dsem, reads=(), writes=(), no_waw=False):
        q = self.E[qname]
        if no_waw:
            for b in reads:
                if b.w is not None:
                    self._wait(q, b.w)
            for b in writes:
                if b.w is not None and b.w[0] is not dsem.h:
                    self._wait(q, b.w)
                for ev in b.rs.values():
                    self._wait(q, ev)
        else:
            self._deps(q, reads, writes)
        ins = fn(q.obj)
        ins.then_inc(dsem.h, 16)
        dsem.val += 16
        ev = (dsem.h, dsem.val)
        self._record(ev, reads, writes)
        return ins

    def barrier(self):
        evs = []
        for e in self.E.values():
            assert not e.pending
            if e.cnt > 0:
                evs.append((e.sem, e.cnt))
        for d in self.dsems:
            if d.val > 0:
                evs.append((d.h, d.val))
        for e in self.E.values():
            for ev in evs:
                if ev[0] is e.sem and e.name == "pe":
                    continue
                self._wait(e, ev)


class Tl:
    def __init__(self, t):
        self.t = t
        self.bufs = {}

    def b(self, key=0):
        r = self.bufs.get(key)
        if r is None:
            r = self.bufs[key] = Buf()
        return r

    def bs(self, keys):
        return [self.b(k) for k in keys]


def build_program(dbg=None):
    dbg = dbg or {}
    nc = bass.Bass("TRN2", target_bir_lowering=False)

    def din(name, shape, dt=F32):
        return nc.dram_tensor(name, list(shape), dt, kind="ExternalInput").ap()

    xT_d = din("xT", [8, 128, T])
    cT_d = din("cT", [128, 8])
    pos_d = din("pos", [1, T], I32)
    cst_d = din("cst", [128, NCST])
    vec_d = din("vec", [128, L * NVL])
    b2_d = din("b2", [L, NE, D])
    adaw_d = din("adaw", [L, 6, 128, 8, 1024])
    whg_d = din("whg", [L, 4, 128, 8, 512])
    wmla_d = din("wmla", [L, 128, 8, 832])
    wpl_d = din("wpl", [L, 128, 8, 512])
    wgate_d = din("wgate", [L, 3, 128, 8, 1024])
    wuq_d = din("wuq", [L, 128, 3, 1536])
    wukv_d = din("wukv", [L, 128, 2, 1024])
    wpool_d = din("wpool", [L, 128, 4, 128])
    wbr_d = din("wbr", [L, 3, 128, 4, 1024])
    wout_d = din("wout", [L, 128, 8, 1024])
    wr_d = din("wr", [L, 128, 8, 32])
    w1_d = din("w1", [L, NE, 8, 128, 8, 256])
    w2f_d = din("w2f", [L, NE, 128, 8, 1024])
    b1r_d = din("b1r", [L * NE * 128, 16])
    xp_d = nc.dram_tensor("xp_scratch", [NROW, D], BF16, kind="Internal").ap()
    yp_d = nc.dram_tensor("yp_scratch", [NROW, D], F32, kind="Internal").ap()
    yT_d = nc.dram_tensor("yT", [8, 128, T], F32, kind="ExternalOutput").ap()
    dbg_out = {}
    for name, shape in dbg.items():
        dbg_out[name] = nc.dram_tensor("dbg_" + name, list(shape), F32, kind="ExternalOutput").ap()

    with ExitStack() as es:
        S = Sched(nc, es)

        uid = [0]

        def sb(stack, name, shape, dt):
            uid[0] += 1
            return Tl(stack.enter_context(nc.sbuf_tensor(f"s{uid[0]}_{name}", list(shape), dt)))

        PS = [Tl(es.enter_context(nc.psum_tensor(f"ps{i}", [128, 512], F32))) for i in range(7)]
        PSBF = Tl(es.enter_context(nc.psum_tensor("psbf", [128, 1024], BF16)))
        rot = [0]

        def psr():
            r = PS[rot[0] % 5]
            rot[0] += 1
            return r

        ACC0, ACC1 = PS[5], PS[6]

        xT = sb(es, "xT", [128, 8, T], F32)
        hT = sb(es, "hT", [128, 8, T], BF16)
        cosT = sb(es, "cosT", [128, T], BF16)
        sinS = sb(es, "sinS", [128, T], BF16)
        identb = sb(es, "identb", [128, 128], BF16)
        identf = sb(es, "identf", [128, 128], F32)
        onesb = sb(es, "onesb", [128, 128], BF16)
        onesf = sb(es, "onesf", [128, 128], F32)
        trib = sb(es, "trib", [128, 128], BF16)
        cstf = sb(es, "cstf", [128, 67], F32)
        vec = sb(es, "vec", [128, L * NVL], F32)
        modT = sb(es, "modT", [128, 48], F32)
        smallv = sb(es, "smallv", [128, 64], F32)
        condT = sb(es, "condT", [128, 8], BF16)

        d_ld = S.dma_sem("d_ld")
        d_w = [S.dma_sem(f"d_w{i}") for i in range(4)]
        d_st = S.dma_sem("d_st")
        d_dbg = S.dma_sem("d_dbg")

        def dump(name, tl_bufs, ap):
            if name in dbg_out:
                S.dma("pool", lambda q: q.dma_start(out=dbg_out[name], in_=ap), d_dbg, reads=tl_bufs)

        XB = lambda k, tb: xT.b((k, tb))
        HB = lambda k, tb: hT.b((k, tb))

        for k in range(8):
            S.dma("sp", lambda q: q.dma_start(out=xT.t[:, k, :], in_=xT_d[k]), d_ld,
                  writes=[XB(k, tb) for tb in range(NTB)])
        S.dma("sp", lambda q: q.dma_start(out=vec.t[:], in_=vec_d), d_ld, writes=[vec.b()])
        S.dma("sp", lambda q: q.dma_start(out=identf.t[:], in_=cst_d[:, C_ID:C_ID + 128]), d_ld, writes=[identf.b()])
        S.dma("sp", lambda q: q.dma_start(out=cstf.t[:], in_=cst_d[:, C_INVC:C_INVC + 67]), d_ld, writes=[cstf.b()])
        S.dma("pool", lambda q: q.dma_start(out=identb.t[:], in_=cst_d[:, C_ID:C_ID + 128]), d_ld, writes=[identb.b()])
        S.dma("pool", lambda q: q.dma_start(out=trib.t[:], in_=cst_d[:, C_TRI:C_TRI + 128]), d_ld, writes=[trib.b()])
        S.op("dve", lambda e: e.memset(onesb.t[:], 1.0), writes=[onesb.b()])
        S.op("dve", lambda e: e.memset(onesf.t[:], 1.0), writes=[onesf.b()])

        for b_ in [XB(k, tb) for k in range(8) for tb in range(NTB)] + [vec.b(), identf.b(), cstf.b(), identb.b(), trib.b()]:
            b_.w = (d_ld.h, d_ld.val)
        with ExitStack() as ph:
            cf = sb(ph, "cf", [128, 8], F32)
            S.dma("sp", lambda q: q.dma_start(out=cf.t[:], in_=cT_d), d_w[2], writes=[cf.b()])
            S.op("act", lambda e: e.activation(out=condT.t[:], in_=cf.t[:], func=AF.Silu), reads=[cf.b()], writes=[condT.b()])
            posi = sb(ph, "posi", [128, T], I32)
            u = sb(ph, "u", [128, T], F32)
            ki = sb(ph, "ki", [128, T], I32)
            kf = sb(ph, "kf", [128, T], F32)
            gt = sb(ph, "gt", [128, T], F32)
            R = slice(64, 96)
            S.dma("sp", lambda q: q.dma_start(out=posi.t[R, :], in_=pos_d[0:1, :].partition_broadcast(32)), d_w[3], writes=[posi.b()])
            S.op("dve", lambda e: e.tensor_copy(out=u.t[R, :], in_=posi.t[R, :]), reads=[posi.b()], writes=[u.b()])
            S.op("dve", lambda e: e.tensor_scalar(out=u.t[R, :], in0=u.t[R, :], scalar1=cstf.t[R, 16:17], scalar2=None, op0=ALU.mult),
                 reads=[u.b(), cstf.b()], writes=[u.b()])
            for shift, dst in ((0.0, sinS), (0.25, cosT)):
                if shift != 0.0:
                    S.op("dve", lambda e: e.tensor_scalar(out=u.t[R, :], in0=u.t[R, :], scalar1=shift, scalar2=None, op0=ALU.add),
                         reads=[u.b()], writes=[u.b()])
                S.op("dve", lambda e: e.tensor_copy(out=ki.t[R, :], in_=u.t[R, :]), reads=[u.b()], writes=[ki.b()])
                S.op("dve", lambda e: e.tensor_copy(out=kf.t[R, :], in_=ki.t[R, :]), reads=[ki.b()], writes=[kf.b()])
                S.op("dve", lambda e: e.tensor_tensor(out=kf.t[R, :], in0=u.t[R, :], in1=kf.t[R, :], op=ALU.subtract),
                     reads=[u.b(), kf.b()], writes=[kf.b()])
                S.op("dve", lambda e: e.tensor_single_scalar(out=gt.t[R, :], in_=kf.t[R, :], scalar=0.5, op=ALU.is_gt),
                     reads=[kf.b()], writes=[gt.b()])
                S.op("dve", lambda e: e.tensor_tensor(out=kf.t[R, :], in0=kf.t[R, :], in1=gt.t[R, :], op=ALU.subtract),
                     reads=[kf.b(), gt.b()], writes=[kf.b()])
                if dst is sinS:
                    S.op("act", lambda e: e.activation(out=dst.t[R, :], in_=kf.t[R, :], func=AF.Sin, scale=cstf.t[R, 17:18]),
                         reads=[kf.b(), cstf.b()], writes=[dst.b()])
                else:
                    S.op("act", lambda e: e.activation(out=dst.t[R, :], in_=kf.t[R, :], func=AF.Sin, scale=2 * math.pi * (1 - 1e-6)),
                         reads=[kf.b()], writes=[dst.b()])
            S.barrier()
        dump("cos", [cosT.b()], cosT.t[:, :])
        dump("sin", [sinS.b()], sinS.t[:, :])

        def rmsnorm_mod(l, which, want_f32=None):
            vb = l * NVL
            gcol = vb + (V_N1G if which == 0 else V_N2G)
            shc = 0 if which == 0 else 24
            scc = shc + 8
            with ExitStack() as ph:
                gs = sb(ph, "gs", [128, 8], F32)
                sq = [sb(ph, f"sq{i}", [128, 512], BF16) for i in range(2)]
                rst = sb(ph, "rst", [128, T], F32)
                tmp = [sb(ph, f"ntmp{i}", [128, 512], F32) for i in range(2)]
                S.op("dve", lambda e: e.scalar_tensor_tensor(out=gs.t[:], in0=modT.t[:, scc:scc + 8], scalar=1.0, in1=vec.t[:, gcol:gcol + 8],
                                                              op0=ALU.add, op1=ALU.mult), reads=[modT.b(), vec.b()], writes=[gs.b()])
                n = 0
                for tb in range(NTB):
                    ts = slice(tb * 512, (tb + 1) * 512)
                    ps = psr()
                    for k in range(8):
                        q = sq[n % 2]
                        n += 1
                        S.op("act", lambda e: e.activation(out=q.t[:], in_=xT.t[:, k, ts], func=AF.Square), reads=[XB(k, tb)], writes=[q.b()])
                        S.op("pe", lambda e: e.matmul(ps.t[:], onesb.t[:], q.t[:], start=(k == 0), stop=(k == 7)),
                             reads=[onesb.b(), q.b()], writes=[ps.b()], inc=True)
                    S.op("act", lambda e: e.activation(out=rst.t[:, ts], in_=ps.t[:], func=AF.Ln, scale=1.0 / D, bias=EPS), reads=[ps.b()], writes=[rst.b(tb)])
                    S.op("act", lambda e: e.activation(out=rst.t[:, ts], in_=rst.t[:, ts], func=AF.Exp, scale=-0.5), reads=[rst.b(tb)], writes=[rst.b(tb)])
                    for k in range(8):
                        tm = tmp[k % 2]
                        S.op("dve", lambda e: e.scalar_tensor_tensor(out=tm.t[:], in0=xT.t[:, k, ts], scalar=gs.t[:, k:k + 1], in1=rst.t[:, ts],
                                                                      op0=ALU.mult, op1=ALU.mult),
                             reads=[XB(k, tb), gs.b(), rst.b(tb)], writes=[tm.b()])
                        S.op("act", lambda e: e.activation(out=hT.t[:, k, ts], in_=tm.t[:], func=AF.Identity, bias=modT.t[:, shc + k:shc + k + 1]),
                             reads=[tm.b(), modT.b()], writes=[HB(k, tb)])
                        if want_f32 is not None:
                            want_f32(k, tb, tm, modT.t[:, shc + k:shc + k + 1])
                    if want_f32 is not None:
                        want_f32(None, tb, None, None)
                S.barrier()

        def adaln(l):
            vb = l * NVL
            with ExitStack() as ph:
                wb = [sb(ph, f"adaw{i}", [128, 8, 1024], BF16) for i in range(2)]
                ps = psr()
                for ty in range(6):
                    w = wb[ty % 2]
                    S.dma("pool", lambda q: q.dma_start(out=w.t[:], in_=adaw_d[l, ty], max_dma_last_dim=8192), d_w[ty % 2], writes=[w.b()])
                    for o in range(8):
                        col = ty * 8 + o
                        for k in range(8):
                            S.op("pe", lambda e: e.matmul(ps.t[:, col:col + 1], w.t[:, k, o * 128:(o + 1) * 128], condT.t[:, k:k + 1],
                                                         start=(k == 0), stop=(k == 7)),
                                 reads=[w.b(), condT.b()], writes=[ps.b()], inc=(k == 7))
                S.op("dve", lambda e: e.tensor_tensor(out=modT.t[:], in0=ps.t[:, 0:48], in1=vec.t[:, vb + V_ADAB:vb + V_ADAB + 48], op=ALU.add),
                     reads=[ps.b(), vec.b()], writes=[modT.b()])
                S.barrier()

        def epilogue(l, br, srcT, src_bufs, nk, kp):
            with ExitStack() as ph:
                wbr = sb(ph, "wbr", [128, 4, 1024], BF16)
                wg = sb(ph, "wg", [128, 8, 1024], BF16)
                wo = sb(ph, "wo", [128, 8, 1024], BF16)
                mT = sb(ph, "mT", [128, 8, 512], BF16)
                sg = [sb(ph, f"sg{i}", [128, 512], F32) for i in range(2)]
                S.dma("pool", lambda q: q.dma_start(out=wbr.t[:], in_=wbr_d[l, br], max_dma_last_dim=8192), d_w[0], writes=[wbr.b()])
                S.dma("pool", lambda q: q.dma_start(out=wg.t[:], in_=wgate_d[l, br], max_dma_last_dim=8192), d_w[1], writes=[wg.b()])
                S.dma("pool", lambda q: q.dma_start(out=wo.t[:], in_=wout_d[l], max_dma_last_dim=8192), d_w[2], writes=[wo.b()])
                n = 0
                for tb in range(NTB):
                    ts = slice(tb * 512, (tb + 1) * 512)
                    for o in range(8):
                        oc = slice(o * 128, (o + 1) * 128)
                        py = psr()
                        for kc in range(nk):
                            S.op("pe", lambda e: e.matmul(py.t[:], wbr.t[0:kp, kc, oc], srcT(kc, ts), start=(kc == 0), stop=(kc == nk - 1)),
                                 reads=[wbr.b()] + src_bufs(kc, tb), writes=[py.b()], inc=(kc == nk - 1))
                        pg = psr()
                        for k in range(8):
                            S.op("pe", lambda e: e.matmul(pg.t[:], wg.t[:, k, oc], hT.t[:, k, ts], start=(k == 0), stop=(k == 7)),
                                 reads=[wg.b(), HB(k, tb)], writes=[pg.b()], inc=(k == 7))
                        s = sg[n % 2]
                        n += 1
                        S.op("act", lambda e: e.activation(out=s.t[:], in_=pg.t[:], func=AF.Sigmoid), reads=[pg.b()], writes=[s.b()])
                        S.op("dve", lambda e: e.tensor_tensor(out=mT.t[:, o, :], in0=py.t[:], in1=s.t[:], op=ALU.mult),
                             reads=[py.b(), s.b()], writes=[mT.b(o)])
                    for o2 in range(8):
                        px = psr()
                        for o in range(8):
                            S.op("pe", lambda e: e.matmul(px.t[:], wo.t[:, o, o2 * 128:(o2 + 1) * 128], mT.t[:, o, :], start=(o == 0), stop=(o == 7)),
                                 reads=[wo.b(), mT.b(o)], writes=[px.b()], inc=(o == 7))
                        S.op("dve", lambda e: e.scalar_tensor_tensor(out=xT.t[:, o2, ts], in0=px.t[:], scalar=modT.t[:, 16 + o2:17 + o2], in1=xT.t[:, o2, ts],
                                                                      op0=ALU.mult, op1=ALU.add),
                             reads=[px.b(), modT.b(), XB(o2, tb)], writes=[XB(o2, tb)])
                S.barrier()

        def hgrn(l):
            vb = l * NVL
            with ExitStack() as outer:
                AT = sb(outer, "AT", [128, 4, T], BF16)
                with ExitStack() as ph:
                    W = sb(ph, "whg", [128, 8, 512], BF16)
                    rmask = sb(ph, "rmask", [128, T], BF16)
                    S.dma("pool", lambda q: q.dma_start(out=rmask.t[:], in_=cst_d[:, C_RMASK:C_RMASK + T]), d_w[1], writes=[rmask.b()])
                    T1 = sb(ph, "T1", [128, T], F32)
                    T2 = sb(ph, "T2", [128, T], F32)
                    T3 = sb(ph, "T3", [128, T], F32)
                    T4 = sb(ph, "T4", [128, T], F32)
                    QpT = sb(ph, "QpT", [128, T], BF16)
                    KpT = sb(ph, "KpT", [128, T], BF16)
                    Kptok = sb(ph, "Kptok", [64, NCH, 128], BF16)
                    Vh = sb(ph, "Vh", [64, NCH, 128], BF16)
                    SGt = sb(ph, "SGt", [128, T], BF16)
                    S32 = sb(ph, "S32", [128, 128], F32)
                    Stmp = sb(ph, "Stmp", [128, 128], F32)
                    Sbf = [sb(ph, f"Sbf{i}", [128, 128], BF16) for i in range(2)]
                    ATT = sb(ph, "ATT", [64, NCH, 64], BF16)
                    S32b = [S32, Stmp]
                    Pb = [sb(ph, f"Pb{i}", [128, 128], F32) for i in range(2)]
                    beta = sb(ph, "beta", [128, 32], F32)
                    mv = sb(ph, "mv", [128, 32], F32)
                    emv = sb(ph, "emv", [128, 32], F32)
                    trii = sb(ph, "trii", [64, 64], I32)
                    S.op("dve", lambda e: e.tensor_copy(out=trii.t[:], in_=trib.t[0:64, 0:64]), reads=[trib.b()], writes=[trii.b()])
                    S.op("dve", lambda e: e.memset(ATT.t[:], 0.0), writes=[ATT.b()])
                    lbv = sb(ph, "lbv", [128, 8], F32)
                    if l == 1:
                        S.op("dve", lambda e: e.tensor_tensor(out=lbv.t[:, 0:4], in0=vec.t[:, NVL + V_LB:NVL + V_LB + 4], in1=vec.t[:, V_LB:V_LB + 4],
                                                               op=ALU.subtract), reads=[vec.b()], writes=[lbv.b()])
                        S.op("act", lambda e: e.activation(out=lbv.t[:, 0:4], in_=lbv.t[:, 0:4], func=AF.Sigmoid), reads=[lbv.b()], writes=[lbv.b()])
                        S.op("dve", lambda e: e.tensor_scalar(out=lbv.t[:, 4:8], in0=lbv.t[:, 0:4], scalar1=-1.0, scalar2=1.0, op0=ALU.mult, op1=ALU.add),
                             reads=[lbv.b()], writes=[lbv.b()])
                    for hd in range(4):
                        S.dma("pool", lambda q: q.dma_start(out=W.t[:], in_=whg_d[l, hd], max_dma_last_dim=8192), d_w[0], writes=[W.b()])
                        cq, cf_, ci, cg = slice(0, 128), slice(128, 256), slice(256, 384), slice(384, 512)
                        for tb in range(NTB):
                            ts = slice(tb * 512, (tb + 1) * 512)
                            ps = psr()
                            for k in range(8):
                                S.op("pe", lambda e: e.matmul(ps.t[:], W.t[:, k, cf_], hT.t[:, k, ts], start=(k == 0), stop=(k == 7)),
                                     reads=[W.b(), HB(k, tb)], writes=[ps.b()], inc=(k == 7))
                            S.op("act", lambda e: e.activation(out=T1.t[:, ts], in_=ps.t[:], func=AF.Sigmoid), reads=[ps.b()], writes=[T1.b(tb)])
                        allT = lambda t_: [t_.b(tb) for tb in range(NTB)]
                        if l == 1:
                            S.op("dve", lambda e: e.tensor_scalar(out=T1.t[:], in0=T1.t[:], scalar1=lbv.t[:, 4 + hd:5 + hd], scalar2=lbv.t[:, hd:hd + 1],
                                                                   op0=ALU.mult, op1=ALU.add), reads=allT(T1) + [lbv.b()], writes=allT(T1))
                        S.op("act", lambda e: e.activation(out=T2.t[:], in_=T1.t[:], func=AF.Ln), reads=allT(T1), writes=allT(T2))
                        S.op("dve", lambda e: e.tensor_tensor_scan(out=T3.t[:], data0=rmask.t[:], data1=T2.t[:], initial=0.0, op0=ALU.mult, op1=ALU.add),
                             reads=allT(T2) + [rmask.b()], writes=allT(T3))
                        S.op("dve", lambda e: e.tensor_copy(out=mv.t[:], in_=T3.t[:, 31::64]), reads=allT(T3), writes=[mv.b()])
                        S.op("dve", lambda e: e.tensor_tensor(out=T3.t[:, :].rearrange("p (c j) -> p c j", j=C), in0=T3.t[:, :].rearrange("p (c j) -> p c j", j=C),
                                                               in1=mv.t[:, :].rearrange("p (c o) -> p c o", o=1).broadcast_to([128, NCH, C]), op=ALU.subtract),
                             reads=allT(T3) + [mv.b()], writes=allT(T3))
                        S.op("act", lambda e: e.activation(out=emv.t[:], in_=mv.t[:], func=AF.Exp), reads=[mv.b()], writes=[emv.b()])
                        S.op("act", lambda e: e.activation(out=T4.t[:], in_=T3.t[:], func=AF.Exp), reads=allT(T3), writes=allT(T4))
                        S.op("act", lambda e: e.activation(out=T2.t[:], in_=T3.t[:], func=AF.Exp, scale=-1.0), reads=allT(T3), writes=allT(T2))
                        S.op("dve", lambda e: e.scalar_tensor_tensor(out=KpT.t[:], in0=T1.t[:], scalar=1.0, in1=T2.t[:], op0=ALU.subtract, op1=ALU.mult),
                             reads=allT(T1) + allT(T2), writes=allT(KpT))
                        for tb in range(NTB):
                            ts = slice(tb * 512, (tb + 1) * 512)
                            ps = psr()
                            for k in range(8):
                                S.op("pe", lambda e: e.matmul(ps.t[:], W.t[:, k, cq], hT.t[:, k, ts], start=(k == 0), stop=(k == 7)),
                                     reads=[W.b(), HB(k, tb)], writes=[ps.b()], inc=(k == 7))
                            S.op("dve", lambda e: e.scalar_tensor_tensor(out=QpT.t[:, ts], in0=ps.t[:], scalar=-1.0, in1=T4.t[:, ts], op0=ALU.mult, op1=ALU.mult),
                                 reads=[ps.b(), T4.b(tb)], writes=[QpT.b(tb)])
                        for tb in range(NTB):
                            ts = slice(tb * 512, (tb + 1) * 512)
                            ps = psr()
                            for k in range(8):
                                S.op("pe", lambda e: e.matmul(ps.t[:], W.t[:, k, cg], hT.t[:, k, ts], start=(k == 0), stop=(k == 7)),
                                     reads=[W.b(), HB(k, tb)], writes=[ps.b()], inc=(k == 7))
                            S.op("act", lambda e: e.activation(out=SGt.t[:, ts], in_=ps.t[:], func=AF.Silu), reads=[ps.b()], writes=[SGt.b(tb)])
                        for c4 in range(NCH // 4):
                            for j in range(4):
                                c = c4 * 4 + j
                                S.op("pe", lambda e: e.transpose(PSBF.t[0:64, j * 128:(j + 1) * 128], KpT.t[:, c * C:(c + 1) * C], identb.t[:]),
                                     reads=[KpT.b(c * C // 512), identb.b()], writes=[PSBF.b()], inc=(j == 3))
                            S.op("act", lambda e: e.copy(out=Kptok.t[:, c4 * 4:c4 * 4 + 4, :], in_=PSBF.t[0:64, 0:512].rearrange("p (a b) -> p a b", a=4)),
                                 reads=[PSBF.b()], writes=[Kptok.b(c4)])
                        for c4 in range(NCH // 4):
                            ps = psr()
                            for j in range(4):
                                c = c4 * 4 + j
                                for k in range(8):
                                    S.op("pe", lambda e: e.matmul(ps.t[0:64, j * 128:(j + 1) * 128], hT.t[:, k, c * C:(c + 1) * C], W.t[:, k, ci],
                                                                 start=(k == 0), stop=(k == 7)),
                                         reads=[W.b(), HB(k, c * C // 512)], writes=[ps.b()], inc=(k == 7 and j == 3))
                            S.op("dve", lambda e: e.tensor_copy(out=Vh.t[:, c4 * 4:c4 * 4 + 4, :], in_=ps.t[0:64, :].rearrange("p (a b) -> p a b", a=4)),
                                 reads=[ps.b()], writes=[Vh.b(c4)])
                        OT = T1
                        for c in range(NCH):
                            cs = slice(c * C, (c + 1) * C)
                            tb = c * C // 512
                            pa = psr()
                            S.op("pe", lambda e: e.matmul(pa.t[0:64, 0:64], KpT.t[:, cs], QpT.t[:, cs], start=True, stop=True),
                                 reads=[KpT.b(tb), QpT.b(tb)], writes=[pa.b()])
                            S.op("dve", lambda e: e.copy_predicated(out=ATT.t[:, c, :], mask=trii.t[:], data=pa.t[0:64, 0:64]),
                                 reads=[pa.b(), trii.b()], writes=[ATT.b(c)])
                        S.op("dve", lambda e: e.tensor_tensor(out=beta.t[:, 0:NCH - 1], in0=T4.t[:, C - 1:T - C:C], in1=emv.t[:, 1:NCH], op=ALU.mult),
                             reads=allT(T4) + [emv.b()], writes=[beta.b()])
                        for c in range(NCH):
                            cs = slice(c * C, (c + 1) * C)
                            tb = c * C // 512
                            if c < NCH - 1:
                                pss = psr()
                                S.op("pe", lambda e: e.matmul(pss.t[:, 0:128], Kptok.t[:, c, :], Vh.t[:, c, :], start=True, stop=True),
                                     reads=[Kptok.b(c // 4), Vh.b(c // 4)], writes=[pss.b()])
                            po = psr()
                            S.op("pe", lambda e: e.matmul(po.t[:, 0:64], Vh.t[:, c, :], ATT.t[:, c, :], start=True, stop=(c == 0)),
                                 reads=[Vh.b(c // 4), ATT.b(c)], writes=[po.b()], inc=(c == 0))
                            if c > 0:
                                sbf = Sbf[c % 2]
                                S.op("pe", lambda e: e.matmul(po.t[:, 0:64], sbf.t[:], QpT.t[:, cs], start=False, stop=True),
                                     reads=[sbf.b(), QpT.b(tb)], writes=[po.b()])
                            S.op("act", lambda e: e.copy(out=OT.t[:, cs], in_=po.t[:, 0:64]), reads=[po.b()], writes=[OT.b(tb)])
                            if c < NCH - 1:
                                bc = beta.t[:, c:c + 1]
                                nsbf = Sbf[(c + 1) % 2]
                                s_old, s_new = S32b[c % 2], S32b[(c + 1) % 2]
                                if c == 0:
                                    S.op("dve", lambda e: e.tensor_scalar(out=nsbf.t[:], in0=pss.t[:, 0:128], scalar1=bc, scalar2=None, op0=ALU.mult),
                                         reads=[pss.b(), beta.b()], writes=[nsbf.b()])
                                    S.op("dve", lambda e: e.tensor_scalar(out=s_new.t[:], in0=pss.t[:, 0:128], scalar1=bc, scalar2=None, op0=ALU.mult),
                                         reads=[pss.b(), beta.b()], writes=[s_new.b()])
                                else:
                                    pb_ = Pb[c % 2]
                                    S.op("dve", lambda e: e.tensor_scalar(out=pb_.t[:], in0=pss.t[:, 0:128], scalar1=bc, scalar2=None, op0=ALU.mult),
                                         reads=[pss.b(), beta.b()], writes=[pb_.b()])
                                    S.op("dve", lambda e: e.scalar_tensor_tensor(out=nsbf.t[:], in0=s_old.t[:], scalar=bc, in1=pb_.t[:], op0=ALU.mult, op1=ALU.add),
                                         reads=[s_old.b(), beta.b(), pb_.b()], writes=[nsbf.b()])
                                    S.op("dve", lambda e: e.scalar_tensor_tensor(out=s_new.t[:], in0=s_old.t[:], scalar=bc, in1=pb_.t[:], op0=ALU.mult, op1=ALU.add),
                                         reads=[s_old.b(), beta.b(), pb_.b()], writes=[s_new.b()])
                        sqb = KpT
                        S.op("act", lambda e: e.activation(out=sqb.t[:], in_=OT.t[:], func=AF.Square), reads=allT(OT), writes=allT(sqb))
                        for tb in range(NTB):
                            ts = slice(tb * 512, (tb + 1) * 512)
                            ps = psr()
                            S.op("pe", lambda e: e.matmul(ps.t[:], onesb.t[:], sqb.t[:, ts], start=True, stop=True), reads=[onesb.b(), sqb.b(tb)], writes=[ps.b()])
                            S.op("act", lambda e: e.activation(out=T3.t[:, ts], in_=ps.t[:], func=AF.Ln, scale=1.0 / 128, bias=EPS), reads=[ps.b()], writes=[T3.b(tb)])
                            S.op("act", lambda e: e.activation(out=T3.t[:, ts], in_=T3.t[:, ts], func=AF.Exp, scale=-0.5), reads=[T3.b(tb)], writes=[T3.b(tb)])
                            S.op("dve", lambda e: e.scalar_tensor_tensor(out=T3.t[:, ts], in0=OT.t[:, ts], scalar=vec.t[:, vb + V_ONG:vb + V_ONG + 1], in1=T3.t[:, ts],
                                                                          op0=ALU.mult, op1=ALU.mult), reads=[OT.b(tb), vec.b(), T3.b(tb)], writes=[T3.b(tb)])
                            S.op("dve", lambda e: e.tensor_tensor(out=AT.t[:, hd, ts], in0=T3.t[:, ts], in1=SGt.t[:, ts], op=ALU.mult),
                                 reads=[T3.b(tb), SGt.b(tb)], writes=[AT.b((hd, tb))])
                    S.barrier()
                dump_bf16("oa", AT, [(hd, tb) for hd in range(4) for tb in range(NTB)], 4)
                epilogue(l, 0, lambda kc, ts: AT.t[:, kc, ts], lambda kc, tb: [AT.b((kc, tb))], 4, 128)

        def mla(l):
            vb = l * NVL
            R = slice(64, 96)
            scale = 1.0 / math.sqrt(96.0)
            with ExitStack() as outer:
                BT = sb(outer, "BT", [128, 4, T], BF16)
                with ExitStack() as ph:
                    qlatN = sb(ph, "qlatN", [128, 3, T], BF16)
                    kvN = sb(ph, "kvN", [128, 2, T], BF16)
                    KRC = sb(ph, "KRC", [128, T], F32)
                    sqkr = sb(ph, "sqkr", [128, T], BF16)
                    with ExitStack() as p1:
                        wm = sb(p1, "wmla", [128, 8, 832], BF16)
                        sq = [sb(p1, f"lsq{i}", [128, 512], BF16) for i in range(2)]
                        rs = sb(p1, "lrs", [128, 512], F32)
                        t1 = sb(p1, "lt1", [128, 512], F32)
                        t2 = sb(p1, "lt2", [128, 512], F32)
                        S.dma("pool", lambda q: q.dma_start(out=wm.t[:], in_=wmla_d[l], max_dma_last_dim=8192), d_w[0], writes=[wm.b()])
                        for tb in range(NTB):
                            ts = slice(tb * 512, (tb + 1) * 512)
                            for dst, c0, nchunk, gcol, dim in ((qlatN, 0, 3, V_QLG, 384.0), (kvN, 384, 2, V_KVG, 256.0)):
                                pls = [psr() for _ in range(nchunk)]
                                for j in range(nchunk):
                                    for k in range(8):
                                        S.op("pe", lambda e: e.matmul(pls[j].t[:], wm.t[:, k, c0 + j * 128:c0 + (j + 1) * 128], hT.t[:, k, ts], start=(k == 0), stop=(k == 7)),
                                             reads=[wm.b(), HB(k, tb)], writes=[pls[j].b()], inc=(k == 7))
                                pss = psr()
                                for j in range(nchunk):
                                    q_ = sq[j % 2]
                                    S.op("act", lambda e: e.activation(out=q_.t[:], in_=pls[j].t[:], func=AF.Square), reads=[pls[j].b()], writes=[q_.b()])
                                    S.op("pe", lambda e: e.matmul(pss.t[:], onesb.t[:], q_.t[:], start=(j == 0), stop=(j == nchunk - 1)),
                                         reads=[onesb.b(), q_.b()], writes=[pss.b()])
                                S.op("act", lambda e: e.activation(out=rs.t[:], in_=pss.t[:], func=AF.Ln, scale=1.0 / dim, bias=EPS), reads=[pss.b()], writes=[rs.b()])
                                S.op("act", lambda e: e.activation(out=rs.t[:], in_=rs.t[:], func=AF.Exp, scale=-0.5), reads=[rs.b()], writes=[rs.b()])
                                for j in range(nchunk):
                                    S.op("dve", lambda e: e.scalar_tensor_tensor(out=dst.t[:, j, ts], in0=pls[j].t[:], scalar=vec.t[:, vb + gcol + j:vb + gcol + j + 1], in1=rs.t[:],
                                                                                  op0=ALU.mult, op1=ALU.mult), reads=[pls[j].b(), vec.b(), rs.b()], writes=[dst.b((j, tb))])
                            pk = psr()
                            pks = psr()
                            for k in range(8):
                                S.op("pe", lambda e: e.matmul(pk.t[0:96, :], wm.t[:, k, 640:736], hT.t[:, k, ts], start=(k == 0), stop=(k == 7)),
                                     reads=[wm.b(), HB(k, tb)], writes=[pk.b()], inc=(k == 7))
                            for k in range(8):
                                S.op("pe", lambda e: e.matmul(pks.t[0:96, :], wm.t[:, k, 736:832], hT.t[:, k, ts], start=(k == 0), stop=(k == 7)),
                                     reads=[wm.b(), HB(k, tb)], writes=[pks.b()], inc=(k == 7))
                            S.op("act", lambda e: e.activation(out=sqkr.t[0:96, ts], in_=pk.t[0:96, :], func=AF.Square), reads=[pk.b()], writes=[sqkr.b(tb)])
                            S.op("dve", lambda e: e.scalar_tensor_tensor(out=t1.t[R, :], in0=pk.t[R, :], scalar=vec.t[R, vb + V_KNG:vb + V_KNG + 1], in1=cosT.t[R, ts],
                                                                          op0=ALU.mult, op1=ALU.mult), reads=[pk.b(), vec.b(), cosT.b()], writes=[t1.b()])
                            S.op("dve", lambda e: e.scalar_tensor_tensor(out=t2.t[R, :], in0=pks.t[R, :], scalar=vec.t[R, vb + V_KNGP:vb + V_KNGP + 1], in1=sinS.t[R, ts],
                                                                          op0=ALU.mult, op1=ALU.mult), reads=[pks.b(), vec.b(), sinS.b()], writes=[t2.b()])
                            S.op("dve", lambda e: e.tensor_tensor(out=KRC.t[R, ts], in0=t1.t[R, :], in1=t2.t[R, :], op=ALU.add), reads=[t1.b(), t2.b()], writes=[KRC.b(tb)])
                        S.barrier()
                    with ExitStack() as p2:
                        wq = sb(p2, "wuq", [128, 3, 1536], BF16)
                        wkv = sb(p2, "wukv", [128, 2, 1024], BF16)
                        Vp = sb(p2, "Vp", [128, 16, 192], BF16)
                        QT = sb(p2, "QT", [128, T], BF16)
                        KT = sb(p2, "KT", [128, T], BF16)
                        sq = sb(p2, "asq", [128, 512], BF16)
                        rs = sb(p2, "ars", [128, 512], F32)
                        t1 = sb(p2, "at1", [128, 512], F32)
                        t2 = sb(p2, "at2", [128, 512], F32)
                        Eb = [sb(p2, f"Eb{i}", [128, 512], BF16) for i in range(4)]
                        RD = [sb(p2, f"RD{i}", [128, 512], F32) for i in range(2)]
                        deferred = []
                        nrd = [0]
                        bcs = sb(p2, "bcs", [128, 512], F32)
                        S.dma("pool", lambda q: q.dma_start(out=wq.t[:], in_=wuq_d[l], max_dma_last_dim=8192), d_w[0], writes=[wq.b()])
                        S.dma("pool", lambda q: q.dma_start(out=wkv.t[:], in_=wukv_d[l], max_dma_last_dim=8192), d_w[1], writes=[wkv.b()])
                        S.op("dve", lambda e: e.memset(Vp.t[:, :, 64:128], 0.0), writes=[Vp.b()])
                        S.op("dve", lambda e: e.memset(Vp.t[:, :, 64:65], 1.0), writes=[Vp.b()])
                        ne = [0]
                        for jp in range(4):
                            for t4 in range(4):
                                ps = psr()
                                for j in range(4):
                                    tt = t4 * 4 + j
                                    for kc in range(2):
                                        S.op("pe", lambda e: e.matmul(ps.t[:, j * 128:(j + 1) * 128], kvN.t[:, kc, tt * 128:(tt + 1) * 128], wkv.t[:, kc, 512 + jp * 128:512 + (jp + 1) * 128],
                                                                     start=(kc == 0), stop=(kc == 1)),
                                             reads=[kvN.b((kc, t4)), wkv.b()], writes=[ps.b()], inc=(kc == 1 and j == 3))
                                pv = ps.t[:, :].rearrange("p (a b) -> p a b", a=4)
                                S.op("act", lambda e: e.copy(out=Vp.t[:, t4 * 4:(t4 + 1) * 4, 0:64], in_=pv[:, :, 0:64]), reads=[ps.b()], writes=[Vp.b()])
                                S.op("dve", lambda e: e.tensor_copy(out=Vp.t[:, t4 * 4:(t4 + 1) * 4, 128:192], in_=pv[:, :, 64:128]), reads=[ps.b()], writes=[Vp.b()])
                            for hh in range(2):
                                h = jp * 2 + hh
                                for tb in range(NTB):
                                    ts = slice(tb * 512, (tb + 1) * 512)
                                    pq = psr()
                                    pqs = psr()
                                    for kc in range(3):
                                        S.op("pe", lambda e: e.matmul(pq.t[0:96, :], wq.t[:, kc, h * 96:(h + 1) * 96], qlatN.t[:, kc, ts], start=(kc == 0), stop=(kc == 2)),
                                             reads=[wq.b(), qlatN.b((kc, tb))], writes=[pq.b()], inc=(kc == 2))
                                    for kc in range(3):
                                        S.op("pe", lambda e: e.matmul(pqs.t[0:96, :], wq.t[:, kc, 768 + h * 96:768 + (h + 1) * 96], qlatN.t[:, kc, ts], start=(kc == 0), stop=(kc == 2)),
                                             reads=[wq.b(), qlatN.b((kc, tb))], writes=[pqs.b()], inc=(kc == 2))
                                    S.op("act", lambda e: e.activation(out=sq.t[0:96, :], in_=pq.t[0:96, :], func=AF.Square), reads=[pq.b()], writes=[sq.b()])
                                    pss = psr()
                                    S.op("pe", lambda e: e.matmul(pss.t[0:96, :], onesb.t[0:96, 0:96], sq.t[0:96, :], start=True, stop=True), reads=[onesb.b(), sq.b()], writes=[pss.b()])
                                    S.op("act", lambda e: e.activation(out=rs.t[0:96, :], in_=pss.t[0:96, :], func=AF.Ln, scale=1.0 / 96, bias=EPS), reads=[pss.b()], writes=[rs.b()])
                                    S.op("act", lambda e: e.activation(out=rs.t[0:96, :], in_=rs.t[0:96, :], func=AF.Exp, scale=-0.5), reads=[rs.b()], writes=[rs.b()])
                                    S.op("dve", lambda e: e.scalar_tensor_tensor(out=QT.t[0:64, ts], in0=pq.t[0:64, :], scalar=vec.t[0:64, vb + V_QNG:vb + V_QNG + 1], in1=rs.t[0:64, :],
                                                                                  op0=ALU.mult, op1=ALU.mult), reads=[pq.b(), vec.b(), rs.b()], writes=[QT.b(tb)])
                                    S.op("dve", lambda e: e.scalar_tensor_tensor(out=t1.t[R, :], in0=pq.t[R, :], scalar=vec.t[R, vb + V_QNG:vb + V_QNG + 1], in1=cosT.t[R, ts],
                                                                                  op0=ALU.mult, op1=ALU.mult), reads=[pq.b(), vec.b(), cosT.b()], writes=[t1.b()])
                                    S.op("dve", lambda e: e.scalar_tensor_tensor(out=t2.t[R, :], in0=pqs.t[R, :], scalar=vec.t[R, vb + V_QNGP:vb + V_QNGP + 1], in1=sinS.t[R, ts],
                                                                                  op0=ALU.mult, op1=ALU.mult), reads=[pqs.b(), vec.b(), sinS.b()], writes=[t2.b()])
                                    S.op("dve", lambda e: e.tensor_tensor(out=t1.t[R, :], in0=t1.t[R, :], in1=t2.t[R, :], op=ALU.add), reads=[t1.b(), t2.b()], writes=[t1.b()])
                                    S.op("dve", lambda e: e.tensor_tensor(out=QT.t[R, ts], in0=t1.t[R, :], in1=rs.t[R, :], op=ALU.mult), reads=[t1.b(), rs.b()], writes=[QT.b(tb)])
                                    pk = psr()
                                    for kc in range(2):
                                        S.op("pe", lambda e: e.matmul(pk.t[0:64, :], wkv.t[:, kc, h * 64:(h + 1) * 64], kvN.t[:, kc, ts], start=(kc == 0), stop=(kc == 1)),
                                             reads=[wkv.b(), kvN.b((kc, tb))], writes=[pk.b()], inc=(kc == 1))
                                    S.op("act", lambda e: e.activation(out=sq.t[0:64, :], in_=pk.t[0:64, :], func=AF.Square), reads=[pk.b()], writes=[sq.b()])
                                    pss = psr()
                                    S.op("pe", lambda e: e.matmul(pss.t[0:96, :], onesb.t[0:64, 0:96], sq.t[0:64, :], start=True, stop=False), reads=[onesb.b(), sq.b()], writes=[pss.b()], inc=False)
                                    S.op("pe", lambda e: e.matmul(pss.t[0:96, :], onesb.t[0:96, 0:96], sqkr.t[0:96, ts], start=False, stop=True), reads=[onesb.b(), sqkr.b(tb)], writes=[pss.b()])
                                    S.op("act", lambda e: e.activation(out=rs.t[0:96, :], in_=pss.t[0:96, :], func=AF.Ln, scale=1.0 / 96, bias=EPS), reads=[pss.b()], writes=[rs.b()])
                                    S.op("act", lambda e: e.activation(out=rs.t[0:96, :], in_=rs.t[0:96, :], func=AF.Exp, scale=-0.5), reads=[rs.b()], writes=[rs.b()])
                                    S.op("dve", lambda e: e.scalar_tensor_tensor(out=KT.t[0:64, ts], in0=pk.t[0:64, :], scalar=vec.t[0:64, vb + V_KNG:vb + V_KNG + 1], in1=rs.t[0:64, :],
                                                                                  op0=ALU.mult, op1=ALU.mult), reads=[pk.b(), vec.b(), rs.b()], writes=[KT.b(tb)])
                                    S.op("dve", lambda e: e.tensor_tensor(out=KT.t[R, ts], in0=KRC.t[R, ts], in1=rs.t[R, :], op=ALU.mult), reads=[KRC.b(tb), rs.b()], writes=[KT.b(tb)])
                                for qb in range(NTB):
                                    po = ACC0 if (h * 4 + qb) % 2 == 0 else ACC1
                                    nkt = 4 * qb + 4
                                    ebs = {}

                                    def s_exp(kt):
                                        d = kt - 4 * qb
                                        c0 = max(d, 0) * 128
                                        ps = psr()
                                        S.op("pe", lambda e: e.matmul(ps.t[:, c0:512], KT.t[0:96, kt * 128:(kt + 1) * 128], QT.t[0:96, qb * 512 + c0:(qb + 1) * 512], start=True, stop=True),
                                             reads=[KT.b(kt // 4), QT.b(qb)], writes=[ps.b()])
                                        eb = Eb[ne[0] % len(Eb)]
                                        ne[0] += 1
                                        ebs[kt] = eb
                                        S.op("act", lambda e: e.activation(out=eb.t[:, c0:512], in_=ps.t[:, c0:512], func=AF.Exp, scale=scale), reads=[ps.b()], writes=[eb.b()])
                                        if d >= 0:
                                            S.op("dve", lambda e: e.tensor_tensor(out=eb.t[:, c0:c0 + 128], in0=eb.t[:, c0:c0 + 128], in1=trib.t[:], op=ALU.mult),
                                                 reads=[eb.b(), trib.b()], writes=[eb.b()])

                                    def pv(kt):
                                        d = kt - 4 * qb
                                        c0 = max(d, 0) * 128
                                        eb = ebs[kt]
                                        if hh == 0:
                                            S.op("pe", lambda e: e.matmul(po.t[0:65, c0:512], Vp.t[:, kt, 0:65], eb.t[:, c0:512], start=(kt == 0), stop=(kt == nkt - 1)),
                                                 reads=[Vp.b(), eb.b()], writes=[po.b()], inc=(kt == nkt - 1))
                                        else:
                                            S.op("pe", lambda e: e.matmul(po.t[:, c0:512], Vp.t[:, kt, 64:192], eb.t[:, c0:512], start=(kt == 0), stop=(kt == nkt - 1)),
                                                 reads=[Vp.b(), eb.b()], writes=[po.b()], inc=(kt == nkt - 1))

                                    SK = 2
                                    for i in range(nkt + SK):
                                        if i < nkt:
                                            s_exp(i)
                                        if i == SK and deferred:
                                            for f_ in deferred:
                                                f_()
                                            deferred.clear()
                                        if i - SK >= 0:
                                            pv(i - SK)
                                    pden = 64 if hh == 0 else 0
                                    orow = slice(0, 64) if hh == 0 else slice(64, 128)
                                    qs = slice(qb * 512, (qb + 1) * 512)
                                    rd = RD[nrd[0] % 2]
                                    nrd[0] += 1
                                    S.op("dve", lambda e: e.reciprocal(out=rd.t[pden:pden + 1, :], in_=po.t[pden:pden + 1, :]), reads=[po.b()], writes=[rd.b()])

                                    def norm2(po=po, pden=pden, orow=orow, qs=qs, rd=rd, jp=jp, qb=qb):
                                        pbc = psr()
                                        S.op("pe", lambda e: e.matmul(pbc.t[orow, :], onesf.t[pden:pden + 1, 0:64], rd.t[pden:pden + 1, :], start=True, stop=True),
                                             reads=[onesf.b(), rd.b()], writes=[pbc.b()])
                                        S.op("act", lambda e: e.copy(out=bcs.t[orow, :], in_=pbc.t[orow, :]), reads=[pbc.b()], writes=[bcs.b()])
                                        S.op("dve", lambda e: e.tensor_tensor(out=BT.t[orow, jp, qs], in0=po.t[orow, :], in1=bcs.t[orow, :], op=ALU.mult),
                                             reads=[po.b(), bcs.b()], writes=[BT.b((jp, qb))])

                                    deferred.append(norm2)
                        for f_ in deferred:
                            f_()
                        deferred.clear()
                        S.barrier()
                dump_bf16("ob", BT, [(jp, tb) for jp in range(4) for tb in range(NTB)], 4)
                epilogue(l, 1, lambda kc, ts: BT.t[:, kc, ts], lambda kc, tb: [BT.b((kc, tb))], 4, 128)

        def poolmix(l):
            vb = l * NVL
            with ExitStack() as outer:
                CT = sb(outer, "CT", [128, 4, T], BF16)
                with ExitStack() as ph:
                    wp = sb(ph, "wpl", [128, 8, 512], BF16)
                    wpo = sb(ph, "wpool", [128, 4, 128], BF16)
                    U = sb(ph, "U", [128, T], F32)
                    A = sb(ph, "A", [128, T], F32)
                    B = sb(ph, "B", [128, T], F32)
                    DT = sb(ph, "DT", [128, T], BF16)
                    tfix = sb(ph, "tfix", [128, 16], F32)
                    S.dma("pool", lambda q: q.dma_start(out=wp.t[:], in_=wpl_d[l], max_dma_last_dim=8192), d_w[0], writes=[wp.b()])
                    S.dma("pool", lambda q: q.dma_start(out=wpo.t[:], in_=wpool_d[l], max_dma_last_dim=8192), d_w[1], writes=[wpo.b()])
                    for g in range(4):
                        w = 2 << g
                        for tb in range(NTB):
                            ts = slice(tb * 512, (tb + 1) * 512)
                            ps = psr()
                            for k in range(8):
                                S.op("pe", lambda e: e.matmul(ps.t[:], wp.t[:, k, g * 128:(g + 1) * 128], hT.t[:, k, ts], start=(k == 0), stop=(k == 7)),
                                     reads=[wp.b(), HB(k, tb)], writes=[ps.b()], inc=(k == 7))
                            S.op("act", lambda e: e.copy(out=U.t[:, ts], in_=ps.t[:]), reads=[ps.b()], writes=[U.b()])
                        src = U
                        sh = 1
                        while sh < w:
                            dst = A if src is not A else B
                            S.op("dve", lambda e: e.tensor_tensor(out=dst.t[:, sh:T], in0=src.t[:, sh:T], in1=src.t[:, 0:T - sh], op=ALU.add), reads=[src.b()], writes=[dst.b()])
                            S.op("dve", lambda e: e.tensor_copy(out=dst.t[:, 0:sh], in_=src.t[:, 0:sh]), reads=[src.b()], writes=[dst.b()])
                            src = dst
                            sh *= 2
                        S.op("dve", lambda e: e.scalar_tensor_tensor(out=DT.t[:], in0=src.t[:], scalar=1.0 / w, in1=U.t[:], op0=ALU.mult, op1=ALU.subtract),
                             reads=[src.b(), U.b()], writes=[DT.b()])
                        S.op("dve", lambda e: e.tensor_tensor(out=tfix.t[:, 0:w - 1], in0=src.t[:, 0:w - 1], in1=cstf.t[:, 0:w - 1], op=ALU.mult), reads=[src.b(), cstf.b()], writes=[tfix.b()])
                        S.op("dve", lambda e: e.tensor_tensor(out=DT.t[:, 0:w - 1], in0=tfix.t[:, 0:w - 1], in1=U.t[:, 0:w - 1], op=ALU.subtract), reads=[tfix.b(), U.b()], writes=[DT.b()])
                        for tb in range(NTB):
                            ts = slice(tb * 512, (tb + 1) * 512)
                            ps = psr()
                            S.op("pe", lambda e: e.matmul(ps.t[:], wpo.t[:, g, :], DT.t[:, ts], start=True, stop=True), reads=[wpo.b(), DT.b()], writes=[ps.b()])
                            S.op("dve", lambda e: e.tensor_scalar(out=CT.t[:, g, ts], in0=ps.t[:], scalar1=vec.t[:, vb + V_PSC + g:vb + V_PSC + g + 1], scalar2=None, op0=ALU.mult),
                                 reads=[ps.b(), vec.b()], writes=[CT.b((g, tb))])
                    S.barrier()
                dump_bf16("oc", CT, [(g, tb) for g in range(4) for tb in range(NTB)], 4)
                epilogue(l, 2, lambda kc, ts: CT.t[:, kc, ts], lambda kc, tb: [CT.b((kc, tb))], 4, 128)

        def moe(l):
            vb = l * NVL
            with ExitStack() as ph:
                combT = sb(ph, "combT", [32, T], F32)
                with ExitStack() as p1:
                    h2f = sb(p1, "h2f", [128, 8, 512], F32)
                    wr = sb(p1, "wr", [128, 8, 32], F32)
                    LOG = sb(p1, "LOG", [128, 16, 32], F32)
                    EX = sb(p1, "EX", [128, 16, 32], F32)
                    MSK = sb(p1, "MSK", [128, 16, 32], F32)
                    mx8 = sb(p1, "mx8", [128, 16, 8], F32)
                    negmax = sb(p1, "negmax", [128, 16], F32)
                    den = sb(p1, "den", [128, 16], F32)
                    S.dma("sp", lambda q: q.dma_start(out=wr.t[:], in_=wr_d[l]), d_w[0], writes=[wr.b()])

                    def want(k, tb, tm, biasap):
                        if k is not None:
                            S.op("act", lambda e: e.activation(out=h2f.t[:, k, :], in_=tm.t[:], func=AF.Identity, bias=biasap), reads=[tm.b(), modT.b()], writes=[h2f.b(k)])
                        else:
                            ps = psr()
                            for j in range(4):
                                for k2 in range(8):
                                    S.op("pe", lambda e: e.matmul(ps.t[:, j * 32:(j + 1) * 32], h2f.t[:, k2, j * 128:(j + 1) * 128], wr.t[:, k2, :], start=(k2 == 0), stop=(k2 == 7)),
                                         reads=[h2f.b(k2), wr.b()], writes=[ps.b()], inc=(k2 == 7 and j == 3))
                            S.op("dve", lambda e: e.tensor_tensor(out=LOG.t[:, tb * 4:(tb + 1) * 4, :], in0=ps.t[:, 0:128].rearrange("p (a b) -> p a b", a=4),
                                                                   in1=vec.t[:, vb + V_BR:vb + V_BR + 32].rearrange("p (o b) -> p o b", o=1).broadcast_to([128, 4, 32]), op=ALU.add),
                                 reads=[ps.b(), vec.b()], writes=[LOG.b()])

                    rmsnorm_mod(l, 1, want_f32=want)
                    for tt in range(16):
                        S.op("dve", lambda e: e.max(out=mx8.t[:, tt, :], in_=LOG.t[:, tt, :]), reads=[LOG.b()], writes=[mx8.b()])
                    S.op("dve", lambda e: e.tensor_scalar(out=negmax.t[:], in0=mx8.t[:, :, 0], scalar1=-1.0, scalar2=None, op0=ALU.mult), reads=[mx8.b()], writes=[negmax.b()])
                    for tt in range(16):
                        S.op("act", lambda e: e.activation(out=EX.t[:, tt, :], in_=LOG.t[:, tt, :], func=AF.Exp, bias=negmax.t[:, tt:tt + 1]), reads=[LOG.b(), negmax.b()], writes=[EX.b()])
                        S.op("dve", lambda e: e.tensor_scalar(out=MSK.t[:, tt, :], in0=LOG.t[:, tt, :], scalar1=mx8.t[:, tt, 3:4], scalar2=None, op0=ALU.is_ge), reads=[LOG.b(), mx8.b()], writes=[MSK.b()])
                    S.op("dve", lambda e: e.tensor_tensor(out=EX.t[:], in0=EX.t[:], in1=MSK.t[:], op=ALU.mult), reads=[EX.b(), MSK.b()], writes=[EX.b()])
                    S.op("dve", lambda e: e.tensor_reduce(out=den.t[:], in_=EX.t[:], axis=mybir.AxisListType.X, op=ALU.add), reads=[EX.b()], writes=[den.b()])
                    S.op("dve", lambda e: e.reciprocal(out=den.t[:], in_=den.t[:]), reads=[den.b()], writes=[den.b()])
                    S.op("dve", lambda e: e.tensor_tensor(out=EX.t[:], in0=EX.t[:], in1=den.t[:, :].rearrange("p (a o) -> p a o", o=1).broadcast_to([128, 16, 32]), op=ALU.mult),
                         reads=[EX.b(), den.b()], writes=[EX.b()])
                    for t4 in range(4):
                        ps = psr()
                        for j in range(4):
                            tt = t4 * 4 + j
                            S.op("pe", lambda e: e.transpose(ps.t[0:32, j * 128:(j + 1) * 128], EX.t[:, tt, :], identf.t[:]), reads=[EX.b(), identf.b()], writes=[ps.b()], inc=(j == 3))
                        S.op("act", lambda e: e.copy(out=combT.t[0:32, t4 * 512:(t4 + 1) * 512], in_=ps.t[0:32, :]), reads=[ps.b()], writes=[combT.b()])
                    S.barrier()
                if l == 0:
                    dump("comb0", [combT.b()], combT.t[:, :])
                with ExitStack() as p2:
                    b1l1 = sb(p2, "b1l1", [128, NE, 8], F32)
                    actT = sb(p2, "actT", [128, 8, T], BF16)
                    CB = sb(p2, "CB", [128, T], F32)
                    Lsel = [sb(p2, f"Lsel{i}", [32, 128], F32) for i in range(2)]
                    NR = 4
                    ND = 4
                    w1r = [sb(p2, f"w1r{i}", [128, 8, 256], BF16) for i in range(NR)]
                    w2r = [sb(p2, f"w2r{i}", [128, 8, 128], BF16) for i in range(NR)]
                    d_w1 = [S.dma_sem(f"d_w1_{l}_{i}") for i in range(NR)]
                    d_w2 = [S.dma_sem(f"d_w2_{l}_{i}") for i in range(NR)]
                    b1v = vec.t[:, vb + V_B1:vb + V_B1 + 512].rearrange("p (e c) -> p e c", c=16)
                    S.op("dve", lambda e: e.tensor_scalar(out=b1l1.t[:], in0=b1v[:, :, 8:16], scalar1=1.0, scalar2=None, op0=ALU.add), reads=[vec.b()], writes=[b1l1.b()])

                    def issue_w1(n):
                        if n < NE * 8:
                            e_, j_ = divmod(n, 8)
                            w = w1r[n % NR]
                            S.dma("pool", lambda q: q.dma_start(out=w.t[:], in_=w1_d[l, e_, j_], max_dma_last_dim=8192), d_w1[n % NR], writes=[w.b()])

                    def issue_w2(n):
                        if n < NE * 8:
                            e_, o_ = divmod(n, 8)
                            w = w2r[n % NR]
                            S.dma("pool", lambda q: q.dma_start(out=w.t[:], in_=w2_d[l, e_, o_], max_dma_last_dim=8192), d_w2[n % NR], writes=[w.b()])

                    for n in range(NR - 1):
                        issue_w1(n)
                        issue_w2(n)
                    with ExitStack() as pb:
                        b2s = sb(pb, "b2s", [32, 1024], F32)
                        S.dma("sp", lambda q: q.dma_start(out=b2s.t[:], in_=b2_d[l]), d_w[0], writes=[b2s.b()])
                        for tb in range(NTB):
                            ts = slice(tb * 512, (tb + 1) * 512)
                            for o in range(8):
                                ps = psr()
                                S.op("pe", lambda e: e.matmul(ps.t[:], b2s.t[0:32, o * 128:(o + 1) * 128], combT.t[0:32, ts], start=True, stop=True), reads=[b2s.b(), combT.b()], writes=[ps.b()])
                                S.op("dve", lambda e: e.scalar_tensor_tensor(out=xT.t[:, o, ts], in0=ps.t[:], scalar=modT.t[:, 40 + o:41 + o], in1=xT.t[:, o, ts], op0=ALU.mult, op1=ALU.add),
                                     reads=[ps.b(), modT.b(), XB(o, tb)], writes=[XB(o, tb)])
                        S.barrier()
                    tA = [sb(p2, f"tA{i}", [128, 512], F32) for i in range(ND)]
                    tC = [sb(p2, f"tC{i}", [128, 512], F32) for i in range(ND)]
                    groups = [(j, tb) for j in range(8) for tb in range(NTB)]
                    NG = len(groups)
                    for ex in range(NE):
                        ls = Lsel[ex % 2]
                        S.op("dve", lambda e: e.tensor_scalar(out=ls.t[:], in0=onesf.t[0:32, :], scalar1=identf.t[0:32, ex:ex + 1], scalar2=None, op0=ALU.mult),
                             reads=[onesf.b(), identf.b()], writes=[ls.b()])
                        for tb in range(NTB):
                            ts = slice(tb * 512, (tb + 1) * 512)
                            ps = psr()
                            S.op("pe", lambda e: e.matmul(ps.t[:], ls.t[:], combT.t[0:32, ts], start=True, stop=True), reads=[ls.b(), combT.b()], writes=[ps.b()])
                            S.op("act", lambda e: e.copy(out=CB.t[:, ts], in_=ps.t[:]), reads=[ps.b()], writes=[CB.b(tb)])

                        def stage01(m):
                            j, tb = groups[m]
                            ts = slice(tb * 512, (tb + 1) * 512)
                            n1 = ex * 8 + j
                            if tb == 0:
                                issue_w1(n1 + NR - 1)
                            w = w1r[n1 % NR]
                            bg = vec.t[:, vb + V_B1 + ex * 16 + j:vb + V_B1 + ex * 16 + j + 1]
                            pg = psr()
                            pl = psr()
                            for k in range(8):
                                S.op("pe", lambda e: e.matmul(pg.t[:], w.t[:, k, 0:128], hT.t[:, k, ts], start=(k == 0), stop=(k == 7)), reads=[w.b(), HB(k, tb)], writes=[pg.b()], inc=(k == 7))
                            for k in range(8):
                                S.op("pe", lambda e: e.matmul(pl.t[:], w.t[:, k, 128:256], hT.t[:, k, ts], start=(k == 0), stop=(k == 7)), reads=[w.b(), HB(k, tb)], writes=[pl.b()], inc=(k == 7))
                            a, c_ = tA[m % ND], tC[m % ND]
                            S.op("dve", lambda e: e.tensor_scalar(out=a.t[:], in0=pg.t[:], scalar1=bg, scalar2=7.0, op0=ALU.add, op1=ALU.min), reads=[pg.b(), vec.b()], writes=[a.b()])
                            S.op("act", lambda e: e.activation(out=c_.t[:], in_=pl.t[:], func=AF.Identity, bias=b1l1.t[:, ex, j:j + 1]), reads=[pl.b(), b1l1.b()], writes=[c_.b()])

                        def stage2(m):
                            j, tb = groups[m]
                            ts = slice(tb * 512, (tb + 1) * 512)
                            a, c_ = tA[m % ND], tC[m % ND]
                            S.op("act", lambda e: e.activation(out=a.t[:], in_=a.t[:], func=AF.Gelu_apprx_sigmoid), reads=[a.b()], writes=[a.b()])
                            S.op("pool", lambda e: e.tensor_scalar(out=c_.t[:], in0=c_.t[:], scalar1=8.0, scalar2=-6.0, op0=ALU.min, op1=ALU.max), reads=[c_.b()], writes=[c_.b()])
                            S.op("pool", lambda e: e.tensor_tensor(out=c_.t[:], in0=c_.t[:], in1=CB.t[:, ts], op=ALU.mult), reads=[c_.b(), CB.b(tb)], writes=[c_.b()])

                        def stage3(m):
                            j, tb = groups[m]
                            ts = slice(tb * 512, (tb + 1) * 512)
                            a, c_ = tA[m % ND], tC[m % ND]
                            S.op("dve", lambda e: e.tensor_tensor(out=actT.t[:, j, ts], in0=a.t[:], in1=c_.t[:], op=ALU.mult), reads=[a.b(), c_.b()], writes=[actT.b((j, tb))])

                        for step in range(NG + 2):
                            if step < NG:
                                stage01(step)
                            if 0 <= step - 1 < NG:
                                stage2(step - 1)
                            if 0 <= step - 2 < NG:
                                stage3(step - 2)
                        for o in range(8):
                            n2 = ex * 8 + o
                            issue_w2(n2 + NR - 1)
                            w = w2r[n2 % NR]
                            for tb in range(NTB):
                                ts = slice(tb * 512, (tb + 1) * 512)
                                ps = psr()
                                for k in range(8):
                                    S.op("pe", lambda e: e.matmul(ps.t[:], w.t[:, k, :], actT.t[:, k, ts], start=(k == 0), stop=(k == 7)), reads=[w.b(), actT.b((k, tb))], writes=[ps.b()], inc=(k == 7))
                                S.op("dve", lambda e: e.scalar_tensor_tensor(out=xT.t[:, o, ts], in0=ps.t[:], scalar=modT.t[:, 40 + o:41 + o], in1=xT.t[:, o, ts], op0=ALU.mult, op1=ALU.add),
                                     reads=[ps.b(), modT.b(), XB(o, tb)], writes=[XB(o, tb)])
                    S.barrier()

        XPB = Buf()
        YPB = Buf()
        d_ind = [S.dma_sem(f"d_ind{i}") for i in range(5)]

        def moe_sparse(l):
            vb = l * NVL
            AX = mybir.AxisListType.X
            with ExitStack() as ph:
                IDXi = sb(ph, "IDXi", [128, 4, 16], I32)
                WK = sb(ph, "WK", [128, 4, 16], F32)
                W1I = sb(ph, "W1I", [128, NT, 8], I32)
                W2I = sb(ph, "W2I", [128, NT, 8], I32)
                BI = sb(ph, "BI", [128, 2, NT], I32)
                with ExitStack() as p1:
                    LOG = sb(p1, "LOG", [128, 16, 32], F32)
                    EX = sb(p1, "EX", [128, 16, 32], F32)
                    MSK = sb(p1, "MSK", [128, 16, 32], F32)
                    mx8 = sb(p1, "mx8", [128, 16, 8], F32)
                    negmax = sb(p1, "negmax", [128, 16], F32)
                    den = sb(p1, "den", [128, 16], F32)
                    with ExitStack() as p0:
                        h2f = sb(p0, "h2f", [128, 8, 512], F32)
                        wr = sb(p0, "wr", [128, 8, 32], F32)
                        S.dma("sp", lambda q: q.dma_start(out=wr.t[:], in_=wr_d[l]), d_w[0], writes=[wr.b()])

                        def want(k, tb, tm, biasap):
                            if k is not None:
                                S.op("act", lambda e: e.activation(out=h2f.t[:, k, :], in_=tm.t[:], func=AF.Identity, bias=biasap), reads=[tm.b(), modT.b()], writes=[h2f.b(k)])
                            else:
                                ps = psr()
                                for j in range(4):
                                    for k2 in range(8):
                                        S.op("pe", lambda e: e.matmul(ps.t[:, j * 32:(j + 1) * 32], h2f.t[:, k2, j * 128:(j + 1) * 128], wr.t[:, k2, :], start=(k2 == 0), stop=(k2 == 7)),
                                             reads=[h2f.b(k2), wr.b()], writes=[ps.b()], inc=(k2 == 7 and j == 3))
                                S.op("dve", lambda e: e.tensor_tensor(out=LOG.t[:, tb * 4:(tb + 1) * 4, :], in0=ps.t[:, 0:128].rearrange("p (a b) -> p a b", a=4),
                                                                       in1=vec.t[:, vb + V_BR:vb + V_BR + 32].rearrange("p (o b) -> p o b", o=1).broadcast_to([128, 4, 32]), op=ALU.add),
                                     reads=[ps.b(), vec.b()], writes=[LOG.b()])

                        rmsnorm_mod(l, 1, want_f32=want)
                    for tt in range(16):
                        S.op("dve", lambda e: e.max(out=mx8.t[:, tt, :], in_=LOG.t[:, tt, :]), reads=[LOG.b()], writes=[mx8.b()])
                    S.op("dve", lambda e: e.tensor_scalar(out=negmax.t[:], in0=mx8.t[:, :, 0], scalar1=-1.0, scalar2=None, op0=ALU.mult), reads=[mx8.b()], writes=[negmax.b()])
                    for tt in range(16):
                        S.op("act", lambda e: e.activation(out=EX.t[:, tt, :], in_=LOG.t[:, tt, :], func=AF.Exp, bias=negmax.t[:, tt:tt + 1]), reads=[LOG.b(), negmax.b()], writes=[EX.b()])
                        S.op("dve", lambda e: e.tensor_scalar(out=MSK.t[:, tt, :], in0=LOG.t[:, tt, :], scalar1=mx8.t[:, tt, 3:4], scalar2=None, op0=ALU.is_ge), reads=[LOG.b(), mx8.b()], writes=[MSK.b()])
                    S.op("dve", lambda e: e.tensor_tensor(out=EX.t[:], in0=EX.t[:], in1=MSK.t[:], op=ALU.mult), reads=[EX.b(), MSK.b()], writes=[EX.b()])
                    S.op("dve", lambda e: e.tensor_reduce(out=den.t[:], in_=EX.t[:], axis=AX, op=ALU.add), reads=[EX.b()], writes=[den.b()])
                    S.op("dve", lambda e: e.reciprocal(out=den.t[:], in_=den.t[:]), reads=[den.b()], writes=[den.b()])
                    S.op("dve", lambda e: e.tensor_tensor(out=EX.t[:], in0=EX.t[:], in1=den.t[:, :].rearrange("p (a o) -> p a o", o=1).broadcast_to([128, 16, 32]), op=ALU.mult),
                         reads=[EX.b(), den.b()], writes=[EX.b()])
                    MSKb = sb(p1, "MSKb", [128, 16, 32], BF16)
                    RANK = sb(p1, "RANK", [128, 16, 32], F32)
                    G = sb(p1, "G", [128, 16, 32], F32)
                    OH = sb(p1, "OH", [128, 16, 32], F32)
                    TMP = sb(p1, "TMP", [128, 16, 32], F32)
                    CNT = sb(p1, "CNT", [128, 32], F32)
                    CNTi = sb(p1, "CNTi", [128, 32], I32)
                    NTf = sb(p1, "NTf", [128, 32], F32)
                    TB = sb(p1, "TB", [128, 32], F32)
                    IDXf = sb(p1, "IDXf", [128, 4, 16], F32)
                    TBc = sb(p1, "TBc", [32, 1], F32)
                    t32 = sb(p1, "t32", [32, 32], F32)
                    CMPb = sb(p1, "CMPb", [32, NT], BF16)
                    TEXP = sb(p1, "TEXP", [128, NT], F32)
                    BASE = sb(p1, "BASE", [128, NT], F32)
                    UNU = sb(p1, "UNU", [128, NT], F32)
                    WIf = sb(p1, "WIf", [128, NT, 8], F32)
                    BIf = sb(p1, "BIf", [128, 2, NT], F32)
                    S.op("dve", lambda e: e.tensor_copy(out=MSKb.t[:], in_=MSK.t[:]), reads=[MSK.b()], writes=[MSKb.b()])
                    for tt in range(16):
                        cols = slice(tt * 32, (tt + 1) * 32)
                        for t2 in range(tt):
                            S.op("pe", lambda e: e.matmul(ACC0.t[:, cols], onesb.t[:], MSKb.t[:, t2, :], start=(t2 == 0), stop=False),
                                 reads=[onesb.b(), MSKb.b()], writes=[ACC0.b()], inc=False)
                        S.op("pe", lambda e: e.matmul(ACC0.t[:, cols], trib.t[:], MSKb.t[:, tt, :], start=(tt == 0), stop=True),
                             reads=[trib.b(), MSKb.b()], writes=[ACC0.b()])
                    S.op("dve", lambda e: e.tensor_tensor(out=RANK.t[:].rearrange("p a b -> p (a b)"), in0=ACC0.t[:], in1=MSK.t[:].rearrange("p a b -> p (a b)"), op=ALU.subtract),
                         reads=[ACC0.b(), MSK.b()], writes=[RANK.b()])
                    pc = psr()
                    for tt in range(16):
                        S.op("pe", lambda e: e.matmul(pc.t[:, 0:32], onesb.t[:], MSKb.t[:, tt, :], start=(tt == 0), stop=(tt == 15)),
                             reads=[onesb.b(), MSKb.b()], writes=[pc.b()], inc=(tt == 15))
                    S.op("dve", lambda e: e.tensor_scalar(out=CNT.t[:], in0=pc.t[:, 0:32], scalar1=float(TS - 1), scalar2=None, op0=ALU.add), reads=[pc.b()], writes=[CNT.b()])
                    S.op("dve", lambda e: e.tensor_copy(out=CNTi.t[:], in_=CNT.t[:]), reads=[CNT.b()], writes=[CNTi.b()])
                    S.op("dve", lambda e: e.tensor_single_scalar(out=CNTi.t[:], in_=CNTi.t[:], scalar=9, op=ALU.arith_shift_right), reads=[CNTi.b()], writes=[CNTi.b()])
                    S.op("dve", lambda e: e.tensor_copy(out=NTf.t[:], in_=CNTi.t[:]), reads=[CNTi.b()], writes=[NTf.b()])
                    S.op("dve", lambda e: e.tensor_tensor_scan(out=TB.t[:], data0=onesf.t[:, 0:32], data1=NTf.t[:], initial=0.0, op0=ALU.mult, op1=ALU.add),
                         reads=[onesf.b(), NTf.b()], writes=[TB.b()])
                    S.op("dve", lambda e: e.tensor_scalar(out=UNU.t[:], in0=cstf.t[:, 19:19 + NT], scalar1=TB.t[:, 31:32], scalar2=1.0e6, op0=ALU.is_ge, op1=ALU.mult),
                         reads=[cstf.b(), TB.b()], writes=[UNU.b()])
                    S.op("dve", lambda e: e.tensor_tensor(out=TB.t[:], in0=TB.t[:], in1=NTf.t[:], op=ALU.subtract), reads=[TB.b(), NTf.b()], writes=[TB.b()])
                    S.op("dve", lambda e: e.scalar_tensor_tensor(out=G.t[:], in0=TB.t[:, :].rearrange("p (o b) -> p o b", o=1).broadcast_to([128, 16, 32]), scalar=float(TS), in1=RANK.t[:],
                                                                  op0=ALU.mult, op1=ALU.add), reads=[TB.b(), RANK.b()], writes=[G.b()])
                    for k in range(4):
                        S.op("dve", lambda e: e.tensor_tensor(out=OH.t[:], in0=LOG.t[:], in1=mx8.t[:, :, k:k + 1].broadcast_to([128, 16, 32]), op=ALU.is_equal),
                             reads=[LOG.b(), mx8.b()], writes=[OH.b()])
                        S.op("dve", lambda e: e.tensor_tensor(out=TMP.t[:], in0=OH.t[:], in1=G.t[:], op=ALU.mult), reads=[OH.b(), G.b()], writes=[TMP.b()])
                        S.op("dve", lambda e: e.tensor_reduce(out=IDXf.t[:, k, :], in_=TMP.t[:], axis=AX, op=ALU.add), reads=[TMP.b()], writes=[IDXf.b()])
                        S.op("dve", lambda e: e.tensor_tensor(out=TMP.t[:], in0=OH.t[:], in1=EX.t[:], op=ALU.mult), reads=[OH.b(), EX.b()], writes=[TMP.b()])
                        S.op("dve", lambda e: e.tensor_reduce(out=WK.t[:, k, :], in_=TMP.t[:], axis=AX, op=ALU.add), reads=[TMP.b()], writes=[WK.b()])
                    S.op("dve", lambda e: e.tensor_scalar(out=IDXf.t[:], in0=IDXf.t[:], scalar1=float(NROW - 1), scalar2=0.0, op0=ALU.min, op1=ALU.max), reads=[IDXf.b()], writes=[IDXf.b()])
                    S.op("dve", lambda e: e.tensor_copy(out=IDXi.t[:], in_=IDXf.t[:]), reads=[IDXf.b()], writes=[IDXi.b()])
                    S.op("dve", lambda e: e.tensor_tensor(out=t32.t[:], in0=TB.t[0:32, :], in1=identf.t[0:32, 0:32], op=ALU.mult), reads=[TB.b(), identf.b()], writes=[t32.b()])
                    S.op("dve", lambda e: e.tensor_reduce(out=TBc.t[:], in_=t32.t[:], axis=AX, op=ALU.add), reads=[t32.b()], writes=[TBc.b()])
                    S.op("dve", lambda e: e.tensor_scalar(out=CMPb.t[:], in0=cstf.t[0:32, 19:19 + NT], scalar1=TBc.t[:, 0:1], scalar2=None, op0=ALU.is_ge), reads=[cstf.b(), TBc.b()], writes=[CMPb.b()])
                    pt = psr()
                    S.op("pe", lambda e: e.matmul(pt.t[:, 0:NT], onesb.t[0:32, :], CMPb.t[:], start=True, stop=True), reads=[onesb.b(), CMPb.b()], writes=[pt.b()])
                    S.op("dve", lambda e: e.tensor_scalar(out=TEXP.t[:], in0=pt.t[:, 0:NT], scalar1=-1.0, scalar2=None, op0=ALU.add), reads=[pt.b()], writes=[TEXP.b()])
                    pcol = cstf.t[:, 18:19]
                    S.op("dve", lambda e: e.tensor_scalar(out=BASE.t[:], in0=TEXP.t[:], scalar1=1024.0, scalar2=pcol, op0=ALU.mult, op1=ALU.add), reads=[TEXP.b(), cstf.b()], writes=[BASE.b()])
                    for jj in range(8):
                        S.op("dve", lambda e: e.tensor_scalar(out=WIf.t[:, :, jj], in0=BASE.t[:], scalar1=float(l * 32768 + jj * 128), scalar2=None, op0=ALU.add), reads=[BASE.b()], writes=[WIf.b()])
                    S.op("dve", lambda e: e.tensor_copy(out=W1I.t[:], in_=WIf.t[:]), reads=[WIf.b()], writes=[W1I.b()])
                    S.op("dve", lambda e: e.tensor_scalar(out=BASE.t[:], in0=TEXP.t[:], scalar1=1024.0, scalar2=float(l * 32768), op0=ALU.mult, op1=ALU.add), reads=[TEXP.b()], writes=[BASE.b()])
                    S.op("dve", lambda e: e.scalar_tensor_tensor(out=BASE.t[:], in0=cstf.t[:, 18:19].broadcast_to([128, NT]), scalar=8.0, in1=BASE.t[:], op0=ALU.mult, op1=ALU.add),
                         reads=[cstf.b(), BASE.b()], writes=[BASE.b()])
                    for k in range(8):
                        S.op("dve", lambda e: e.tensor_scalar(out=WIf.t[:, :, k], in0=BASE.t[:], scalar1=float(k), scalar2=None, op0=ALU.add), reads=[BASE.b()], writes=[WIf.b()])
                    S.op("dve", lambda e: e.tensor_copy(out=W2I.t[:], in_=WIf.t[:]), reads=[WIf.b()], writes=[W2I.b()])
                    S.op("dve", lambda e: e.tensor_scalar(out=BIf.t[:, 0, :], in0=TEXP.t[:], scalar1=128.0, scalar2=pcol, op0=ALU.mult, op1=ALU.add), reads=[TEXP.b(), cstf.b()], writes=[BIf.b()])
                    S.op("dve", lambda e: e.tensor_scalar(out=BIf.t[:, 0, :], in0=BIf.t[:, 0, :], scalar1=float(l * 4096), scalar2=None, op0=ALU.add), reads=[BIf.b()], writes=[BIf.b()])
                    S.op("dve", lambda e: e.tensor_scalar(out=BIf.t[:, 1, :], in0=TEXP.t[:], scalar1=float(l * 32), scalar2=None, op0=ALU.add), reads=[TEXP.b()], writes=[BIf.b()])
                    S.op("dve", lambda e: e.tensor_copy(out=BI.t[:], in_=BIf.t[:]), reads=[BIf.b()], writes=[BI.b()])
                    if l == 0 and "texp" in dbg_out:
                        dump("texp", [TEXP.b()], TEXP.t[:, :])
                        dump("idxf", [IDXf.b()], IDXf.t[:, :, :].rearrange("p a b -> p (a b)"))
                        dump("wk", [WK.b()], WK.t[:, :, :].rearrange("p a b -> p (a b)"))
                        dump("w1i", [W1I.b()], W1I.t[:, :, :].rearrange("p a b -> p (a b)"))
                        dump("w2i", [W2I.b()], W2I.t[:, :, :].rearrange("p a b -> p (a b)"))
                        dump("bi", [BI.b()], BI.t[:, :, :].rearrange("p a b -> p (a b)"))
                    stg = [sb(p1, f"stg{i}", [128, 1024], BF16) for i in range(2)]
                    d_stg = [S.dma_sem(f"d_stg_{l}_{i}") for i in range(2)]
                    for tt in range(16 if SPARSE_STOP != 1 else 0):
                        for k in range(8):
                            S.op("pe", lambda e: e.transpose(PSBF.t[:, k * 128:(k + 1) * 128], hT.t[:, k, tt * 128:(tt + 1) * 128], identb.t[:]),
                                 reads=[HB(k, tt // 4), identb.b()], writes=[PSBF.b()], inc=(k == 7))
                        st = stg[tt % 2]
                        S.op("act", lambda e: e.copy(out=st.t[:], in_=PSBF.t[:, :]), reads=[PSBF.b()], writes=[st.b()])
                        for k in range(4):
                            S.dma("pool", lambda q: q.indirect_dma_start(out=xp_d[:, :], out_offset=bass.IndirectOffsetOnAxis(ap=IDXi.t[:, k, tt:tt + 1], axis=0), in_=st.t[:, :], in_offset=None),
                                  d_stg[tt % 2], reads=[st.b(), IDXi.b()])
                    S.barrier()
                if SPARSE_STOP in (1, 2):
                    return
                with ExitStack() as p2:
                    Xtok = sb(p2, "Xtok", [128, 4, 1024], BF16)
                    XT = [sb(p2, f"XT{i}", [128, 8, TS], BF16) for i in range(2)]
                    NR = 5
                    ND = 4
                    w1r = [sb(p2, f"w1r{i}", [128, 8, 256], BF16) for i in range(NR)]
                    d_w1 = [S.dma_sem(f"d_w1s_{l}_{i}") for i in range(NR)]
                    d_w2 = [S.dma_sem(f"d_w2s_{l}_{i}") for i in range(2)]
                    d_b = [S.dma_sem(f"d_bs_{l}_{i}") for i in range(2)]
                    d_b2 = [S.dma_sem(f"d_b2s_{l}_{i}") for i in range(2)]
                    d_x = S.dma_sem(f"d_x_{l}")
                    d_y = [S.dma_sem(f"d_y_{l}_{i}") for i in range(3)]
                    W2B = [Buf(), Buf()]
                    w2v = [hT.t[:, :, i * 1024:(i + 1) * 1024] for i in range(2)]
                    b1t = [sb(p2, f"b1t{i}", [128, 16], F32) for i in range(2)]
                    b2bc = [sb(p2, f"b2bc{i}", [128, 1024], F32) for i in range(2)]
                    actT = sb(p2, "actTs", [128, 8, TS], BF16)
                    tA = [sb(p2, f"tA{i}", [128, TS], F32) for i in range(ND)]
                    tC = [sb(p2, f"tC{i}", [128, TS], F32) for i in range(ND)]
                    Yo = [sb(p2, f"Yo{i}", [128, 1024], F32) for i in range(3)]
                    w1rows = w1_d.rearrange("l e j p k n -> (l e j p) (k n)")
                    w2rows = w2f_d.rearrange("l e p k o -> (l e p k) o")
                    b2rows = b2_d.rearrange("l e o -> (l e) o")

                    def issue_w1(n):
                        if n < NT * 8:
                            j_, jj_ = divmod(n, 8)
                            w = w1r[n % NR]
                            S.dma("pool", lambda q: q.indirect_dma_start(out=w.t[:, :, :].rearrange("p k n -> p (k n)"), out_offset=None, in_=w1rows,
                                                                          in_offset=bass.IndirectOffsetOnAxis(ap=W1I.t[:, j_, jj_:jj_ + 1], axis=0)),
                                  d_w1[n % NR], reads=[W1I.b()], writes=[w.b()])

                    def issue_tile_misc(j_):
                        if j_ < NT:
                            i = j_ % 2
                            for k in range(8):
                                S.dma("pool", lambda q: q.indirect_dma_start(out=w2v[i][:, k, :], out_offset=None, in_=w2rows,
                                                                              in_offset=bass.IndirectOffsetOnAxis(ap=W2I.t[:, j_, k:k + 1], axis=0)),
                                      d_w2[i], reads=[W2I.b()], writes=[W2B[i]], no_waw=True)
                            S.dma("pool", lambda q: q.indirect_dma_start(out=b1t[i].t[:, :], out_offset=None, in_=b1r_d,
                                                                          in_offset=bass.IndirectOffsetOnAxis(ap=BI.t[:, 0, j_:j_ + 1], axis=0)),
                                  d_b[i], reads=[BI.b()], writes=[b1t[i].b()])
                            S.dma("pool", lambda q: q.indirect_dma_start(out=b2bc[i].t[:, :], out_offset=None, in_=b2rows,
                                                                          in_offset=bass.IndirectOffsetOnAxis(ap=BI.t[:, 1, j_:j_ + 1], axis=0)),
                                  d_b2[i], reads=[BI.b()], writes=[b2bc[i].b()])

                    def load_x(j_):
                        if j_ < NT:
                            S.dma("sp", lambda q: q.dma_start(out=Xtok.t[:], in_=xp_d[j_ * TS:(j_ + 1) * TS, :].rearrange("(a p) f -> p a f", p=128)), d_x, writes=[Xtok.b()])
                            xt = XT[j_ % 2]
                            for kk in range(4):
                                for k in (2 * kk, 2 * kk + 1):
                                    for it in range(4):
                                        S.op("pe", lambda e: e.transpose(PSBF.t[:, (k % 2) * 512 + it * 128:(k % 2) * 512 + (it + 1) * 128], Xtok.t[:, it, k * 128:(k + 1) * 128], identb.t[:]),
                                             reads=[Xtok.b(), identb.b()], writes=[PSBF.b()], inc=(k % 2 == 1 and it == 3))
                                S.op("act", lambda e: e.copy(out=xt.t[:, 2 * kk:2 * kk + 2, :], in_=PSBF.t[:, :].rearrange("p (a b) -> p a b", a=2)), reads=[PSBF.b()], writes=[xt.b()])

                    def b1fix(j_):
                        i = j_ % 2
                        S.op("dve", lambda e: e.tensor_scalar(out=b1t[i].t[:, 8:16], in0=b1t[i].t[:, 8:16], scalar1=1.0, scalar2=None, op0=ALU.add), reads=[b1t[i].b()], writes=[b1t[i].b()])

                    def stage01(j_, m):
                        i2 = j_ % 2
                        xt = XT[i2]
                        n1 = j_ * 8 + m
                        issue_w1(n1 + NR - 1)
                        w = w1r[n1 % NR]
                        pg = psr()
                        pl = psr()
                        for k in range(8):
                            S.op("pe", lambda e: e.matmul(pg.t[:], w.t[:, k, 0:128], xt.t[:, k, :], start=(k == 0), stop=(k == 7)), reads=[w.b(), xt.b()], writes=[pg.b()], inc=(k == 7))
                        for k in range(8):
                            S.op("pe", lambda e: e.matmul(pl.t[:], w.t[:, k, 128:256], xt.t[:, k, :], start=(k == 0), stop=(k == 7)), reads=[w.b(), xt.b()], writes=[pl.b()], inc=(k == 7))
                        a, c_ = tA[n1 % ND], tC[n1 % ND]
                        S.op("dve", lambda e: e.tensor_scalar(out=a.t[:], in0=pg.t[:], scalar1=b1t[i2].t[:, m:m + 1], scalar2=7.0, op0=ALU.add, op1=ALU.min), reads=[pg.b(), b1t[i2].b()], writes=[a.b()])
                        S.op("act", lambda e: e.activation(out=c_.t[:], in_=pl.t[:], func=AF.Identity, bias=b1t[i2].t[:, 8 + m:9 + m]), reads=[pl.b(), b1t[i2].b()], writes=[c_.b()])

                    def stage2(j_, m):
                        n1 = j_ * 8 + m
                        a, c_ = tA[n1 % ND], tC[n1 % ND]
                        S.op("act", lambda e: e.activation(out=a.t[:], in_=a.t[:], func=AF.Gelu_apprx_sigmoid), reads=[a.b()], writes=[a.b()])
                        S.op("dve", lambda e: e.tensor_scalar(out=c_.t[:], in0=c_.t[:], scalar1=8.0, scalar2=-6.0, op0=ALU.min, op1=ALU.max), reads=[c_.b()], writes=[c_.b()])

                    def stage3(j_, m):
                        n1 = j_ * 8 + m
                        a, c_ = tA[n1 % ND], tC[n1 % ND]
                        S.op("dve", lambda e: e.tensor_tensor(out=actT.t[:, m, :], in0=a.t[:], in1=c_.t[:], op=ALU.mult), reads=[a.b(), c_.b()], writes=[actT.b(m)])

                    def steps(j_, lo, hi):
                        for step in range(lo, hi):
                            if step < 8:
                                stage01(j_, step)
                            if 0 <= step - 1 < 8:
                                stage2(j_, step - 1)
                            if 0 <= step - 2 < 8:
                                stage3(j_, step - 2)

                    for n in range(NR - 1):
                        issue_w1(n)
                    issue_tile_misc(0)
                    load_x(0)
                    b1fix(0)
                    steps(0, 0, 2)
                    for j in range(NT):
                        i2 = j % 2
                        issue_tile_misc(j + 1)
                        steps(j, 2, 10)
                        if j + 1 < NT:
                            load_x(j + 1)
                            b1fix(j + 1)
                            steps(j + 1, 0, 2)
                        for it in range(4):
                            yo = Yo[it % len(Yo)]
                            for half in range(2):
                                ps = psr()
                                for k in range(8):
                                    S.op("pe", lambda e: e.matmul(ps.t[:], actT.t[:, k, it * 128:(it + 1) * 128], w2v[i2][:, k, half * 512:(half + 1) * 512], start=(k == 0), stop=(k == 7)),
                                         reads=[actT.b(k), W2B[i2]], writes=[ps.b()], inc=(k == 7))
                                S.op("dve", lambda e: e.tensor_tensor(out=yo.t[:, half * 512:(half + 1) * 512], in0=ps.t[:], in1=b2bc[i2].t[:, half * 512:(half + 1) * 512], op=ALU.add),
                                     reads=[ps.b(), b2bc[i2].b()], writes=[yo.b()])
                            S.dma("sp", lambda q: q.dma_start(out=yp_d[j * TS + it * 128:j * TS + (it + 1) * 128, :], in_=yo.t[:]), d_y[it % len(Yo)], reads=[yo.b()])
                    S.barrier()
                if SPARSE_STOP == 3:
                    return
                with ExitStack() as p3:
                    Gk = [sb(p3, f"Gk{i}", [128, 1024], F32) for i in range(4)]
                    ACCt = [sb(p3, f"ACCt{i}", [128, 1024], F32) for i in range(2)]
                    for tt in range(16):
                        acc = ACCt[tt % 2]
                        for k in range(4):
                            S.dma("pool", lambda q: q.indirect_dma_start(out=Gk[k].t[:, :], out_offset=None, in_=yp_d[:, :],
                                                                          in_offset=bass.IndirectOffsetOnAxis(ap=IDXi.t[:, k, tt:tt + 1], axis=0)),
                                  d_ind[1 + k], reads=[IDXi.b()], writes=[Gk[k].b()])
                        for k in range(4):
                            if k == 0:
                                S.op("dve", lambda e: e.tensor_scalar(out=acc.t[:], in0=Gk[k].t[:], scalar1=WK.t[:, k, tt:tt + 1], scalar2=None, op0=ALU.mult), reads=[Gk[k].b(), WK.b()], writes=[acc.b()])
                            else:
                                S.op("dve", lambda e: e.scalar_tensor_tensor(out=acc.t[:], in0=Gk[k].t[:], scalar=WK.t[:, k, tt:tt + 1], in1=acc.t[:], op0=ALU.mult, op1=ALU.add),
                                     reads=[Gk[k].b(), WK.b(), acc.b()], writes=[acc.b()])
                        for o4 in range(2):
                            ps = psr()
                            for oo in range(4):
                                o = o4 * 4 + oo
                                S.op("pe", lambda e: e.transpose(ps.t[:, oo * 128:(oo + 1) * 128], acc.t[:, o * 128:(o + 1) * 128], identf.t[:]), reads=[acc.b(), identf.b()], writes=[ps.b()], inc=(oo == 3))
                            for oo in range(4):
                                o = o4 * 4 + oo
                                xs = xT.t[:, o, tt * 128:(tt + 1) * 128]
                                S.op("dve", lambda e: e.scalar_tensor_tensor(out=xs, in0=ps.t[:, oo * 128:(oo + 1) * 128], scalar=modT.t[:, 40 + o:41 + o], in1=xs, op0=ALU.mult, op1=ALU.add),
                                     reads=[ps.b(), modT.b(), XB(o, tt // 4)], writes=[XB(o, tt // 4)])
                    S.barrier()

        def dump_x(name):
            if name in dbg_out:
                for k in range(8):
                    S.dma("sp", lambda q: q.dma_start(out=dbg_out[name][k], in_=xT.t[:, k, :]), d_dbg, reads=[XB(k, tb) for tb in range(NTB)])

        def dump_bf16(name, tl, keys, n):
            if name not in dbg_out:
                return
            with ExitStack() as ph:
                st = sb(ph, "dbgst", [128, T], F32)
                for i in range(n):
                    S.op("dve", lambda e: e.tensor_copy(out=st.t[:], in_=tl.t[:, i, :]), reads=tl.bs(keys), writes=[st.b()])
                    S.dma("sp", lambda q: q.dma_start(out=dbg_out[name][i], in_=st.t[:]), d_dbg, reads=[st.b()])
                S.barrier()

        for l in range(L):
            adaln(l)
            if l == 0:
                dump("mod0", [modT.b()], modT.t[:, :])
            rmsnorm_mod(l, 0)
            if l == 0:
                dump_bf16("h0", hT, [(k, tb) for k in range(8) for tb in range(NTB)], 8)
            if "hgrn" not in SKIP:
                hgrn(l)
            if l == 0:
                dump_x("xa0")
            if STOP_AFTER == "hgrn":
                break
            if "mla" not in SKIP:
                mla(l)
            if STOP_AFTER == "mla":
                break
            if "pool" not in SKIP:
                poolmix(l)
            if l == 0:
                dump_x("xm0")
            if STOP_AFTER == "pool":
                break
            moe_sparse(l)
            if l == 0:
                dump_x("x0")
            if STOP_AFTER == "moe":
                break

        for k in range(8):
            S.dma("sp", lambda q: q.dma_start(out=yT_d[k], in_=xT.t[:, k, :]), d_st, reads=[XB(k, tb) for tb in range(NTB)])
        S.E["sp"].obj.wait_ge(d_st.h, d_st.val)
        if d_dbg.val:
            S.E["sp"].obj.wait_ge(d_dbg.h, d_dbg.val)
    return nc


STOP_AFTER = None
SKIP = set()
SPARSE_STOP = None


def _kp(w, ncols=None):
    K = w.shape[0] // 128
    return np.ascontiguousarray(w.reshape(K, 128, w.shape[1]).transpose(1, 0, 2))


def _col(v, n=128):
    K = v.shape[0] // n
    out = np.zeros((128, K), np.float32)
    out[:n, :] = v.reshape(K, n).T
    return out


def prepare_shared(inp):
    f = np.float32
    sh = {}
    ada_w, w_in = inp["ada_w"], inp["w_in"]
    sh["adaw"] = np.ascontiguousarray(np.stack([np.stack([_kp(ada_w[l][:, ty * 1024:(ty + 1) * 1024]) for ty in range(6)]) for l in range(L)]))
    whg = np.zeros((L, 4, 128, 8, 512), f)
    for l in range(L):
        for hd in range(4):
            for i, off in enumerate((O_HQ, O_HF, O_HI, O_HG)):
                whg[l, hd, :, :, i * 128:(i + 1) * 128] = _kp(w_in[l][:, off + hd * 128:off + (hd + 1) * 128])
    sh["whg"] = whg
    wmla = np.zeros((L, 128, 8, 832), f)
    for l in range(L):
        wmla[l, :, :, 0:640] = _kp(w_in[l][:, O_QL:O_QL + 640])
        r = _kp(w_in[l][:, O_KR:O_KR + 32])
        wmla[l, :, :, 640 + 64:640 + 96] = r
        wmla[l, :, :, 736 + 64:736 + 80] = r[:, :, 16:32]
        wmla[l, :, :, 736 + 80:736 + 96] = r[:, :, 0:16]
    sh["wmla"] = wmla
    sh["wpl"] = np.stack([_kp(w_in[l][:, O_PL:O_PL + 512]) for l in range(L)])
    sh["wgate"] = np.stack([np.stack([_kp(w_in[l][:, o:o + 1024]) for o in (O_GA, O_GB, O_GC)]) for l in range(L)])
    wuq = np.zeros((L, 128, 3, 1536), f)
    for l in range(L):
        a = _kp(inp["w_uq"][l])
        wuq[l, :, :, 0:768] = a
        s = a.reshape(128, 3, 8, 96).copy()
        s2 = s.copy()
        s2[..., 64:80] = s[..., 80:96]
        s2[..., 80:96] = s[..., 64:80]
        wuq[l, :, :, 768:1536] = s2.reshape(128, 3, 768)
    sh["wuq"] = wuq
    wukv = np.zeros((L, 128, 2, 1024), f)
    for l in range(L):
        a = _kp(inp["w_ukv"][l]).reshape(128, 2, 8, 128)
        wukv[l, :, :, 0:512] = a[..., 0:64].reshape(128, 2, 512)
        wukv[l, :, :, 512:1024] = a[..., 64:128].reshape(128, 2, 512)
    sh["wukv"] = wukv
    sh["wpool"] = np.ascontiguousarray(inp["w_pool"].transpose(0, 2, 1, 3))
    sh["wbr"] = np.stack([np.stack([_kp(inp[n][l]) for n in ("w_br_a", "w_br_b", "w_br_c")]) for l in range(L)])
    sh["wout"] = np.stack([_kp(inp["w_out"][l]) for l in range(L)])
    sh["wr"] = np.stack([_kp(inp["w_router"][l]) for l in range(L)])
    w1 = inp["w_exp1"].reshape(L, NE, 8, 128, 2, 8, 128)
    sh["w1"] = np.ascontiguousarray(w1.transpose(0, 1, 5, 3, 2, 4, 6)).reshape(L, NE, 8, 128, 8, 256)
    w2 = inp["w_exp2"].reshape(L, NE, 8, 128, 1024)
    sh["w2f"] = np.ascontiguousarray(w2.transpose(0, 1, 3, 2, 4))
    sh["b1r"] = np.ascontiguousarray(inp["b_exp1"].reshape(L, NE, 16, 128).transpose(0, 1, 3, 2)).reshape(L * NE * 128, 16)
    sh["b2"] = np.ascontiguousarray(inp["b_exp2"])
    vec = np.zeros((128, L * NVL), f)
    for l in range(L):
        b = l * NVL
        vec[:, b + V_N1G:b + V_N1G + 8] = _col(inp["norm1_g"][l])
        vec[:, b + V_N2G:b + V_N2G + 8] = _col(inp["norm2_g"][l])
        vec[:, b + V_ADAB:b + V_ADAB + 48] = _col(inp["ada_b"][l])
        vec[:, b + V_LB:b + V_LB + 4] = _col(inp["hgrn_lb"][l])
        vec[:, b + V_ONG:b + V_ONG + 1] = _col(inp["hgrn_onorm_g"][l])
        vec[:, b + V_QLG:b + V_QLG + 3] = _col(inp["mla_qlat_g"][l])
        vec[:, b + V_KVG:b + V_KVG + 2] = _col(inp["mla_kvlat_g"][l])
        for name, c0, c1 in (("q_norm_g", V_QNG, V_QNGP), ("k_norm_g", V_KNG, V_KNGP)):
            g = inp[name][l]
            vec[:96, b + c0] = g
            gp = g.copy()
            gp[64:80] = g[80:96]
            gp[80:96] = g[64:80]
            vec[:96, b + c1] = gp
        vec[:, b + V_PSC:b + V_PSC + 4] = _col(inp["pool_scale"][l])
        vec[:, b + V_BR:b + V_BR + 32] = np.broadcast_to(inp["b_router"][l][None, :], (128, 32))
        vec[:, b + V_B1:b + V_B1 + 512] = inp["b_exp1"][l].reshape(NE, 16, 128).transpose(2, 0, 1).reshape(128, 512)
    sh["vec"] = vec
    cst = np.zeros((128, NCST), f)
    cst[:, C_ID:C_ID + 128] = np.eye(128, dtype=f)
    cst[:, C_TRI:C_TRI + 128] = np.triu(np.ones((128, 128), f))
    rm = np.ones(T, f)
    rm[::C] = 0.0
    cst[:, C_RMASK:C_RMASK + T] = rm[None, :]
    cst[:, C_INVC:C_INVC + 16] = (1.0 / np.arange(1, 17, dtype=np.float64)).astype(f)[None, :]
    invf = 1.0 / (10000.0 ** (np.arange(0, 32, 2, dtype=np.float64) / 32.0))
    cst[64:80, C_INVF] = (invf / (2 * math.pi)).astype(f)
    cst[80:96, C_INVF] = (invf / (2 * math.pi)).astype(f)
    sc = 2 * math.pi * (1 - 1e-6)
    cst[64:80, C_SINSC] = -sc
    cst[80:96, C_SINSC] = sc
    cst[:, C_PCOL] = np.arange(128, dtype=f)
    cst[:, C_IOTA:C_IOTA + 48] = np.arange(48, dtype=f)[None, :]
    sh["cst"] = cst
    return sh


def kernel(**inputs):
    inp = {k: np.asarray(v) for k, v in inputs.items()}
    B = inp["x"].shape[0]
    sh = prepare_shared(inp)
    in_maps = []
    for b in range(B):
        m = dict(sh)
        m["xT"] = np.ascontiguousarray(inp["x"][b].T.reshape(8, 128, T))
        m["cT"] = np.ascontiguousarray(inp["c"][b].reshape(8, 128).T)
        m["pos"] = np.ascontiguousarray(inp["positions"][b].reshape(1, T).astype(np.int32))
        in_maps.append(m)
    nc = build_program()
    res = run_bass_kernel_spmd(nc, in_maps, core_ids=list(range(B)))
    out = np.empty((B, T, D), np.float32)
    for b in range(B):
        out[b] = res.results[b]["yT"].reshape(D, T).T
    return out
```

```python
import math
from contextlib import ExitStack

import numpy as np
import concourse.bass as bass
import concourse.mybir as mybir
from concourse.bass_utils import run_bass_kernel_spmd

F32 = mybir.dt.float32
BF16 = mybir.dt.bfloat16
I32 = mybir.dt.int32
AF = mybir.ActivationFunctionType
ALU = mybir.AluOpType

L = 2
D = 1024
T = 2048
NE = 32
EPS = 1e-6
NTB = 4
C = 64
NCH = T // C

V_N1G = 0
V_N2G = 8
V_ADAB = 16
V_LB = 64
V_ONG = 68
V_QLG = 69
V_KVG = 72
V_QNG = 74
V_QNGP = 75
V_KNG = 76
V_KNGP = 77
V_PSC = 78
V_BR = 82
V_B1 = 114
NVL = 626
C_ID = 0
C_TRI = 128
C_RMASK = 256
C_INVC = 256 + 2048
C_INVF = C_INVC + 16
C_SINSC = C_INVF + 1
C_PCOL = C_SINSC + 1
C_IOTA = C_PCOL + 1
NCST = C_IOTA + 48
TS = 512
NT = 48
NROW = NT * TS

O_HQ, O_HF, O_HI, O_HG, O_QL, O_KVL, O_KR, O_PL, O_GA, O_GB, O_GC = 0, 512, 1024, 1536, 2048, 2432, 2688, 2720, 3232, 4256, 5280


class Buf:
    __slots__ = ("w", "rs")

    def __init__(self):
        self.w = None
        self.rs = {}


class _Eng:
    def __init__(self, name, obj):
        self.name = name
        self.obj = obj
        self.sem = None
        self.cnt = 0
        self.pending = False
        self.known = {}


class _DmaSem:
    def __init__(self, h):
        self.h = h
        self.val = 0


class Sched:
    EPOCH = 12000

    def __init__(self, nc, es):
        self.nc = nc
        self.es = es
        self.E = {
            "pe": _Eng("pe", nc.tensor),
            "act": _Eng("act", nc.scalar),
            "dve": _Eng("dve", nc.vector),
            "pool": _Eng("pool", nc.gpsimd),
            "sp": _Eng("sp", nc.sync),
        }
        self.nsem = 0
        for e in self.E.values():
            e.sem = self._new_sem(e.name)
            e.mysems = [e.sem]
        self.dsems = []
        self.allsems = {}

    def _new_sem(self, name):
        self.nsem += 1
        return self.es.enter_context(self.nc.semaphore(f"{name}_{self.nsem}"))

    def dma_sem(self, name):
        d = _DmaSem(self._new_sem(name))
        self.dsems.append(d)
        return d

    def _wait(self, eng, ev):
        sem, val = ev
        k = id(sem)
        if eng.known.get(k, 0) >= val:
            return
        if eng.name == "pe" and any(sem is s for s in eng.mysems):
            return
        eng.obj.wait_ge(sem, val)
        eng.known[k] = val

    def _deps(self, eng, reads, writes):
        for b in reads:
            if b.w is not None:
                self._wait(eng, b.w)
        for b in writes:
            if b.w is not None:
                self._wait(eng, b.w)
            for ev in b.rs.values():
                self._wait(eng, ev)

    def _record(self, ev, reads, writes):
        k = id(ev[0])
        for b in reads:
            old = b.rs.get(k)
            if old is None or old[1] < ev[1]:
                b.rs[k] = ev
        for b in writes:
            b.w = ev
            b.rs = {}

    def op(self, engname, fn, reads=(), writes=(), inc=True):
        eng = self.E[engname]
        self._deps(eng, reads, writes)
        if eng.cnt >= self.EPOCH and not eng.pending:
            eng.sem = self._new_sem(eng.name)
            eng.mysems.append(eng.sem)
            eng.cnt = 0
        ins = fn(eng.obj)
        if inc:
            ins.then_inc(eng.sem, 1)
            eng.cnt += 1
            eng.pending = False
            ev = (eng.sem, eng.cnt)
        else:
            eng.pending = True
            ev = (eng.sem, eng.cnt + 1)
        self._record(ev, reads, writes)
        return ins

    def dma(self, qname, fn, dsem, reads=(), writes=(), no_waw=False):
        q = self.E[qname]
        if no_waw:
            for b in reads:
                if b.w is not None:
                    self._wait(q, b.w)
            for b in writes:
                if b.w is not None and b.w[0] is not dsem.h:
                    self._wait(q, b.w)
                for ev in b.rs.values():
                    self._wait(q, ev)
        else:
            self._deps(q, reads, writes)
        ins = fn(q.obj)
        ins.then_inc(dsem.h, 16)
        dsem.val += 16
        ev = (dsem.h, dsem.val)
        self._record(ev, reads, writes)
        return ins

    def barrier(self):
        evs = []
        for e in self.E.values():
            assert not e.pending
            if e.cnt > 0:
                evs.append((e.sem, e.cnt))
        for d in self.dsems:
            if d.val > 0:
                evs.append((d.h, d.val))
        for e in self.E.values():
            for ev in evs:
                if ev[0] is e.sem and e.name == "pe":
                    continue
                self._wait(e, ev)


class Tl:
    def __init__(self, t):
        self.t = t
        self.bufs = {}

    def b(self, key=0):
        r = self.bufs.get(key)
        if r is None:
            r = self.bufs[key] = Buf()
        return r

    def bs(self, keys):
        return [self.b(k) for k in keys]


def build_program(dbg=None):
    dbg = dbg or {}
    nc = bass.Bass("TRN2", target_bir_lowering=False)

    def din(name, shape, dt=F32):
        return nc.dram_tensor(name, list(shape), dt, kind="ExternalInput").ap()

    xT_d = din("xT", [8, 128, T])
    cT_d = din("cT", [128, 8])
    pos_d = din("pos", [1, T], I32)
    cst_d = din("cst", [128, NCST])
    vec_d = din("vec", [128, L * NVL])
    b2_d = din("b2", [L, NE, D])
    adaw_d = din("adaw", [L, 6, 128, 8, 1024])
    whg_d = din("whg", [L, 4, 128, 8, 512])
    wmla_d = din("wmla", [L, 128, 8, 832])
    wpl_d = din("wpl", [L, 128, 8, 512])
    wgate_d = din("wgate", [L, 3, 128, 8, 1024])
    wuq_d = din("wuq", [L, 128, 3, 1536])
    wukv_d = din("wukv", [L, 128, 2, 1024])
    wpool_d = din("wpool", [L, 128, 4, 128])
    wbr_d = din("wbr", [L, 3, 128, 4, 1024])
    wout_d = din("wout", [L, 128, 8, 1024])
    wr_d = din("wr", [L, 128, 8, 32])
    w1_d = din("w1", [L, NE, 8, 128, 8, 256])
    w2f_d = din("w2f", [L, NE, 128, 8, 1024])
    b1r_d = din("b1r", [L * NE * 128, 16])
    xp_d = nc.dram_tensor("xp_scratch", [NROW, D], BF16, kind="Internal").ap()
    yp_d = nc.dram_tensor("yp_scratch", [NROW, D], F32, kind="Internal").ap()
    yT_d = nc.dram_tensor("yT", [8, 128, T], F32, kind="ExternalOutput").ap()
    dbg_out = {}
    for name, shape in dbg.items():
        dbg_out[name] = nc.dram_tensor("dbg_" + name, list(shape), F32, kind="ExternalOutput").ap()

    with ExitStack() as es:
        S = Sched(nc, es)

        uid = [0]

        def sb(stack, name, shape, dt):
            uid[0] += 1
            return Tl(stack.enter_context(nc.sbuf_tensor(f"s{uid[0]}_{name}", list(shape), dt)))

        PS = [Tl(es.enter_context(nc.psum_tensor(f"ps{i}", [128, 512], F32))) for i in range(7)]
        PSBF = Tl(es.enter_context(nc.psum_tensor("psbf", [128, 1024], BF16)))
        rot = [0]

        def psr():
            r = PS[rot[0] % 5]
            rot[0] += 1
            return r

        ACC0, ACC1 = PS[5], PS[6]

        xT = sb(es, "xT", [128, 8, T], F32)
        hT = sb(es, "hT", [128, 8, T], BF16)
        cosT = sb(es, "cosT", [128, T], BF16)
        sinS = sb(es, "sinS", [128, T], BF16)
        identb = sb(es, "identb", [128, 128], BF16)
        identf = sb(es, "identf", [128, 128], F32)
        onesb = sb(es, "onesb", [128, 128], BF16)
        onesf = sb(es, "onesf", [128, 128], F32)
        trib = sb(es, "trib", [128, 128], BF16)
        cstf = sb(es, "cstf", [128, 67], F32)
        vec = sb(es, "vec", [128, L * NVL], F32)
        modT = sb(es, "modT", [128, 48], F32)
        smallv = sb(es, "smallv", [128, 64], F32)
        condT = sb(es, "condT", [128, 8], BF16)

        d_ld = S.dma_sem("d_ld")
        d_w = [S.dma_sem(f"d_w{i}") for i in range(4)]
        d_st = S.dma_sem("d_st")
        d_dbg = S.dma_sem("d_dbg")

        def dump(name, tl_bufs, ap):
            if name in dbg_out:
                S.dma("pool", lambda q: q.dma_start(out=dbg_out[name], in_=ap), d_dbg, reads=tl_bufs)

        XB = lambda k, tb: xT.b((k, tb))
        HB = lambda k, tb: hT.b((k, tb))

        for k in range(8):
            S.dma("sp", lambda q: q.dma_start(out=xT.t[:, k, :], in_=xT_d[k]), d_ld,
                  writes=[XB(k, tb) for tb in range(NTB)])
        S.dma("sp", lambda q: q.dma_start(out=vec.t[:], in_=vec_d), d_ld, writes=[vec.b()])
        S.dma("sp", lambda q: q.dma_start(out=identf.t[:], in_=cst_d[:, C_ID:C_ID + 128]), d_ld, writes=[identf.b()])
        S.dma("sp", lambda q: q.dma_start(out=cstf.t[:], in_=cst_d[:, C_INVC:C_INVC + 67]), d_ld, writes=[cstf.b()])
        S.dma("pool", lambda q: q.dma_start(out=identb.t[:], in_=cst_d[:, C_ID:C_ID + 128]), d_ld, writes=[identb.b()])
        S.dma("pool", lambda q: q.dma_start(out=trib.t[:], in_=cst_d[:, C_TRI:C_TRI + 128]), d_ld, writes=[trib.b()])
        S.op("dve", lambda e: e.memset(onesb.t[:], 1.0), writes=[onesb.b()])
        S.op("dve", lambda e: e.memset(onesf.t[:], 1.0), writes=[onesf.b()])

        for b_ in [XB(k, tb) for k in range(8) for tb in range(NTB)] + [vec.b(), identf.b(), cstf.b(), identb.b(), trib.b()]:
            b_.w = (d_ld.h, d_ld.val)
        with ExitStack() as ph:
            cf = sb(ph, "cf", [128, 8], F32)
            S.dma("sp", lambda q: q.dma_start(out=cf.t[:], in_=cT_d), d_w[2], writes=[cf.b()])
            S.op("act", lambda e: e.activation(out=condT.t[:], in_=cf.t[:], func=AF.Silu), reads=[cf.b()], writes=[condT.b()])
            posi = sb(ph, "posi", [128, T], I32)
            u = sb(ph, "u", [128, T], F32)
            ki = sb(ph, "ki", [128, T], I32)
            kf = sb(ph, "kf", [128, T], F32)
            gt = sb(ph, "gt", [128, T], F32)
            R = slice(64, 96)
            S.dma("sp", lambda q: q.dma_start(out=posi.t[R, :], in_=pos_d[0:1, :].partition_broadcast(32)), d_w[3], writes=[posi.b()])
            S.op("dve", lambda e: e.tensor_copy(out=u.t[R, :], in_=posi.t[R, :]), reads=[posi.b()], writes=[u.b()])
            S.op("dve", lambda e: e.tensor_scalar(out=u.t[R, :], in0=u.t[R, :], scalar1=cstf.t[R, 16:17], scalar2=None, op0=ALU.mult),
                 reads=[u.b(), cstf.b()], writes=[u.b()])
            for shift, dst in ((0.0, sinS), (0.25, cosT)):
                if shift != 0.0:
                    S.op("dve", lambda e: e.tensor_scalar(out=u.t[R, :], in0=u.t[R, :], scalar1=shift, scalar2=None, op0=ALU.add),
                         reads=[u.b()], writes=[u.b()])
                S.op("dve", lambda e: e.tensor_copy(out=ki.t[R, :], in_=u.t[R, :]), reads=[u.b()], writes=[ki.b()])
                S.op("dve", lambda e: e.tensor_copy(out=kf.t[R, :], in_=ki.t[R, :]), reads=[ki.b()], writes=[kf.b()])
                S.op("dve", lambda e: e.tensor_tensor(out=kf.t[R, :], in0=u.t[R, :], in1=kf.t[R, :], op=ALU.subtract),
                     reads=[u.b(), kf.b()], writes=[kf.b()])
                S.op("dve", lambda e: e.tensor_single_scalar(out=gt.t[R, :], in_=kf.t[R, :], scalar=0.5, op=ALU.is_gt),
                     reads=[kf.b()], writes=[gt.b()])
                S.op("dve", lambda e: e.tensor_tensor(out=kf.t[R, :], in0=kf.t[R, :], in1=gt.t[R, :], op=ALU.subtract),
                     reads=[kf.b(), gt.b()], writes=[kf.b()])
                if dst is sinS:
                    S.op("act", lambda e: e.activation(out=dst.t[R, :], in_=kf.t[R, :], func=AF.Sin, scale=cstf.t[R, 17:18]),
                         reads=[kf.b(), cstf.b()], writes=[dst.b()])
                else:
                    S.op("act", lambda e: e.activation(out=dst.t[R, :], in_=kf.t[R, :], func=AF.Sin, scale=2 * math.pi * (1 - 1e-6)),
                         reads=[kf.b()], writes=[dst.b()])
            S.barrier()
        dump("cos", [cosT.b()], cosT.t[:, :])
        dump("sin", [sinS.b()], sinS.t[:, :])

        def rmsnorm_mod(l, which, want_f32=None):
            vb = l * NVL
            gcol = vb + (V_N1G if which == 0 else V_N2G)
            shc = 0 if which == 0 else 24
            scc = shc + 8
            with ExitStack() as ph:
                gs = sb(ph, "gs", [128, 8], F32)
                sq = [sb(ph, f"sq{i}", [128, 512], BF16) for i in range(2)]
                rst = sb(ph, "rst", [128, T], F32)
                tmp = [sb(ph, f"ntmp{i}", [128, 512], F32) for i in range(2)]
                S.op("dve", lambda e: e.scalar_tensor_tensor(out=gs.t[:], in0=modT.t[:, scc:scc + 8], scalar=1.0, in1=vec.t[:, gcol:gcol + 8],
                                                              op0=ALU.add, op1=ALU.mult), reads=[modT.b(), vec.b()], writes=[gs.b()])
                n = 0
                for tb in range(NTB):
                    ts = slice(tb * 512, (tb + 1) * 512)
                    ps = psr()
                    for k in range(8):
                        q = sq[n % 2]
                        n += 1
                        S.op("act", lambda e: e.activation(out=q.t[:], in_=xT.t[:, k, ts], func=AF.Square), reads=[XB(k, tb)], writes=[q.b()])
                        S.op("pe", lambda e: e.matmul(ps.t[:], onesb.t[:], q.t[:], start=(k == 0), stop=(k == 7)),
                             reads=[onesb.b(), q.b()], writes=[ps.b()], inc=True)
                    S.op("act", lambda e: e.activation(out=rst.t[:, ts], in_=ps.t[:], func=AF.Ln, scale=1.0 / D, bias=EPS), reads=[ps.b()], writes=[rst.b(tb)])
                    S.op("act", lambda e: e.activation(out=rst.t[:, ts], in_=rst.t[:, ts], func=AF.Exp, scale=-0.5), reads=[rst.b(tb)], writes=[rst.b(tb)])
                    for k in range(8):
                        tm = tmp[k % 2]
                        S.op("dve", lambda e: e.scalar_tensor_tensor(out=tm.t[:], in0=xT.t[:, k, ts], scalar=gs.t[:, k:k + 1], in1=rst.t[:, ts],
                                                                      op0=ALU.mult, op1=ALU.mult),
                             reads=[XB(k, tb), gs.b(), rst.b(tb)], writes=[tm.b()])
                        S.op("act", lambda e: e.activation(out=hT.t[:, k, ts], in_=tm.t[:], func=AF.Identity, bias=modT.t[:, shc + k:shc + k + 1]),
                             reads=[tm.b(), modT.b()], writes=[HB(k, tb)])
                        if want_f32 is not None:
                            want_f32(k, tb, tm, modT.t[:, shc + k:shc + k + 1])
                    if want_f32 is not None:
                        want_f32(None, tb, None, None)
                S.barrier()

        def adaln(l):
            vb = l * NVL
            with ExitStack() as ph:
                wb = [sb(ph, f"adaw{i}", [128, 8, 1024], BF16) for i in range(2)]
                ps = psr()
                for ty in range(6):
                    w = wb[ty % 2]
                    S.dma("pool", lambda q: q.dma_start(out=w.t[:], in_=adaw_d[l, ty], max_dma_last_dim=8192), d_w[ty % 2], writes=[w.b()])
                    for o in range(8):
                        col = ty * 8 + o
                        for k in range(8):
                            S.op("pe", lambda e: e.matmul(ps.t[:, col:col + 1], w.t[:, k, o * 128:(o + 1) * 128], condT.t[:, k:k + 1],
                                                         start=(k == 0), stop=(k == 7)),
                                 reads=[w.b(), condT.b()], writes=[ps.b()], inc=(k == 7))
                S.op("dve", lambda e: e.tensor_tensor(out=modT.t[:], in0=ps.t[:, 0:48], in1=vec.t[:, vb + V_ADAB:vb + V_ADAB + 48], op=ALU.add),
                     reads=[ps.b(), vec.b()], writes=[modT.b()])
                S.barrier()

        def epilogue(l, br, srcT, src_bufs, nk, kp):
            with ExitStack() as ph:
                wbr = sb(ph, "wbr", [128, 4, 1024], BF16)
                wg = sb(ph, "wg", [128, 8, 1024], BF16)
                wo = sb(ph, "wo", [128, 8, 1024], BF16)
                mT = sb(ph, "mT", [128, 8, 512], BF16)
                sg = [sb(ph, f"sg{i}", [128, 512], F32) for i in range(2)]
                S.dma("pool", lambda q: q.dma_start(out=wbr.t[:], in_=wbr_d[l, br], max_dma_last_dim=8192), d_w[0], writes=[wbr.b()])
                S.dma("pool", lambda q: q.dma_start(out=wg.t[:], in_=wgate_d[l, br], max_dma_last_dim=8192), d_w[1], writes=[wg.b()])
                S.dma("pool", lambda q: q.dma_start(out=wo.t[:], in_=wout_d[l], max_dma_last_dim=8192), d_w[2], writes=[wo.b()])
                n = 0
                for tb in range(NTB):
                    ts = slice(tb * 512, (tb + 1) * 512)
                    for o in range(8):
                        oc = slice(o * 128, (o + 1) * 128)
                        py = psr()
                        for kc in range(nk):
                            S.op("pe", lambda e: e.matmul(py.t[:], wbr.t[0:kp, kc, oc], srcT(kc, ts), start=(kc == 0), stop=(kc == nk - 1)),
                                 reads=[wbr.b()] + src_bufs(kc, tb), writes=[py.b()], inc=(kc == nk - 1))
                        pg = psr()
                        for k in range(8):
                            S.op("pe", lambda e: e.matmul(pg.t[:], wg.t[:, k, oc], hT.t[:, k, ts], start=(k == 0), stop=(k == 7)),
                                 reads=[wg.b(), HB(k, tb)], writes=[pg.b()], inc=(k == 7))
                        s = sg[n % 2]
                        n += 1
                        S.op("act", lambda e: e.activation(out=s.t[:], in_=pg.t[:], func=AF.Sigmoid), reads=[pg.b()], writes=[s.b()])
                        S.op("dve", lambda e: e.tensor_tensor(out=mT.t[:, o, :], in0=py.t[:], in1=s.t[:], op=ALU.mult),
                             reads=[py.b(), s.b()], writes=[mT.b(o)])
                    for o2 in range(8):
                        px = psr()
                        for o in range(8):
                            S.op("pe", lambda e: e.matmul(px.t[:], wo.t[:, o, o2 * 128:(o2 + 1) * 128], mT.t[:, o, :], start=(o == 0), stop=(o == 7)),
                                 reads=[wo.b(), mT.b(o)], writes=[px.b()], inc=(o == 7))
                        S.op("dve", lambda e: e.scalar_tensor_tensor(out=xT.t[:, o2, ts], in0=px.t[:], scalar=modT.t[:, 16 + o2:17 + o2], in1=xT.t[:, o2, ts],
                                                                      op0=ALU.mult, op1=ALU.add),
                             reads=[px.b(), modT.b(), XB(o2, tb)], writes=[XB(o2, tb)])
                S.barrier()

        def hgrn(l):
            vb = l * NVL
            with ExitStack() as outer:
                AT = sb(outer, "AT", [128, 4, T], BF16)
                with ExitStack() as ph:
                    W = sb(ph, "whg", [128, 8, 512], BF16)
                    rmask = sb(ph, "rmask", [128, T], BF16)
                    S.dma("pool", lambda q: q.dma_start(out=rmask.t[:], in_=cst_d[:, C_RMASK:C_RMASK + T]), d_w[1], writes=[rmask.b()])
                    T1 = sb(ph, "T1", [128, T], F32)
                    T2 = sb(ph, "T2", [128, T], F32)
                    T3 = sb(ph, "T3", [128, T], F32)
                    T4 = sb(ph, "T4", [128, T], F32)
                    QpT = sb(ph, "QpT", [128, T], BF16)
                    KpT = sb(ph, "KpT", [128, T], BF16)
                    Kptok = sb(ph, "Kptok", [64, NCH, 128], BF16)
                    Vh = sb(ph, "Vh", [64, NCH, 128], BF16)
                    SGt = sb(ph, "SGt", [128, T], BF16)
                    S32 = sb(ph, "S32", [128, 128], F32)
                    Stmp = sb(ph, "Stmp", [128, 128], F32)
                    Sbf = [sb(ph, f"Sbf{i}", [128, 128], BF16) for i in range(2)]
                    ATT = sb(ph, "ATT", [64, NCH, 64], BF16)
                    S32b = [S32, Stmp]
                    Pb = [sb(ph, f"Pb{i}", [128, 128], F32) for i in range(2)]
                    beta = sb(ph, "beta", [128, 32], F32)
                    mv = sb(ph, "mv", [128, 32], F32)
                    emv = sb(ph, "emv", [128, 32], F32)
                    trii = sb(ph, "trii", [64, 64], I32)
                    S.op("dve", lambda e: e.tensor_copy(out=trii.t[:], in_=trib.t[0:64, 0:64]), reads=[trib.b()], writes=[trii.b()])
                    S.op("dve", lambda e: e.memset(ATT.t[:], 0.0), writes=[ATT.b()])
                    lbv = sb(ph, "lbv", [128, 8], F32)
                    if l == 1:
                        S.op("dve", lambda e: e.tensor_tensor(out=lbv.t[:, 0:4], in0=vec.t[:, NVL + V_LB:NVL + V_LB + 4], in1=vec.t[:, V_LB:V_LB + 4],
                                                               op=ALU.subtract), reads=[vec.b()], writes=[lbv.b()])
                        S.op("act", lambda e: e.activation(out=lbv.t[:, 0:4], in_=lbv.t[:, 0:4], func=AF.Sigmoid), reads=[lbv.b()], writes=[lbv.b()])
                        S.op("dve", lambda e: e.tensor_scalar(out=lbv.t[:, 4:8], in0=lbv.t[:, 0:4], scalar1=-1.0, scalar2=1.0, op0=ALU.mult, op1=ALU.add),
                             reads=[lbv.b()], writes=[lbv.b()])
                    for hd in range(4):
                        S.dma("pool", lambda q: q.dma_start(out=W.t[:], in_=whg_d[l, hd], max_dma_last_dim=8192), d_w[0], writes=[W.b()])
                        cq, cf_, ci, cg = slice(0, 128), slice(128, 256), slice(256, 384), slice(384, 512)
                        for tb in range(NTB):
                            ts = slice(tb * 512, (tb + 1) * 512)
                            ps = psr()
                            for k in range(8):
                                S.op("pe", lambda e: e.matmul(ps.t[:], W.t[:, k, cf_], hT.t[:, k, ts], start=(k == 0), stop=(k == 7)),
                                     reads=[W.b(), HB(k, tb)], writes=[ps.b()], inc=(k == 7))
                            S.op("act", lambda e: e.activation(out=T1.t[:, ts], in_=ps.t[:], func=AF.Sigmoid), reads=[ps.b()], writes=[T1.b(tb)])
                        allT = lambda t_: [t_.b(tb) for tb in range(NTB)]
                        if l == 1:
                            S.op("dve", lambda e: e.tensor_scalar(out=T1.t[:], in0=T1.t[:], scalar1=lbv.t[:, 4 + hd:5 + hd], scalar2=lbv.t[:, hd:hd + 1],
                                                                   op0=ALU.mult, op1=ALU.add), reads=allT(T1) + [lbv.b()], writes=allT(T1))
                        S.op("act", lambda e: e.activation(out=T2.t[:], in_=T1.t[:], func=AF.Ln), reads=allT(T1), writes=allT(T2))
                        S.op("dve", lambda e: e.tensor_tensor_scan(out=T3.t[:], data0=rmask.t[:], data1=T2.t[:], initial=0.0, op0=ALU.mult, op1=ALU.add),
                             reads=allT(T2) + [rmask.b()], writes=allT(T3))
                        S.op("dve", lambda e: e.tensor_copy(out=mv.t[:], in_=T3.t[:, 31::64]), reads=allT(T3), writes=[mv.b()])
                        S.op("dve", lambda e: e.tensor_tensor(out=T3.t[:, :].rearrange("p (c j) -> p c j", j=C), in0=T3.t[:, :].rearrange("p (c j) -> p c j", j=C),
                                                               in1=mv.t[:, :].rearrange("p (c o) -> p c o", o=1).broadcast_to([128, NCH, C]), op=ALU.subtract),
                             reads=allT(T3) + [mv.b()], writes=allT(T3))
                        S.op("act", lambda e: e.activation(out=emv.t[:], in_=mv.t[:], func=AF.Exp), reads=[mv.b()], writes=[emv.b()])
                        S.op("act", lambda e: e.activation(out=T4.t[:], in_=T3.t[:], func=AF.Exp), reads=allT(T3), writes=allT(T4))
                        S.op("act", lambda e: e.activation(out=T2.t[:], in_=T3.t[:], func=AF.Exp, scale=-1.0), reads=allT(T3), writes=allT(T2))
                        S.op("dve", lambda e: e.scalar_tensor_tensor(out=KpT.t[:], in0=T1.t[:], scalar=1.0, in1=T2.t[:], op0=ALU.subtract, op1=ALU.mult),
                             reads=allT(T1) + allT(T2), writes=allT(KpT))
                        for tb in range(NTB):
                            ts = slice(tb * 512, (tb + 1) * 512)
                            ps = psr()
                            for k in range(8):
                                S.op("pe", lambda e: e.matmul(ps.t[:], W.t[:, k, cq], hT.t[:, k, ts], start=(k == 0), stop=(k == 7)),
                                     reads=[W.b(), HB(k, tb)], writes=[ps.b()], inc=(k == 7))
                            S.op("dve", lambda e: e.scalar_tensor_tensor(out=QpT.t[:, ts], in0=ps.t[:], scalar=-1.0, in1=T4.t[:, ts], op0=ALU.mult, op1=ALU.mult),
                                 reads=[ps.b(), T4.b(tb)], writes=[QpT.b(tb)])
                        for tb in range(NTB):
                            ts = slice(tb * 512, (tb + 1) * 512)
                            ps = psr()
                            for k in range(8):
                                S.op("pe", lambda e: e.matmul(ps.t[:], W.t[:, k, cg], hT.t[:, k, ts], start=(k == 0), stop=(k == 7)),
                                     reads=[W.b(), HB(k, tb)], writes=[ps.b()], inc=(k == 7))
                            S.op("act", lambda e: e.activation(out=SGt.t[:, ts], in_=ps.t[:], func=AF.Silu), reads=[ps.b()], writes=[SGt.b(tb)])
                        for c4 in range(NCH // 4):
                            for j in range(4):
                                c = c4 * 4 + j
                                S.op("pe", lambda e: e.transpose(PSBF.t[0:64, j * 128:(j + 1) * 128], KpT.t[:, c * C:(c + 1) * C], identb.t[:]),
                                     reads=[KpT.b(c * C // 512), identb.b()], writes=[PSBF.b()], inc=(j == 3))
                            S.op("act", lambda e: e.copy(out=Kptok.t[:, c4 * 4:c4 * 4 + 4, :], in_=PSBF.t[0:64, 0:512].rearrange("p (a b) -> p a b", a=4)),
                                 reads=[PSBF.b()], writes=[Kptok.b(c4)])
                        for c4 in range(NCH // 4):
                            ps = psr()
                            for j in range(4):
                                c = c4 * 4 + j
                                for k in range(8):
                                    S.op("pe", lambda e: e.matmul(ps.t[0:64, j * 128:(j + 1) * 128], hT.t[:, k, c * C:(c + 1) * C], W.t[:, k, ci],
                                                                 start=(k == 0), stop=(k == 7)),
                                         reads=[W.b(), HB(k, c * C // 512)], writes=[ps.b()], inc=(k == 7 and j == 3))
                            S.op("dve", lambda e: e.tensor_copy(out=Vh.t[:, c4 * 4:c4 * 4 + 4, :], in_=ps.t[0:64, :].rearrange("p (a b) -> p a b", a=4)),
                                 reads=[ps.b()], writes=[Vh.b(c4)])
                        OT = T1
                        for c in range(NCH):
                            cs = slice(c * C, (c + 1) * C)
                            tb = c * C // 512
                            pa = psr()
                            S.op("pe", lambda e: e.matmul(pa.t[0:64, 0:64], KpT.t[:, cs], QpT.t[:, cs], start=True, stop=True),
                                 reads=[KpT.b(tb), QpT.b(tb)], writes=[pa.b()])
                            S.op("dve", lambda e: e.copy_predicated(out=ATT.t[:, c, :], mask=trii.t[:], data=pa.t[0:64, 0:64]),
                                 reads=[pa.b(), trii.b()], writes=[ATT.b(c)])
                        S.op("dve", lambda e: e.tensor_tensor(out=beta.t[:, 0:NCH - 1], in0=T4.t[:, C - 1:T - C:C], in1=emv.t[:, 1:NCH], op=ALU.mult),
                             reads=allT(T4) + [emv.b()], writes=[beta.b()])
                        for c in range(NCH):
                            cs = slice(c * C, (c + 1) * C)
                            tb = c * C // 512
                            if c < NCH - 1:
                                pss = psr()
                                S.op("pe", lambda e: e.matmul(pss.t[:, 0:128], Kptok.t[:, c, :], Vh.t[:, c, :], start=True, stop=True),
                                     reads=[Kptok.b(c // 4), Vh.b(c // 4)], writes=[pss.b()])
                            po = psr()
                            S.op("pe", lambda e: e.matmul(po.t[:, 0:64], Vh.t[:, c, :], ATT.t[:, c, :], start=True, stop=(c == 0)),
                                 reads=[Vh.b(c // 4), ATT.b(c)], writes=[po.b()], inc=(c == 0))
                            if c > 0:
                                sbf = Sbf[c % 2]
                                S.op("pe", lambda e: e.matmul(po.t[:, 0:64], sbf.t[:], QpT.t[:, cs], start=False, stop=True),
                                     reads=[sbf.b(), QpT.b(tb)], writes=[po.b()])
                            S.op("act", lambda e: e.copy(out=OT.t[:, cs], in_=po.t[:, 0:64]), reads=[po.b()], writes=[OT.b(tb)])
                            if c < NCH - 1:
                                bc = beta.t[:, c:c + 1]
                                nsbf = Sbf[(c + 1) % 2]
                                s_old, s_new = S32b[c % 2], S32b[(c + 1) % 2]
                                if c == 0:
                                    S.op("dve", lambda e: e.tensor_scalar(out=nsbf.t[:], in0=pss.t[:, 0:128], scalar1=bc, scalar2=None, op0=ALU.mult),
                                         reads=[pss.b(), beta.b()], writes=[nsbf.b()])
                                    S.op("dve", lambda e: e.tensor_scalar(out=s_new.t[:], in0=pss.t[:, 0:128], scalar1=bc, scalar2=None, op0=ALU.mult),
                                         reads=[pss.b(), beta.b()], writes=[s_new.b()])
                                else:
                                    pb_ = Pb[c % 2]
                                    S.op("dve", lambda e: e.tensor_scalar(out=pb_.t[:], in0=pss.t[:, 0:128], scalar1=bc, scalar2=None, op0=ALU.mult),
                                         reads=[pss.b(), beta.b()], writes=[pb_.b()])
                                    S.op("dve", lambda e: e.scalar_tensor_tensor(out=nsbf.t[:], in0=s_old.t[:], scalar=bc, in1=pb_.t[:], op0=ALU.mult, op1=ALU.add),
                                         reads=[s_old.b(), beta.b(), pb_.b()], writes=[nsbf.b()])
                                    S.op("dve", lambda e: e.scalar_tensor_tensor(out=s_new.t[:], in0=s_old.t[:], scalar=bc, in1=pb_.t[:], op0=ALU.mult, op1=ALU.add),
                                         reads=[s_old.b(), beta.b(), pb_.b()], writes=[s_new.b()])
                        sqb = KpT
                        S.op("act", lambda e: e.activation(out=sqb.t[:], in_=OT.t[:], func=AF.Square), reads=allT(OT), writes=allT(sqb))
                        for tb in range(NTB):
                            ts = slice(tb * 512, (tb + 1) * 512)
                            ps = psr()
                            S.op("pe", lambda e: e.matmul(ps.t[:], onesb.t[:], sqb.t[:, ts], start=True, stop=True), reads=[onesb.b(), sqb.b(tb)], writes=[ps.b()])
                            S.op("act", lambda e: e.activation(out=T3.t[:, ts], in_=ps.t[:], func=AF.Ln, scale=1.0 / 128, bias=EPS), reads=[ps.b()], writes=[T3.b(tb)])
                            S.op("act", lambda e: e.activation(out=T3.t[:, ts], in_=T3.t[:, ts], func=AF.Exp, scale=-0.5), reads=[T3.b(tb)], writes=[T3.b(tb)])
                            S.op("dve", lambda e: e.scalar_tensor_tensor(out=T3.t[:, ts], in0=OT.t[:, ts], scalar=vec.t[:, vb + V_ONG:vb + V_ONG + 1], in1=T3.t[:, ts],
                                                                          op0=ALU.mult, op1=ALU.mult), reads=[OT.b(tb), vec.b(), T3.b(tb)], writes=[T3.b(tb)])
                            S.op("dve", lambda e: e.tensor_tensor(out=AT.t[:, hd, ts], in0=T3.t[:, ts], in1=SGt.t[:, ts], op=ALU.mult),
                                 reads=[T3.b(tb), SGt.b(tb)], writes=[AT.b((hd, tb))])
                    S.barrier()
                dump_bf16("oa", AT, [(hd, tb) for hd in range(4) for tb in range(NTB)], 4)
                epilogue(l, 0, lambda kc, ts: AT.t[:, kc, ts], lambda kc, tb: [AT.b((kc, tb))], 4, 128)

        def mla(l):
            vb = l * NVL
            R = slice(64, 96)
            scale = 1.0 / math.sqrt(96.0)
            with ExitStack() as outer:
                BT = sb(outer, "BT", [128, 4, T], BF16)
                with ExitStack() as ph:
                    qlatN = sb(ph, "qlatN", [128, 3, T], BF16)
                    kvN = sb(ph, "kvN", [128, 2, T], BF16)
                    KRC = sb(ph, "KRC", [128, T], F32)
                    sqkr = sb(ph, "sqkr", [128, T], BF16)
                    with ExitStack() as p1:
                        wm = sb(p1, "wmla", [128, 8, 832], BF16)
                        sq = [sb(p1, f"lsq{i}", [128, 512], BF16) for i in range(2)]
                        rs = sb(p1, "lrs", [128, 512], F32)
                        t1 = sb(p1, "lt1", [128, 512], F32)
                        t2 = sb(p1, "lt2", [128, 512], F32)
                        S.dma("pool", lambda q: q.dma_start(out=wm.t[:], in_=wmla_d[l], max_dma_last_dim=8192), d_w[0], writes=[wm.b()])
                        for tb in range(NTB):
                            ts = slice(tb * 512, (tb + 1) * 512)
                            for dst, c0, nchunk, gcol, dim in ((qlatN, 0, 3, V_QLG, 384.0), (kvN, 384, 2, V_KVG, 256.0)):
                                pls = [psr() for _ in range(nchunk)]
                                for j in range(nchunk):
                                    for k in range(8):
                                        S.op("pe", lambda e: e.matmul(pls[j].t[:], wm.t[:, k, c0 + j * 128:c0 + (j + 1) * 128], hT.t[:, k, ts], start=(k == 0), stop=(k == 7)),
                                             reads=[wm.b(), HB(k, tb)], writes=[pls[j].b()], inc=(k == 7))
                                pss = psr()
                                for j in range(nchunk):
                                    q_ = sq[j % 2]
                                    S.op("act", lambda e: e.activation(out=q_.t[:], in_=pls[j].t[:], func=AF.Square), reads=[pls[j].b()], writes=[q_.b()])
                                    S.op("pe", lambda e: e.matmul(pss.t[:], onesb.t[:], q_.t[:], start=(j == 0), stop=(j == nchunk - 1)),
                                         reads=[onesb.b(), q_.b()], writes=[pss.b()])
                                S.op("act", lambda e: e.activation(out=rs.t[:], in_=pss.t[:], func=AF.Ln, scale=1.0 / dim, bias=EPS), reads=[pss.b()], writes=[rs.b()])
                                S.op("act", lambda e: e.activation(out=rs.t[:], in_=rs.t[:], func=AF.Exp, scale=-0.5), reads=[rs.b()], writes=[rs.b()])
                                for j in range(nchunk):
                                    S.op("dve", lambda e: e.scalar_tensor_tensor(out=dst.t[:, j, ts], in0=pls[j].t[:], scalar=vec.t[:, vb + gcol + j:vb + gcol + j + 1], in1=rs.t[:],
                                                                                  op0=ALU.mult, op1=ALU.mult), reads=[pls[j].b(), vec.b(), rs.b()], writes=[dst.b((j, tb))])
                            pk = psr()
                            pks = psr()
                            for k in range(8):
                                S.op("pe", lambda e: e.matmul(pk.t[0:96, :], wm.t[:, k, 640:736], hT.t[:, k, ts], start=(k == 0), stop=(k == 7)),
                                     reads=[wm.b(), HB(k, tb)], writes=[pk.b()], inc=(k == 7))
                            for k in range(8):
                                S.op("pe", lambda e: e.matmul(pks.t[0:96, :], wm.t[:, k, 736:832], hT.t[:, k, ts], start=(k == 0), stop=(k == 7)),
                                     reads=[wm.b(), HB(k, tb)], writes=[pks.b()], inc=(k == 7))
                            S.op("act", lambda e: e.activation(out=sqkr.t[0:96, ts], in_=pk.t[0:96, :], func=AF.Square), reads=[pk.b()], writes=[sqkr.b(tb)])
                            S.op("dve", lambda e: e.scalar_tensor_tensor(out=t1.t[R, :], in0=pk.t[R, :], scalar=vec.t[R, vb + V_KNG:vb + V_KNG + 1], in1=cosT.t[R, ts],
                                                                          op0=ALU.mult, op1=ALU.mult), reads=[pk.b(), vec.b(), cosT.b()], writes=[t1.b()])
                            S.op("dve", lambda e: e.scalar_tensor_tensor(out=t2.t[R, :], in0=pks.t[R, :], scalar=vec.t[R, vb + V_KNGP:vb + V_KNGP + 1], in1=sinS.t[R, ts],
                                                                          op0=ALU.mult, op1=ALU.mult), reads=[pks.b(), vec.b(), sinS.b()], writes=[t2.b()])
                            S.op("dve", lambda e: e.tensor_tensor(out=KRC.t[R, ts], in0=t1.t[R, :], in1=t2.t[R, :], op=ALU.add), reads=[t1.b(), t2.b()], writes=[KRC.b(tb)])
                        S.barrier()
                    with ExitStack() as p2:
                        wq = sb(p2, "wuq", [128, 3, 1536], BF16)
                        wkv = sb(p2, "wukv", [128, 2, 1024], BF16)
                        Vp = sb(p2, "Vp", [128, 16, 192], BF16)
                        QT = sb(p2, "QT", [128, T], BF16)
                        KT = sb(p2, "KT", [128, T], BF16)
                        sq = sb(p2, "asq", [128, 512], BF16)
                        rs = sb(p2, "ars", [128, 512], F32)
                        t1 = sb(p2, "at1", [128, 512], F32)
                        t2 = sb(p2, "at2", [128, 512], F32)
                        Eb = [sb(p2, f"Eb{i}", [128, 512], BF16) for i in range(4)]
                        RD = [sb(p2, f"RD{i}", [128, 512], F32) for i in range(2)]
                        deferred = []
                        nrd = [0]
                        bcs = sb(p2, "bcs", [128, 512], F32)
                        S.dma("pool", lambda q: q.dma_start(out=wq.t[:], in_=wuq_d[l], max_dma_last_dim=8192), d_w[0], writes=[wq.b()])
                        S.dma("pool", lambda q: q.dma_start(out=wkv.t[:], in_=wukv_d[l], max_dma_last_dim=8192), d_w[1], writes=[wkv.b()])
                        S.op("dve", lambda e: e.memset(Vp.t[:, :, 64:128], 0.0), writes=[Vp.b()])
                        S.op("dve", lambda e: e.memset(Vp.t[:, :, 64:65], 1.0), writes=[Vp.b()])
                        ne = [0]
                        for jp in range(4):
                            for t4 in range(4):
                                ps = psr()
                                for j in range(4):
                                    tt = t4 * 4 + j
                                    for kc in range(2):
                                        S.op("pe", lambda e: e.matmul(ps.t[:, j * 128:(j + 1) * 128], kvN.t[:, kc, tt * 128:(tt + 1) * 128], wkv.t[:, kc, 512 + jp * 128:512 + (jp + 1) * 128],
                                                                     start=(kc == 0), stop=(kc == 1)),
                                             reads=[kvN.b((kc, t4)), wkv.b()], writes=[ps.b()], inc=(kc == 1 and j == 3))
                                pv = ps.t[:, :].rearrange("p (a b) -> p a b", a=4)
                                S.op("act", lambda e: e.copy(out=Vp.t[:, t4 * 4:(t4 + 1) * 4, 0:64], in_=pv[:, :, 0:64]), reads=[ps.b()], writes=[Vp.b()])
                                S.op("dve", lambda e: e.tensor_copy(out=Vp.t[:, t4 * 4:(t4 + 1) * 4, 128:192], in_=pv[:, :, 64:128]), reads=[ps.b()], writes=[Vp.b()])
                            for hh in range(2):
                                h = jp * 2 + hh
                                for tb in range(NTB):
                                    ts = slice(tb * 512, (tb + 1) * 512)
                                    pq = psr()
                                    pqs = psr()
                                    for kc in range(3):
                                        S.op("pe", lambda e: e.matmul(pq.t[0:96, :], wq.t[:, kc, h * 96:(h + 1) * 96], qlatN.t[:, kc, ts], start=(kc == 0), stop=(kc == 2)),
                                             reads=[wq.b(), qlatN.b((kc, tb))], writes=[pq.b()], inc=(kc == 2))
                                    for kc in range(3):
                                        S.op("pe", lambda e: e.matmul(pqs.t[0:96, :], wq.t[:, kc, 768 + h * 96:768 + (h + 1) * 96], qlatN.t[:, kc, ts], start=(kc == 0), stop=(kc == 2)),
                                             reads=[wq.b(), qlatN.b((kc, tb))], writes=[pqs.b()], inc=(kc == 2))
                                    S.op("act", lambda e: e.activation(out=sq.t[0:96, :], in_=pq.t[0:96, :], func=AF.Square), reads=[pq.b()], writes=[sq.b()])
                                    pss = psr()
                                    S.op("pe", lambda e: e.matmul(pss.t[0:96, :], onesb.t[0:96, 0:96], sq.t[0:96, :], start=True, stop=True), reads=[onesb.b(), sq.b()], writes=[pss.b()])
                                    S.op("act", lambda e: e.activation(out=rs.t[0:96, :], in_=pss.t[0:96, :], func=AF.Ln, scale=1.0 / 96, bias=EPS), reads=[pss.b()], writes=[rs.b()])
                                    S.op("act", lambda e: e.activation(out=rs.t[0:96, :], in_=rs.t[0:96, :], func=AF.Exp, scale=-0.5), reads=[rs.b()], writes=[rs.b()])
                                    S.op("dve", lambda e: e.scalar_tensor_tensor(out=QT.t[0:64, ts], in0=pq.t[0:64, :], scalar=vec.t[0:64, vb + V_QNG:vb + V_QNG + 1], in1=rs.t[0:64, :],
                                                                                  op0=ALU.mult, op1=ALU.mult), reads=[pq.b(), vec.b(), rs.b()], writes=[QT.b(tb)])
                                    S.op("dve", lambda e: e.scalar_tensor_tensor(out=t1.t[R, :], in0=pq.t[R, :], scalar=vec.t[R, vb + V_QNG:vb + V_QNG + 1], in1=cosT.t[R, ts],
                                                                                  op0=ALU.mult, op1=ALU.mult), reads=[pq.b(), vec.b(), cosT.b()], writes=[t1.b()])
                                    S.op("dve", lambda e: e.scalar_tensor_tensor(out=t2.t[R, :], in0=pqs.t[R, :], scalar=vec.t[R, vb + V_QNGP:vb + V_QNGP + 1], in1=sinS.t[R, ts],
                                                                                  op0=ALU.mult, op1=ALU.mult), reads=[pqs.b(), vec.b(), sinS.b()], writes=[t2.b()])
                                    S.op("dve", lambda e: e.tensor_tensor(out=t1.t[R, :], in0=t1.t[R, :], in1=t2.t[R, :], op=ALU.add), reads=[t1.b(), t2.b()], writes=[t1.b()])
                                    S.op("dve", lambda e: e.tensor_tensor(out=QT.t[R, ts], in0=t1.t[R, :], in1=rs.t[R, :], op=ALU.mult), reads=[t1.b(), rs.b()], writes=[QT.b(tb)])
                                    pk = psr()
                                    for kc in range(2):
                                        S.op("pe", lambda e: e.matmul(pk.t[0:64, :], wkv.t[:, kc, h * 64:(h + 1) * 64], kvN.t[:, kc, ts], start=(kc == 0), stop=(kc == 1)),
                                             reads=[wkv.b(), kvN.b((kc, tb))], writes=[pk.b()], inc=(kc == 1))
                                    S.op("act", lambda e: e.activation(out=sq.t[0:64, :], in_=pk.t[0:64, :], func=AF.Square), reads=[pk.b()], writes=[sq.b()])
                                    pss = psr()
                                    S.op("pe", lambda e: e.matmul(pss.t[0:96, :], onesb.t[0:64, 0:96], sq.t[0:64, :], start=True, stop=False), reads=[onesb.b(), sq.b()], writes=[pss.b()], inc=False)
                                    S.op("pe", lambda e: e.matmul(pss.t[0:96, :], onesb.t[0:96, 0:96], sqkr.t[0:96, ts], start=False, stop=True), reads=[onesb.b(), sqkr.b(tb)], writes=[pss.b()])
                                    S.op("act", lambda e: e.activation(out=rs.t[0:96, :], in_=pss.t[0:96, :], func=AF.Ln, scale=1.0 / 96, bias=EPS), reads=[pss.b()], writes=[rs.b()])
                                    S.op("act", lambda e: e.activation(out=rs.t[0:96, :], in_=rs.t[0:96, :], func=AF.Exp, scale=-0.5), reads=[rs.b()], writes=[rs.b()])
                                    S.op("dve", lambda e: e.scalar_tensor_tensor(out=KT.t[0:64, ts], in0=pk.t[0:64, :], scalar=vec.t[0:64, vb + V_KNG:vb + V_KNG + 1], in1=rs.t[0:64, :],
                                                                                  op0=ALU.mult, op1=ALU.mult), reads=[pk.b(), vec.b(), rs.b()], writes=[KT.b(tb)])
                                    S.op("dve", lambda e: e.tensor_tensor(out=KT.t[R, ts], in0=KRC.t[R, ts], in1=rs.t[R, :], op=ALU.mult), reads=[KRC.b(tb), rs.b()], writes=[KT.b(tb)])
                                for qb in range(NTB):
                                    po = ACC0 if (h * 4 + qb) % 2 == 0 else ACC1
                                    nkt = 4 * qb + 4
                                    ebs = {}

                                    def s_exp(kt):
                                        d = kt - 4 * qb
                                        c0 = max(d, 0) * 128
                                        ps = psr()
                                        S.op("pe", lambda e: e.matmul(ps.t[:, c0:512], KT.t[0:96, kt * 128:(kt + 1) * 128], QT.t[0:96, qb * 512 + c0:(qb + 1) * 512], start=True, stop=True),
                                             reads=[KT.b(kt // 4), QT.b(qb)], writes=[ps.b()])
                                        eb = Eb[ne[0] % len(Eb)]
                                        ne[0] += 1
                                        ebs[kt] = eb
                                        S.op("act", lambda e: e.activation(out=eb.t[:, c0:512], in_=ps.t[:, c0:512], func=AF.Exp, scale=scale), reads=[ps.b()], writes=[eb.b()])
                                        if d >= 0:
                                            S.op("dve", lambda e: e.tensor_tensor(out=eb.t[:, c0:c0 + 128], in0=eb.t[:, c0:c0 + 128], in1=trib.t[:], op=ALU.mult),
                                                 reads=[eb.b(), trib.b()], writes=[eb.b()])

                                    def pv(kt):
                                        d = kt - 4 * qb
                                        c0 = max(d, 0) * 128
                                        eb = ebs[kt]
                                        if hh == 0:
                                            S.op("pe", lambda e: e.matmul(po.t[0:65, c0:512], Vp.t[:, kt, 0:65], eb.t[:, c0:512], start=(kt == 0), stop=(kt == nkt - 1)),
                                                 reads=[Vp.b(), eb.b()], writes=[po.b()], inc=(kt == nkt - 1))
                                        else:
                                            S.op("pe", lambda e: e.matmul(po.t[:, c0:512], Vp.t[:, kt, 64:192], eb.t[:, c0:512], start=(kt == 0), stop=(kt == nkt - 1)),
                                                 reads=[Vp.b(), eb.b()], writes=[po.b()], inc=(kt == nkt - 1))

                                    SK = 2
                                    for i in range(nkt + SK):
                                        if i < nkt:
                                            s_exp(i)
                                        if i == SK and deferred:
                                            for f_ in deferred:
                                                f_()
                                            deferred.clear()
                                        if i - SK >= 0:
                                            pv(i - SK)
                                    pden = 64 if hh == 0 else 0
                                    orow = slice(0, 64) if hh == 0 else slice(64, 128)
                                    qs = slice(qb * 512, (qb + 1) * 512)
                                    rd = RD[nrd[0] % 2]
                                    nrd[0] += 1
                                    S.op("dve", lambda e: e.reciprocal(out=rd.t[pden:pden + 1, :], in_=po.t[pden:pden + 1, :]), reads=[po.b()], writes=[rd.b()])

                                    def norm2(po=po, pden=pden, orow=orow, qs=qs, rd=rd, jp=jp, qb=qb):
                                        pbc = psr()
                                        S.op("pe", lambda e: e.matmul(pbc.t[orow, :], onesf.t[pden:pden + 1, 0:64], rd.t[pden:pden + 1, :], start=True, stop=True),
                                             reads=[onesf.b(), rd.b()], writes=[pbc.b()])
                                        S.op("act", lambda e: e.copy(out=bcs.t[orow, :], in_=pbc.t[orow, :]), reads=[pbc.b()], writes=[bcs.b()])
                                        S.op("dve", lambda e: e.tensor_tensor(out=BT.t[orow, jp, qs], in0=po.t[orow, :], in1=bcs.t[orow, :], op=ALU.mult),
                                             reads=[po.b(), bcs.b()], writes=[BT.b((jp, qb))])

                                    deferred.append(norm2)
                        for f_ in deferred:
                            f_()
                        deferred.clear()
                        S.barrier()
                dump_bf16("ob", BT, [(jp, tb) for jp in range(4) for tb in range(NTB)], 4)
                epilogue(l, 1, lambda kc, ts: BT.t[:, kc, ts], lambda kc, tb: [BT.b((kc, tb))], 4, 128)

        def poolmix(l):
            vb = l * NVL
            with ExitStack() as outer:
                CT = sb(outer, "CT", [128, 4, T], BF16)
                with ExitStack() as ph:
                    wp = sb(ph, "wpl", [128, 8, 512], BF16)
                    wpo = sb(ph, "wpool", [128, 4, 128], BF16)
                    U = sb(ph, "U", [128, T], F32)
                    A = sb(ph, "A", [128, T], F32)
                    B = sb(ph, "B", [128, T], F32)
                    DT = sb(ph, "DT", [128, T], BF16)
                    tfix = sb(ph, "tfix", [128, 16], F32)
                    S.dma("pool", lambda q: q.dma_start(out=wp.t[:], in_=wpl_d[l], max_dma_last_dim=8192), d_w[0], writes=[wp.b()])
                    S.dma("pool", lambda q: q.dma_start(out=wpo.t[:], in_=wpool_d[l], max_dma_last_dim=8192), d_w[1], writes=[wpo.b()])
                    for g in range(4):
                        w = 2 << g
                        for tb in range(NTB):
                            ts = slice(tb * 512, (tb + 1) * 512)
                            ps = psr()
                            for k in range(8):
                                S.op("pe", lambda e: e.matmul(ps.t[:], wp.t[:, k, g * 128:(g + 1) * 128], hT.t[:, k, ts], start=(k == 0), stop=(k == 7)),
                                     reads=[wp.b(), HB(k, tb)], writes=[ps.b()], inc=(k == 7))
                            S.op("act", lambda e: e.copy(out=U.t[:, ts], in_=ps.t[:]), reads=[ps.b()], writes=[U.b()])
                        src = U
                        sh = 1
                        while sh < w:
                            dst = A if src is not A else B
                            S.op("dve", lambda e: e.tensor_tensor(out=dst.t[:, sh:T], in0=src.t[:, sh:T], in1=src.t[:, 0:T - sh], op=ALU.add), reads=[src.b()], writes=[dst.b()])
                            S.op("dve", lambda e: e.tensor_copy(out=dst.t[:, 0:sh], in_=src.t[:, 0:sh]), reads=[src.b()], writes=[dst.b()])
                            src = dst
                            sh *= 2
                        S.op("dve", lambda e: e.scalar_tensor_tensor(out=DT.t[:], in0=src.t[:], scalar=1.0 / w, in1=U.t[:], op0=ALU.mult, op1=ALU.subtract),
                             reads=[src.b(), U.b()], writes=[DT.b()])
                        S.op("dve", lambda e: e.tensor_tensor(out=tfix.t[:, 0:w - 1], in0=src.t[:, 0:w - 1], in1=cstf.t[:, 0:w - 1], op=ALU.mult), reads=[src.b(), cstf.b()], writes=[tfix.b()])
                        S.op("dve", lambda e: e.tensor_tensor(out=DT.t[:, 0:w - 1], in0=tfix.t[:, 0:w - 1], in1=U.t[:, 0:w - 1], op=ALU.subtract), reads=[tfix.b(), U.b()], writes=[DT.b()])
                        for tb in range(NTB):
                            ts = slice(tb * 512, (tb + 1) * 512)
                            ps = psr()
                            S.op("pe", lambda e: e.matmul(ps.t[:], wpo.t[:, g, :], DT.t[:, ts], start=True, stop=True), reads=[wpo.b(), DT.b()], writes=[ps.b()])
                            S.op("dve", lambda e: e.tensor_scalar(out=CT.t[:, g, ts], in0=ps.t[:], scalar1=vec.t[:, vb + V_PSC + g:vb + V_PSC + g + 1], scalar2=None, op0=ALU.mult),
                                 reads=[ps.b(), vec.b()], writes=[CT.b((g, tb))])
                    S.barrier()
                dump_bf16("oc", CT, [(g, tb) for g in range(4) for tb in range(NTB)], 4)
                epilogue(l, 2, lambda kc, ts: CT.t[:, kc, ts], lambda kc, tb: [CT.b((kc, tb))], 4, 128)

        def moe(l):
            vb = l * NVL
            with ExitStack() as ph:
                combT = sb(ph, "combT", [32, T], F32)
                with ExitStack() as p1:
                    h2f = sb(p1, "h2f", [128, 8, 512], F32)
                    wr = sb(p1, "wr", [128, 8, 32], F32)
                    LOG = sb(p1, "LOG", [128, 16, 32], F32)
                    EX = sb(p1, "EX", [128, 16, 32], F32)
                    MSK = sb(p1, "MSK", [128, 16, 32], F32)
                    mx8 = sb(p1, "mx8", [128, 16, 8], F32)
                    negmax = sb(p1, "negmax", [128, 16], F32)
                    den = sb(p1, "den", [128, 16], F32)
                    S.dma("sp", lambda q: q.dma_start(out=wr.t[:], in_=wr_d[l]), d_w[0], writes=[wr.b()])

                    def want(k, tb, tm, biasap):
                        if k is not None:
                            S.op("act", lambda e: e.activation(out=h2f.t[:, k, :], in_=tm.t[:], func=AF.Identity, bias=biasap), reads=[tm.b(), modT.b()], writes=[h2f.b(k)])
                        else:
                            ps = psr()
                            for j in range(4):
                                for k2 in range(8):
                                    S.op("pe", lambda e: e.matmul(ps.t[:, j * 32:(j + 1) * 32], h2f.t[:, k2, j * 128:(j + 1) * 128], wr.t[:, k2, :], start=(k2 == 0), stop=(k2 == 7)),
                                         reads=[h2f.b(k2), wr.b()], writes=[ps.b()], inc=(k2 == 7 and j == 3))
                            S.op("dve", lambda e: e.tensor_tensor(out=LOG.t[:, tb * 4:(tb + 1) * 4, :], in0=ps.t[:, 0:128].rearrange("p (a b) -> p a b", a=4),
                                                                   in1=vec.t[:, vb + V_BR:vb + V_BR + 32].rearrange("p (o b) -> p o b", o=1).broadcast_to([128, 4, 32]), op=ALU.add),
                                 reads=[ps.b(), vec.b()], writes=[LOG.b()])

                    rmsnorm_mod(l, 1, want_f32=want)
                    for tt in range(16):
                        S.op("dve", lambda e: e.max(out=mx8.t[:, tt, :], in_=LOG.t[:, tt, :]), reads=[LOG.b()], writes=[mx8.b()])
                    S.op("dve", lambda e: e.tensor_scalar(out=negmax.t[:], in0=mx8.t[:, :, 0], scalar1=-1.0, scalar2=None, op0=ALU.mult), reads=[mx8.b()], writes=[negmax.b()])
                    for tt in range(16):
                        S.op("act", lambda e: e.activation(out=EX.t[:, tt, :], in_=LOG.t[:, tt, :], func=AF.Exp, bias=negmax.t[:, tt:tt + 1]), reads=[LOG.b(), negmax.b()], writes=[EX.b()])
                        S.op("dve", lambda e: e.tensor_scalar(out=MSK.t[:, tt, :], in0=LOG.t[:, tt, :], scalar1=mx8.t[:, tt, 3:4], scalar2=None, op0=ALU.is_ge), reads=[LOG.b(), mx8.b()], writes=[MSK.b()])
                    S.op("dve", lambda e: e.tensor_tensor(out=EX.t[:], in0=EX.t[:], in1=MSK.t[:], op=ALU.mult), reads=[EX.b(), MSK.b()], writes=[EX.b()])
                    S.op("dve", lambda e: e.tensor_reduce(out=den.t[:], in_=EX.t[:], axis=mybir.AxisListType.X, op=ALU.add), reads=[EX.b()], writes=[den.b()])
                    S.op("dve", lambda e: e.reciprocal(out=den.t[:], in_=den.t[:]), reads=[den.b()], writes=[den.b()])
                    S.op("dve", lambda e: e.tensor_tensor(out=EX.t[:], in0=EX.t[:], in1=den.t[:, :].rearrange("p (a o) -> p a o", o=1).broadcast_to([128, 16, 32]), op=ALU.mult),
                         reads=[EX.b(), den.b()], writes=[EX.b()])
                    for t4 in range(4):
                        ps = psr()
                        for j in range(4):
                            tt = t4 * 4 + j
                            S.op("pe", lambda e: e.transpose(ps.t[0:32, j * 128:(j + 1) * 128], EX.t[:, tt, :], identf.t[:]), reads=[EX.b(), identf.b()], writes=[ps.b()], inc=(j == 3))
                        S.op("act", lambda e: e.copy(out=combT.t[0:32, t4 * 512:(t4 + 1) * 512], in_=ps.t[0:32, :]), reads=[ps.b()], writes=[combT.b()])
                    S.barrier()
                if l == 0:
                    dump("comb0", [combT.b()], combT.t[:, :])
                with ExitStack() as p2:
                    b1l1 = sb(p2, "b1l1", [128, NE, 8], F32)
                    actT = sb(p2, "actT", [128, 8, T], BF16)
                    CB = sb(p2, "CB", [128, T], F32)
                    Lsel = [sb(p2, f"Lsel{i}", [32, 128], F32) for i in range(2)]
                    NR = 4
                    ND = 4
                    w1r = [sb(p2, f"w1r{i}", [128, 8, 256], BF16) for i in range(NR)]
                    w2r = [sb(p2, f"w2r{i}", [128, 8, 128], BF16) for i in range(NR)]
                    d_w1 = [S.dma_sem(f"d_w1_{l}_{i}") for i in range(NR)]
                    d_w2 = [S.dma_sem(f"d_w2_{l}_{i}") for i in range(NR)]
                    b1v = vec.t[:, vb + V_B1:vb + V_B1 + 512].rearrange("p (e c) -> p e c", c=16)
                    S.op("dve", lambda e: e.tensor_scalar(out=b1l1.t[:], in0=b1v[:, :, 8:16], scalar1=1.0, scalar2=None, op0=ALU.add), reads=[vec.b()], writes=[b1l1.b()])

                    def issue_w1(n):
                        if n < NE * 8:
                            e_, j_ = divmod(n, 8)
                            w = w1r[n % NR]
                            S.dma("pool", lambda q: q.dma_start(out=w.t[:], in_=w1_d[l, e_, j_], max_dma_last_dim=8192), d_w1[n % NR], writes=[w.b()])

                    def issue_w2(n):
                        if n < NE * 8:
                            e_, o_ = divmod(n, 8)
                            w = w2r[n % NR]
                            S.dma("pool", lambda q: q.dma_start(out=w.t[:], in_=w2_d[l, e_, o_], max_dma_last_dim=8192), d_w2[n % NR], writes=[w.b()])

                    for n in range(NR - 1):
                        issue_w1(n)
                        issue_w2(n)
                    with ExitStack() as pb:
                        b2s = sb(pb, "b2s", [32, 1024], F32)
                        S.dma("sp", lambda q: q.dma_start(out=b2s.t[:], in_=b2_d[l]), d_w[0], writes=[b2s.b()])
                        for tb in range(NTB):
                            ts = slice(tb * 512, (tb + 1) * 512)
                            for o in range(8):
                                ps = psr()
                                S.op("pe", lambda e: e.matmul(ps.t[:], b2s.t[0:32, o * 128:(o + 1) * 128], combT.t[0:32, ts], start=True, stop=True), reads=[b2s.b(), combT.b()], writes=[ps.b()])
                                S.op("dve", lambda e: e.scalar_tensor_tensor(out=xT.t[:, o, ts], in0=ps.t[:], scalar=modT.t[:, 40 + o:41 + o], in1=xT.t[:, o, ts], op0=ALU.mult, op1=ALU.add),
                                     reads=[ps.b(), modT.b(), XB(o, tb)], writes=[XB(o, tb)])
                        S.barrier()
                    tA = [sb(p2, f"tA{i}", [128, 512], F32) for i in range(ND)]
                    tC = [sb(p2, f"tC{i}", [128, 512], F32) for i in range(ND)]
                    groups = [(j, tb) for j in range(8) for tb in range(NTB)]
                    NG = len(groups)
                    for ex in range(NE):
                        ls = Lsel[ex % 2]
                        S.op("dve", lambda e: e.tensor_scalar(out=ls.t[:], in0=onesf.t[0:32, :], scalar1=identf.t[0:32, ex:ex + 1], scalar2=None, op0=ALU.mult),
                             reads=[onesf.b(), identf.b()], writes=[ls.b()])
                        for tb in range(NTB):
                            ts = slice(tb * 512, (tb + 1) * 512)
                            ps = psr()
                            S.op("pe", lambda e: e.matmul(ps.t[:], ls.t[:], combT.t[0:32, ts], start=True, stop=True), reads=[ls.b(), combT.b()], writes=[ps.b()])
                            S.op("act", lambda e: e.copy(out=CB.t[:, ts], in_=ps.t[:]), reads=[ps.b()], writes=[CB.b(tb)])

                        def stage01(m):
                            j, tb = groups[m]
                            ts = slice(tb * 512, (tb + 1) * 512)
                            n1 = ex * 8 + j
                            if tb == 0:
                                issue_w1(n1 + NR - 1)
                            w = w1r[n1 % NR]
                            bg = vec.t[:, vb + V_B1 + ex * 16 + j:vb + V_B1 + ex * 16 + j + 1]
                            pg = psr()
                            pl = psr()
                            for k in range(8):
                                S.op("pe", lambda e: e.matmul(pg.t[:], w.t[:, k, 0:128], hT.t[:, k, ts], start=(k == 0), stop=(k == 7)), reads=[w.b(), HB(k, tb)], writes=[pg.b()], inc=(k == 7))
                            for k in range(8):
                                S.op("pe", lambda e: e.matmul(pl.t[:], w.t[:, k, 128:256], hT.t[:, k, ts], start=(k == 0), stop=(k == 7)), reads=[w.b(), HB(k, tb)], writes=[pl.b()], inc=(k == 7))
                            a, c_ = tA[m % ND], tC[m % ND]
                            S.op("dve", lambda e: e.tensor_scalar(out=a.t[:], in0=pg.t[:], scalar1=bg, scalar2=7.0, op0=ALU.add, op1=ALU.min), reads=[pg.b(), vec.b()], writes=[a.b()])
                            S.op("act", lambda e: e.activation(out=c_.t[:], in_=pl.t[:], func=AF.Identity, bias=b1l1.t[:, ex, j:j + 1]), reads=[pl.b(), b1l1.b()], writes=[c_.b()])

                        def stage2(m):
                            j, tb = groups[m]
                            ts = slice(tb * 512, (tb + 1) * 512)
                            a, c_ = tA[m % ND], tC[m % ND]
                            S.op("act", lambda e: e.activation(out=a.t[:], in_=a.t[:], func=AF.Gelu_apprx_sigmoid), reads=[a.b()], writes=[a.b()])
                            S.op("pool", lambda e: e.tensor_scalar(out=c_.t[:], in0=c_.t[:], scalar1=8.0, scalar2=-6.0, op0=ALU.min, op1=ALU.max), reads=[c_.b()], writes=[c_.b()])
                            S.op("pool", lambda e: e.tensor_tensor(out=c_.t[:], in0=c_.t[:], in1=CB.t[:, ts], op=ALU.mult), reads=[c_.b(), CB.b(tb)], writes=[c_.b()])

                        def stage3(m):
                            j, tb = groups[m]
                            ts = slice(tb * 512, (tb + 1) * 512)
                            a, c_ = tA[m % ND], tC[m % ND]
                            S.op("dve", lambda e: e.tensor_tensor(out=actT.t[:, j, ts], in0=a.t[:], in1=c_.t[:], op=ALU.mult), reads=[a.b(), c_.b()], writes=[actT.b((j, tb))])

                        for step in range(NG + 2):
                            if step < NG:
                                stage01(step)
                            if 0 <= step - 1 < NG:
                                stage2(step - 1)
                            if 0 <= step - 2 < NG:
                                stage3(step - 2)
                        for o in range(8):
                            n2 = ex * 8 + o
                            issue_w2(n2 + NR - 1)
                            w = w2r[n2 % NR]
                            for tb in range(NTB):
                                ts = slice(tb * 512, (tb + 1) * 512)
                                ps = psr()
                                for k in range(8):
                                    S.op("pe", lambda e: e.matmul(ps.t[:], w.t[:, k, :], actT.t[:, k, ts], start=(k == 0), stop=(k == 7)), reads=[w.b(), actT.b((k, tb))], writes=[ps.b()], inc=(k == 7))
                                S.op("dve", lambda e: e.scalar_tensor_tensor(out=xT.t[:, o, ts], in0=ps.t[:], scalar=modT.t[:, 40 + o:41 + o], in1=xT.t[:, o, ts], op0=ALU.mult, op1=ALU.add),
                                     reads=[ps.b(), modT.b(), XB(o, tb)], writes=[XB(o, tb)])
                    S.barrier()

        XPB = Buf()
        YPB = Buf()
        d_ind = [S.dma_sem(f"d_ind{i}") for i in range(5)]

        def moe_sparse(l):
            vb = l * NVL
            AX = mybir.AxisListType.X
            with ExitStack() as ph:
                IDXi = sb(ph, "IDXi", [128, 4, 16], I32)
                WK = sb(ph, "WK", [128, 4, 16], F32)
                W1I = sb(ph, "W1I", [128, NT, 8], I32)
                W2I = sb(ph, "W2I", [128, NT, 8], I32)
                BI = sb(ph, "BI", [128, 2, NT], I32)
                with ExitStack() as p1:
                    LOG = sb(p1, "LOG", [128, 16, 32], F32)
                    EX = sb(p1, "EX", [128, 16, 32], F32)
                    MSK = sb(p1, "MSK", [128, 16, 32], F32)
                    mx8 = sb(p1, "mx8", [128, 16, 8], F32)
                    negmax = sb(p1, "negmax", [128, 16], F32)
                    den = sb(p1, "den", [128, 16], F32)
                    with ExitStack() as p0:
                        h2f = sb(p0, "h2f", [128, 8, 512], F32)
                        wr = sb(p0, "wr", [128, 8, 32], F32)
                        S.dma("sp", lambda q: q.dma_start(out=wr.t[:], in_=wr_d[l]), d_w[0], writes=[wr.b()])

                        def want(k, tb, tm, biasap):
                            if k is not None:
                                S.op("act", lambda e: e.activation(out=h2f.t[:, k, :], in_=tm.t[:], func=AF.Identity, bias=biasap), reads=[tm.b(), modT.b()], writes=[h2f.b(k)])
                            else:
                                ps = psr()
                                for j in range(4):
                                    for k2 in range(8):
                                        S.op("pe", lambda e: e.matmul(ps.t[:, j * 32:(j + 1) * 32], h2f.t[:, k2, j * 128:(j + 1) * 128], wr.t[:, k2, :], start=(k2 == 0), stop=(k2 == 7)),
                                             reads=[h2f.b(k2), wr.b()], writes=[ps.b()], inc=(k2 == 7 and j == 3))
                                S.op("dve", lambda e: e.tensor_tensor(out=LOG.t[:, tb * 4:(tb + 1) * 4, :], in0=ps.t[:, 0:128].rearrange("p (a b) -> p a b", a=4),
                                                                       in1=vec.t[:, vb + V_BR:vb + V_BR + 32].rearrange("p (o b) -> p o b", o=1).broadcast_to([128, 4, 32]), op=ALU.add),
                                     reads=[ps.b(), vec.b()], writes=[LOG.b()])

                        rmsnorm_mod(l, 1, want_f32=want)
                    for tt in range(16):
                        S.op("dve", lambda e: e.max(out=mx8.t[:, tt, :], in_=LOG.t[:, tt, :]), reads=[LOG.b()], writes=[mx8.b()])
                    S.op("dve", lambda e: e.tensor_scalar(out=negmax.t[:], in0=mx8.t[:, :, 0], scalar1=-1.0, scalar2=None, op0=ALU.mult), reads=[mx8.b()], writes=[negmax.b()])
                    for tt in range(16):
                        S.op("act", lambda e: e.activation(out=EX.t[:, tt, :], in_=LOG.t[:, tt, :], func=AF.Exp, bias=negmax.t[:, tt:tt + 1]), reads=[LOG.b(), negmax.b()], writes=[EX.b()])
                        S.op("dve", lambda e: e.tensor_scalar(out=MSK.t[:, tt, :], in0=LOG.t[:, tt, :], scalar1=mx8.t[:, tt, 3:4], scalar2=None, op0=ALU.is_ge), reads=[LOG.b(), mx8.b()], writes=[MSK.b()])
                    S.op("dve", lambda e: e.tensor_tensor(out=EX.t[:], in0=EX.t[:], in1=MSK.t[:], op=ALU.mult), reads=[EX.b(), MSK.b()], writes=[EX.b()])
                    S.op("dve", lambda e: e.tensor_reduce(out=den.t[:], in_=EX.t[:], axis=AX, op=ALU.add), reads=[EX.b()], writes=[den.b()])
                    S.op("dve", lambda e: e.reciprocal(out=den.t[:], in_=den.t[:]), reads=[den.b()], writes=[den.b()])
                    S.op("dve", lambda e: e.tensor_tensor(out=EX.t[:], in0=EX.t[:], in1=den.t[:, :].rearrange("p (a o) -> p a o", o=1).broadcast_to([128, 16, 32]), op=ALU.mult),
                         reads=[EX.b(), den.b()], writes=[EX.b()])
                    MSKb = sb(p1, "MSKb", [128, 16, 32], BF16)
                    RANK = sb(p1, "RANK", [128, 16, 32], F32)
                    G = sb(p1, "G", [128, 16, 32], F32)
                    OH = sb(p1, "OH", [128, 16, 32], F32)
                    TMP = sb(p1, "TMP", [128, 16, 32], F32)
                    CNT = sb(p1, "CNT", [128, 32], F32)
                    CNTi = sb(p1, "CNTi", [128, 32], I32)
                    NTf = sb(p1, "NTf", [128, 32], F32)
                    TB = sb(p1, "TB", [128, 32], F32)
                    IDXf = sb(p1, "IDXf", [128, 4, 16], F32)
                    TBc = sb(p1, "TBc", [32, 1], F32)
                    t32 = sb(p1, "t32", [32, 32], F32)
                    CMPb = sb(p1, "CMPb", [32, NT], BF16)
                    TEXP = sb(p1, "TEXP", [128, NT], F32)
                    BASE = sb(p1, "BASE", [128, NT], F32)
                    UNU = sb(p1, "UNU", [128, NT], F32)
                    WIf = sb(p1, "WIf", [128, NT, 8], F32)
                    BIf = sb(p1, "BIf", [128, 2, NT], F32)
                    S.op("dve", lambda e: e.tensor_copy(out=MSKb.t[:], in_=MSK.t[:]), reads=[MSK.b()], writes=[MSKb.b()])
                    for tt in range(16):
                        cols = slice(tt * 32, (tt + 1) * 32)
                        for t2 in range(tt):
                            S.op("pe", lambda e: e.matmul(ACC0.t[:, cols], onesb.t[:], MSKb.t[:, t2, :], start=(t2 == 0), stop=False),
                                 reads=[onesb.b(), MSKb.b()], writes=[ACC0.b()], inc=False)
                        S.op("pe", lambda e: e.matmul(ACC0.t[:, cols], trib.t[:], MSKb.t[:, tt, :], start=(tt == 0), stop=True),
                             reads=[trib.b(), MSKb.b()], writes=[ACC0.b()])
                    S.op("dve", lambda e: e.tensor_tensor(out=RANK.t[:].rearrange("p a b -> p (a b)"), in0=ACC0.t[:], in1=MSK.t[:].rearrange("p a b -> p (a b)"), op=ALU.subtract),
                         reads=[ACC0.b(), MSK.b()], writes=[RANK.b()])
                    pc = psr()
                    for tt in range(16):
                        S.op("pe", lambda e: e.matmul(pc.t[:, 0:32], onesb.t[:], MSKb.t[:, tt, :], start=(tt == 0), stop=(tt == 15)),
                             reads=[onesb.b(), MSKb.b()], writes=[pc.b()], inc=(tt == 15))
                    S.op("dve", lambda e: e.tensor_scalar(out=CNT.t[:], in0=pc.t[:, 0:32], scalar1=float(TS - 1), scalar2=None, op0=ALU.add), reads=[pc.b()], writes=[CNT.b()])
                    S.op("dve", lambda e: e.tensor_copy(out=CNTi.t[:], in_=CNT.t[:]), reads=[CNT.b()], writes=[CNTi.b()])
                    S.op("dve", lambda e: e.tensor_single_scalar(out=CNTi.t[:], in_=CNTi.t[:], scalar=9, op=ALU.arith_shift_right), reads=[CNTi.b()], writes=[CNTi.b()])
                    S.op("dve", lambda e: e.tensor_copy(out=NTf.t[:], in_=CNTi.t[:]), reads=[CNTi.b()], writes=[NTf.b()])
                    S.op("dve", lambda e: e.tensor_tensor_scan(out=TB.t[:], data0=onesf.t[:, 0:32], data1=NTf.t[:], initial=0.0, op0=ALU.mult, op1=ALU.add),
                         reads=[onesf.b(), NTf.b()], writes=[TB.b()])
                    S.op("dve", lambda e: e.tensor_scalar(out=UNU.t[:], in0=cstf.t[:, 19:19 + NT], scalar1=TB.t[:, 31:32], scalar2=1.0e6, op0=ALU.is_ge, op1=ALU.mult),
                         reads=[cstf.b(), TB.b()], writes=[UNU.b()])
                    S.op("dve", lambda e: e.tensor_tensor(out=TB.t[:], in0=TB.t[:], in1=NTf.t[:], op=ALU.subtract), reads=[TB.b(), NTf.b()], writes=[TB.b()])
                    S.op("dve", lambda e: e.scalar_tensor_tensor(out=G.t[:], in0=TB.t[:, :].rearrange("p (o b) -> p o b", o=1).broadcast_to([128, 16, 32]), scalar=float(TS), in1=RANK.t[:],
                                                                  op0=ALU.mult, op1=ALU.add), reads=[TB.b(), RANK.b()], writes=[G.b()])
                    for k in range(4):
                        S.op("dve", lambda e: e.tensor_tensor(out=OH.t[:], in0=LOG.t[:], in1=mx8.t[:, :, k:k + 1].broadcast_to([128, 16, 32]), op=ALU.is_equal),
                             reads=[LOG.b(), mx8.b()], writes=[OH.b()])
                        S.op("dve", lambda e: e.tensor_tensor(out=TMP.t[:], in0=OH.t[:], in1=G.t[:], op=ALU.mult), reads=[OH.b(), G.b()], writes=[TMP.b()])
                        S.op("dve", lambda e: e.tensor_reduce(out=IDXf.t[:, k, :], in_=TMP.t[:], axis=AX, op=ALU.add), reads=[TMP.b()], writes=[IDXf.b()])
                        S.op("dve", lambda e: e.tensor_tensor(out=TMP.t[:], in0=OH.t[:], in1=EX.t[:], op=ALU.mult), reads=[OH.b(), EX.b()], writes=[TMP.b()])
                        S.op("dve", lambda e: e.tensor_reduce(out=WK.t[:, k, :], in_=TMP.t[:], axis=AX, op=ALU.add), reads=[TMP.b()], writes=[WK.b()])
                    S.op("dve", lambda e: e.tensor_scalar(out=IDXf.t[:], in0=IDXf.t[:], scalar1=float(NROW - 1), scalar2=0.0, op0=ALU.min, op1=ALU.max), reads=[IDXf.b()], writes=[IDXf.b()])
                    S.op("dve", lambda e: e.tensor_copy(out=IDXi.t[:], in_=IDXf.t[:]), reads=[IDXf.b()], writes=[IDXi.b()])
                    S.op("dve", lambda e: e.tensor_tensor(out=t32.t[:], in0=TB.t[0:32, :], in1=identf.t[0:32, 0:32], op=ALU.mult), reads=[TB.b(), identf.b()], writes=[t32.b()])
                    S.op("dve", lambda e: e.tensor_reduce(out=TBc.t[:], in_=t32.t[:], axis=AX, op=ALU.add), reads=[t32.b()], writes=[TBc.b()])
                    S.op("dve", lambda e: e.tensor_scalar(out=CMPb.t[:], in0=cstf.t[0:32, 19:19 + NT], scalar1=TBc.t[:, 0:1], scalar2=None, op0=ALU.is_ge), reads=[cstf.b(), TBc.b()], writes=[CMPb.b()])
                    pt = psr()
                    S.op("pe", lambda e: e.matmul(pt.t[:, 0:NT], onesb.t[0:32, :], CMPb.t[:], start=True, stop=True), reads=[onesb.b(), CMPb.b()], writes=[pt.b()])
                    S.op("dve", lambda e: e.tensor_scalar(out=TEXP.t[:], in0=pt.t[:, 0:NT], scalar1=-1.0, scalar2=None, op0=ALU.add), reads=[pt.b()], writes=[TEXP.b()])
                    pcol = cstf.t[:, 18:19]
                    S.op("dve", lambda e: e.tensor_scalar(out=BASE.t[:], in0=TEXP.t[:], scalar1=1024.0, scalar2=pcol, op0=ALU.mult, op1=ALU.add), reads=[TEXP.b(), cstf.b()], writes=[BASE.b()])
                    for jj in range(8):
                        S.op("dve", lambda e: e.tensor_scalar(out=WIf.t[:, :, jj], in0=BASE.t[:], scalar1=float(l * 32768 + jj * 128), scalar2=None, op0=ALU.add), reads=[BASE.b()], writes=[WIf.b()])
                    S.op("dve", lambda e: e.tensor_copy(out=W1I.t[:], in_=WIf.t[:]), reads=[WIf.b()], writes=[W1I.b()])
                    S.op("dve", lambda e: e.tensor_scalar(out=BASE.t[:], in0=TEXP.t[:], scalar1=1024.0, scalar2=float(l * 32768), op0=ALU.mult, op1=ALU.add), reads=[TEXP.b()], writes=[BASE.b()])
                    S.op("dve", lambda e: e.scalar_tensor_tensor(out=BASE.t[:], in0=cstf.t[:, 18:19].broadcast_to([128, NT]), scalar=8.0, in1=BASE.t[:], op0=ALU.mult, op1=ALU.add),
                         reads=[cstf.b(), BASE.b()], writes=[BASE.b()])
                    for k in range(8):
                        S.op("dve", lambda e: e.tensor_scalar(out=WIf.t[:, :, k], in0=BASE.t[:], scalar1=float(k), scalar2=None, op0=ALU.add), reads=[BASE.b()], writes=[WIf.b()])
                    S.op("dve", lambda e: e.tensor_copy(out=W2I.t[:], in_=WIf.t[:]), reads=[WIf.b()], writes=[W2I.b()])
                    S.op("dve", lambda e: e.tensor_scalar(out=BIf.t[:, 0, :], in0=TEXP.t[:], scalar1=128.0, scalar2=pcol, op0=ALU.mult, op1=ALU.add), reads=[TEXP.b(), cstf.b()], writes=[BIf.b()])
                    S.op("dve", lambda e: e.tensor_scalar(out=BIf.t[:, 0, :], in0=BIf.t[:, 0, :], scalar1=float(l * 4096), scalar2=None, op0=ALU.add), reads=[BIf.b()], writes=[BIf.b()])
                    S.op("dve", lambda e: e.tensor_scalar(out=BIf.t[:, 1, :], in0=TEXP.t[:], scalar1=float(l * 32), scalar2=None, op0=ALU.add), reads=[TEXP.b()], writes=[BIf.b()])
                    S.op("dve", lambda e: e.tensor_copy(out=BI.t[:], in_=BIf.t[:]), reads=[BIf.b()], writes=[BI.b()])
                    if l == 0 and "texp" in dbg_out:
                        dump("texp", [TEXP.b()], TEXP.t[:, :])
                        dump("idxf", [IDXf.b()], IDXf.t[:, :, :].rearrange("p a b -> p (a b)"))
                        dump("wk", [WK.b()], WK.t[:, :, :].rearrange("p a b -> p (a b)"))
                        dump("w1i", [W1I.b()], W1I.t[:, :, :].rearrange("p a b -> p (a b)"))
                        dump("w2i", [W2I.b()], W2I.t[:, :, :].rearrange("p a b -> p (a b)"))
                        dump("bi", [BI.b()], BI.t[:, :, :].rearrange("p a b -> p (a b)"))
                    stg = [sb(p1, f"stg{i}", [128, 1024], BF16) for i in range(2)]
                    d_stg = [S.dma_sem(f"d_stg_{l}_{i}") for i in range(2)]
                    for tt in range(16 if SPARSE_STOP != 1 else 0):
                        for k in range(8):
                            S.op("pe", lambda e: e.transpose(PSBF.t[:, k * 128:(k + 1) * 128], hT.t[:, k, tt * 128:(tt + 1) * 128], identb.t[:]),
                                 reads=[HB(k, tt // 4), identb.b()], writes=[PSBF.b()], inc=(k == 7))
                        st = stg[tt % 2]
                        S.op("act", lambda e: e.copy(out=st.t[:], in_=PSBF.t[:, :]), reads=[PSBF.b()], writes=[st.b()])
                        for k in range(4):
                            S.dma("pool", lambda q: q.indirect_dma_start(out=xp_d[:, :], out_offset=bass.IndirectOffsetOnAxis(ap=IDXi.t[:, k, tt:tt + 1], axis=0), in_=st.t[:, :], in_offset=None),
                                  d_stg[tt % 2], reads=[st.b(), IDXi.b()])
                    S.barrier()
                if SPARSE_STOP in (1, 2):
                    return
                with ExitStack() as p2:
                    Xtok = sb(p2, "Xtok", [128, 4, 1024], BF16)
                    XT = [sb(p2, f"XT{i}", [128, 8, TS], BF16) for i in range(2)]
                    NR = 6
                    ND = 3
                    w1r = [sb(p2, f"w1r{i}", [128, 8, 256], BF16) for i in range(NR)]
                    d_w1 = [S.dma_sem(f"d_w1s_{l}_{i}") for i in range(NR)]
                    d_w2 = [S.dma_sem(f"d_w2s_{l}_{i}") for i in range(2)]
                    d_b = [S.dma_sem(f"d_bs_{l}_{i}") for i in range(2)]
                    d_b2 = [S.dma_sem(f"d_b2s_{l}_{i}") for i in range(2)]
                    d_x = S.dma_sem(f"d_x_{l}")
                    d_y = [S.dma_sem(f"d_y_{l}_{i}") for i in range(2)]
                    W2B = [Buf(), Buf()]
                    w2v = [hT.t[:, :, i * 1024:(i + 1) * 1024] for i in range(2)]
                    b1t = [sb(p2, f"b1t{i}", [128, 16], F32) for i in range(2)]
                    b2bc = [sb(p2, f"b2bc{i}", [128, 1024], F32) for i in range(2)]
                    actT = sb(p2, "actTs", [128, 8, TS], BF16)
                    tA = [sb(p2, f"tA{i}", [128, TS], F32) for i in range(ND)]
                    tC = [sb(p2, f"tC{i}", [128, TS], F32) for i in range(ND)]
                    Yo = [sb(p2, f"Yo{i}", [128, 1024], F32) for i in range(2)]
                    w1rows = w1_d.rearrange("l e j p k n -> (l e j p) (k n)")
                    w2rows = w2f_d.rearrange("l e p k o -> (l e p k) o")
                    b2rows = b2_d.rearrange("l e o -> (l e) o")

                    def issue_w1(n):
                        if n < NT * 8:
                            j_, jj_ = divmod(n, 8)
                            w = w1r[n % NR]
                            S.dma("pool", lambda q: q.indirect_dma_start(out=w.t[:, :, :].rearrange("p k n -> p (k n)"), out_offset=None, in_=w1rows,
                                                                          in_offset=bass.IndirectOffsetOnAxis(ap=W1I.t[:, j_, jj_:jj_ + 1], axis=0)),
                                  d_w1[n % NR], reads=[W1I.b()], writes=[w.b()])

                    def issue_w2_piece(j_, k):
                        if j_ < NT:
                            i = j_ % 2
                            S.dma("pool", lambda q: q.indirect_dma_start(out=w2v[i][:, k, :], out_offset=None, in_=w2rows,
                                                                          in_offset=bass.IndirectOffsetOnAxis(ap=W2I.t[:, j_, k:k + 1], axis=0)),
                                  d_w2[i], reads=[W2I.b()], writes=[W2B[i]], no_waw=True)

                    def issue_bias(j_):
                        if j_ < NT:
                            i = j_ % 2
                            S.dma("pool", lambda q: q.indirect_dma_start(out=b1t[i].t[:, :], out_offset=None, in_=b1r_d,
                                                                          in_offset=bass.IndirectOffsetOnAxis(ap=BI.t[:, 0, j_:j_ + 1], axis=0)),
                                  d_b[i], reads=[BI.b()], writes=[b1t[i].b()])
                            S.dma("pool", lambda q: q.indirect_dma_start(out=b2bc[i].t[:, :], out_offset=None, in_=b2rows,
                                                                          in_offset=bass.IndirectOffsetOnAxis(ap=BI.t[:, 1, j_:j_ + 1], axis=0)),
                                  d_b2[i], reads=[BI.b()], writes=[b2bc[i].b()])

                    def issue_tile_misc(j_):
                        issue_bias(j_)
                        for k in range(8):
                            issue_w2_piece(j_, k)

                    def load_x(j_):
                        if j_ < NT:
                            S.dma("sp", lambda q: q.dma_start(out=Xtok.t[:], in_=xp_d[j_ * TS:(j_ + 1) * TS, :].rearrange("(a p) f -> p a f", p=128)), d_x, writes=[Xtok.b()])
                            xt = XT[j_ % 2]
                            for kk in range(4):
                                for k in (2 * kk, 2 * kk + 1):
                                    for it in range(4):
                                        S.op("pe", lambda e: e.transpose(PSBF.t[:, (k % 2) * 512 + it * 128:(k % 2) * 512 + (it + 1) * 128], Xtok.t[:, it, k * 128:(k + 1) * 128], identb.t[:]),
                                             reads=[Xtok.b(), identb.b()], writes=[PSBF.b()], inc=(k % 2 == 1 and it == 3))
                                S.op("act", lambda e: e.copy(out=xt.t[:, 2 * kk:2 * kk + 2, :], in_=PSBF.t[:, :].rearrange("p (a b) -> p a b", a=2)), reads=[PSBF.b()], writes=[xt.b()])

                    for n in range(NR - 1):
                        issue_w1(n)
                    issue_tile_misc(0)
                    load_x(0)
                    for j in range(NT):
                        i2 = j % 2
                        xt = XT[i2]
                        S.op("dve", lambda e: e.tensor_scalar(out=b1t[i2].t[:, 8:16], in0=b1t[i2].t[:, 8:16], scalar1=1.0, scalar2=None, op0=ALU.add), reads=[b1t[i2].b()], writes=[b1t[i2].b()])

                        def stage01(m):
                            n1 = j * 8 + m
                            issue_w1(n1 + NR - 1)
                            if m == 0:
                                issue_bias(j + 1)
                            issue_w2_piece(j + 1, m)
                            w = w1r[n1 % NR]
                            pg = psr()
                            pl = psr()
                            for k in range(8):
                                S.op("pe", lambda e: e.matmul(pg.t[:], w.t[:, k, 0:128], xt.t[:, k, :], start=(k == 0), stop=(k == 7)), reads=[w.b(), xt.b()], writes=[pg.b()], inc=(k == 7))
                            for k in range(8):
                                S.op("pe", lambda e: e.matmul(pl.t[:], w.t[:, k, 128:256], xt.t[:, k, :], start=(k == 0), stop=(k == 7)), reads=[w.b(), xt.b()], writes=[pl.b()], inc=(k == 7))
                            a, c_ = tA[m % ND], tC[m % ND]
                            S.op("dve", lambda e: e.tensor_scalar(out=a.t[:], in0=pg.t[:], scalar1=b1t[i2].t[:, m:m + 1], scalar2=7.0, op0=ALU.add, op1=ALU.min), reads=[pg.b(), b1t[i2].b()], writes=[a.b()])
                            S.op("act", lambda e: e.activation(out=c_.t[:], in_=pl.t[:], func=AF.Identity, bias=b1t[i2].t[:, 8 + m:9 + m]), reads=[pl.b(), b1t[i2].b()], writes=[c_.b()])

                        def stage2(m):
                            a, c_ = tA[m % ND], tC[m % ND]
                            S.op("act", lambda e: e.activation(out=a.t[:], in_=a.t[:], func=AF.Gelu_apprx_sigmoid), reads=[a.b()], writes=[a.b()])
                            S.op("pool", lambda e: e.tensor_scalar(out=c_.t[:], in0=c_.t[:], scalar1=8.0, scalar2=-6.0, op0=ALU.min, op1=ALU.max), reads=[c_.b()], writes=[c_.b()])

                        def stage3(m):
                            a, c_ = tA[m % ND], tC[m % ND]
                            S.op("dve", lambda e: e.tensor_tensor(out=actT.t[:, m, :], in0=a.t[:], in1=c_.t[:], op=ALU.mult), reads=[a.b(), c_.b()], writes=[actT.b(m)])

                        for step in range(8 + 2):
                            if step < 8:
                                stage01(step)
                            if 0 <= step - 1 < 8:
                                stage2(step - 1)
                            if 0 <= step - 2 < 8:
                                stage3(step - 2)
                        load_x(j + 1)
                        for it in range(4):
                            yo = Yo[it % 2]
                            for half in range(2):
                                ps = psr()
                                for k in range(8):
                                    S.op("pe", lambda e: e.matmul(ps.t[:], actT.t[:, k, it * 128:(it + 1) * 128], w2v[i2][:, k, half * 512:(half + 1) * 512], start=(k == 0), stop=(k == 7)),
                                         reads=[actT.b(k), W2B[i2]], writes=[ps.b()], inc=(k == 7))
                                S.op("dve", lambda e: e.tensor_tensor(out=yo.t[:, half * 512:(half + 1) * 512], in0=ps.t[:], in1=b2bc[i2].t[:, half * 512:(half + 1) * 512], op=ALU.add),
                                     reads=[ps.b(), b2bc[i2].b()], writes=[yo.b()])
                            S.dma("sp", lambda q: q.dma_start(out=yp_d[j * TS + it * 128:j * TS + (it + 1) * 128, :], in_=yo.t[:]), d_y[it % 2], reads=[yo.b()])
                    S.barrier()
                if SPARSE_STOP == 3:
                    return
                with ExitStack() as p3:
                    Gk = [sb(p3, f"Gk{i}", [128, 1024], F32) for i in range(4)]
                    ACCt = [sb(p3, f"ACCt{i}", [128, 1024], F32) for i in range(2)]
                    for tt in range(16):
                        acc = ACCt[tt % 2]
                        for k in range(4):
                            S.dma("pool", lambda q: q.indirect_dma_start(out=Gk[k].t[:, :], out_offset=None, in_=yp_d[:, :],
                                                                          in_offset=bass.IndirectOffsetOnAxis(ap=IDXi.t[:, k, tt:tt + 1], axis=0)),
                                  d_ind[1 + k], reads=[IDXi.b()], writes=[Gk[k].b()])
                        for k in range(4):
                            if k == 0:
                                S.op("dve", lambda e: e.tensor_scalar(out=acc.t[:], in0=Gk[k].t[:], scalar1=WK.t[:, k, tt:tt + 1], scalar2=None, op0=ALU.mult), reads=[Gk[k].b(), WK.b()], writes=[acc.b()])
                            else:
                                S.op("dve", lambda e: e.scalar_tensor_tensor(out=acc.t[:], in0=Gk[k].t[:], scalar=WK.t[:, k, tt:tt + 1], in1=acc.t[:], op0=ALU.mult, op1=ALU.add),
                                     reads=[Gk[k].b(), WK.b(), acc.b()], writes=[acc.b()])
                        for o4 in range(2):
                            ps = psr()
                            for oo in range(4):
                                o = o4 * 4 + oo
                                S.op("pe", lambda e: e.transpose(ps.t[:, oo * 128:(oo + 1) * 128], acc.t[:, o * 128:(o + 1) * 128], identf.t[:]), reads=[acc.b(), identf.b()], writes=[ps.b()], inc=(oo == 3))
                            for oo in range(4):
                                o = o4 * 4 + oo
                                xs = xT.t[:, o, tt * 128:(tt + 1) * 128]
                                S.op("dve", lambda e: e.scalar_tensor_tensor(out=xs, in0=ps.t[:, oo * 128:(oo + 1) * 128], scalar=modT.t[:, 40 + o:41 + o], in1=xs, op0=ALU.mult, op1=ALU.add),
                                     reads=[ps.b(), modT.b(), XB(o, tt // 4)], writes=[XB(o, tt // 4)])
                    S.barrier()

        def dump_x(name):
            if name in dbg_out:
                for k in range(8):
                    S.dma("sp", lambda q: q.dma_start(out=dbg_out[name][k], in_=xT.t[:, k, :]), d_dbg, reads=[XB(k, tb) for tb in range(NTB)])

        def dump_bf16(name, tl, keys, n):
            if name not in dbg_out:
                return
            with ExitStack() as ph:
                st = sb(ph, "dbgst", [128, T], F32)
                for i in range(n):
                    S.op("dve", lambda e: e.tensor_copy(out=st.t[:], in_=tl.t[:, i, :]), reads=tl.bs(keys), writes=[st.b()])
                    S.dma("sp", lambda q: q.dma_start(out=dbg_out[name][i], in_=st.t[:]), d_dbg, reads=[st.b()])
                S.barrier()

        for l in range(L):
            adaln(l)
            if l == 0:
                dump("mod0", [modT.b()], modT.t[:, :])
            rmsnorm_mod(l, 0)
            if l == 0:
                dump_bf16("h0", hT, [(k, tb) for k in range(8) for tb in range(NTB)], 8)
            if "hgrn" not in SKIP:
                hgrn(l)
            if l == 0:
                dump_x("xa0")
            if STOP_AFTER == "hgrn":
                break
            if "mla" not in SKIP:
                mla(l)
            if STOP_AFTER == "mla":
                break
            if "pool" not in SKIP:
                poolmix(l)
            if l == 0:
                dump_x("xm0")
            if STOP_AFTER == "pool":
                break
            moe_sparse(l)
            if l == 0:
                dump_x("x0")
            if STOP_AFTER == "moe":
                break

        for k in range(8):
            S.dma("sp", lambda q: q.dma_start(out=yT_d[k], in_=xT.t[:, k, :]), d_st, reads=[XB(k, tb) for tb in range(NTB)])
        S.E["sp"].obj.wait_ge(d_st.h, d_st.val)
        if d_dbg.val:
            S.E["sp"].obj.wait_ge(d_dbg.h, d_dbg.val)
    return nc


STOP_AFTER = None
SKIP = set()
SPARSE_STOP = None


def _kp(w, ncols=None):
    K = w.shape[0] // 128
    return np.ascontiguousarray(w.reshape(K, 128, w.shape[1]).transpose(1, 0, 2))


def _col(v, n=128):
    K = v.shape[0] // n
    out = np.zeros((128, K), np.float32)
    out[:n, :] = v.reshape(K, n).T
    return out


def prepare_shared(inp):
    f = np.float32
    sh = {}
    ada_w, w_in = inp["ada_w"], inp["w_in"]
    sh["adaw"] = np.ascontiguousarray(np.stack([np.stack([_kp(ada_w[l][:, ty * 1024:(ty + 1) * 1024]) for ty in range(6)]) for l in range(L)]))
    whg = np.zeros((L, 4, 128, 8, 512), f)
    for l in range(L):
        for hd in range(4):
            for i, off in enumerate((O_HQ, O_HF, O_HI, O_HG)):
                whg[l, hd, :, :, i * 128:(i + 1) * 128] = _kp(w_in[l][:, off + hd * 128:off + (hd + 1) * 128])
    sh["whg"] = whg
    wmla = np.zeros((L, 128, 8, 832), f)
    for l in range(L):
        wmla[l, :, :, 0:640] = _kp(w_in[l][:, O_QL:O_QL + 640])
        r = _kp(w_in[l][:, O_KR:O_KR + 32])
        wmla[l, :, :, 640 + 64:640 + 96] = r
        wmla[l, :, :, 736 + 64:736 + 80] = r[:, :, 16:32]
        wmla[l, :, :, 736 + 80:736 + 96] = r[:, :, 0:16]
    sh["wmla"] = wmla
    sh["wpl"] = np.stack([_kp(w_in[l][:, O_PL:O_PL + 512]) for l in range(L)])
    sh["wgate"] = np.stack([np.stack([_kp(w_in[l][:, o:o + 1024]) for o in (O_GA, O_GB, O_GC)]) for l in range(L)])
    wuq = np.zeros((L, 128, 3, 1536), f)
    for l in range(L):
        a = _kp(inp["w_uq"][l])
        wuq[l, :, :, 0:768] = a
        s = a.reshape(128, 3, 8, 96).copy()
        s2 = s.copy()
        s2[..., 64:80] = s[..., 80:96]
        s2[..., 80:96] = s[..., 64:80]
        wuq[l, :, :, 768:1536] = s2.reshape(128, 3, 768)
    sh["wuq"] = wuq
    wukv = np.zeros((L, 128, 2, 1024), f)
    for l in range(L):
        a = _kp(inp["w_ukv"][l]).reshape(128, 2, 8, 128)
        wukv[l, :, :, 0:512] = a[..., 0:64].reshape(128, 2, 512)
        wukv[l, :, :, 512:1024] = a[..., 64:128].reshape(128, 2, 512)
    sh["wukv"] = wukv
    sh["wpool"] = np.ascontiguousarray(inp["w_pool"].transpose(0, 2, 1, 3))
    sh["wbr"] = np.stack([np.stack([_kp(inp[n][l]) for n in ("w_br_a", "w_br_b", "w_br_c")]) for l in range(L)])
    sh["wout"] = np.stack([_kp(inp["w_out"][l]) for l in range(L)])
    sh["wr"] = np.stack([_kp(inp["w_router"][l]) for l in range(L)])
    w1 = inp["w_exp1"].reshape(L, NE, 8, 128, 2, 8, 128)
    sh["w1"] = np.ascontiguousarray(w1.transpose(0, 1, 5, 3, 2, 4, 6)).reshape(L, NE, 8, 128, 8, 256)
    w2 = inp["w_exp2"].reshape(L, NE, 8, 128, 1024)
    sh["w2f"] = np.ascontiguousarray(w2.transpose(0, 1, 3, 2, 4))
    sh["b1r"] = np.ascontiguousarray(inp["b_exp1"].reshape(L, NE, 16, 128).transpose(0, 1, 3, 2)).reshape(L * NE * 128, 16)
    sh["b2"] = np.ascontiguousarray(inp["b_exp2"])
    vec = np.zeros((128, L * NVL), f)
    for l in range(L):
        b = l * NVL
        vec[:, b + V_N1G:b + V_N1G + 8] = _col(inp["norm1_g"][l])
        vec[:, b + V_N2G:b + V_N2G + 8] = _col(inp["norm2_g"][l])
        vec[:, b + V_ADAB:b + V_ADAB + 48] = _col(inp["ada_b"][l])
        vec[:, b + V_LB:b + V_LB + 4] = _col(inp["hgrn_lb"][l])
        vec[:, b + V_ONG:b + V_ONG + 1] = _col(inp["hgrn_onorm_g"][l])
        vec[:, b + V_QLG:b + V_QLG + 3] = _col(inp["mla_qlat_g"][l])
        vec[:, b + V_KVG:b + V_KVG + 2] = _col(inp["mla_kvlat_g"][l])
        for name, c0, c1 in (("q_norm_g", V_QNG, V_QNGP), ("k_norm_g", V_KNG, V_KNGP)):
            g = inp[name][l]
            vec[:96, b + c0] = g
            gp = g.copy()
            gp[64:80] = g[80:96]
            gp[80:96] = g[64:80]
            vec[:96, b + c1] = gp
        vec[:, b + V_PSC:b + V_PSC + 4] = _col(inp["pool_scale"][l])
        vec[:, b + V_BR:b + V_BR + 32] = np.broadcast_to(inp["b_router"][l][None, :], (128, 32))
        vec[:, b + V_B1:b + V_B1 + 512] = inp["b_exp1"][l].reshape(NE, 16, 128).transpose(2, 0, 1).reshape(128, 512)
    sh["vec"] = vec
    cst = np.zeros((128, NCST), f)
    cst[:, C_ID:C_ID + 128] = np.eye(128, dtype=f)
    cst[:, C_TRI:C_TRI + 128] = np.triu(np.ones((128, 128), f))
    rm = np.ones(T, f)
    rm[::C] = 0.0
    cst[:, C_RMASK:C_RMASK + T] = rm[None, :]
    cst[:, C_INVC:C_INVC + 16] = (1.0 / np.arange(1, 17, dtype=np.float64)).astype(f)[None, :]
    invf = 1.0 / (10000.0 ** (np.arange(0, 32, 2, dtype=np.float64) / 32.0))
    cst[64:80, C_INVF] = (invf / (2 * math.pi)).astype(f)
    cst[80:96, C_INVF] = (invf / (2 * math.pi)).astype(f)
    sc = 2 * math.pi * (1 - 1e-6)
    cst[64:80, C_SINSC] = -sc
    cst[80:96, C_SINSC] = sc
    cst[:, C_PCOL] = np.arange(128, dtype=f)
    cst[:, C_IOTA:C_IOTA + 48] = np.arange(48, dtype=f)[None, :]
    sh["cst"] = cst
    return sh


def kernel(**inputs):
    inp = {k: np.asarray(v) for k, v in inputs.items()}
    B = inp["x"].shape[0]
    sh = prepare_shared(inp)
    in_maps = []
    for b in range(B):
        m = dict(sh)
        m["xT"] = np.ascontiguousarray(inp["x"][b].T.reshape(8, 128, T))
        m["cT"] = np.ascontiguousarray(inp["c"][b].reshape(8, 128).T)
        m["pos"] = np.ascontiguousarray(inp["positions"][b].reshape(1, T).astype(np.int32))
        in_maps.append(m)
    nc = build_program()
    res = run_bass_kernel_spmd(nc, in_maps, core_ids=list(range(B)))
    out = np.empty((B, T, D), np.float32)
    for b in range(B):
        out[b] = res.results[b]["yT"].reshape(D, T).T
    return out
```
